# Optimizing a Trainium2 kernel written in Bass

```python
import math
import jax
import jax.numpy as jnp
from jax import lax
import numpy as np

D_MODEL = 1024
BATCH = 4
SEQ = 4096
DEPTH = 4

HY_WIDTH = D_MODEL // 4
HY_GROUPS = 4
ATT_WIDTH = D_MODEL // 2
ATT_HEADS = 4
ATT_HEAD_DIM = ATT_WIDTH // (2 * ATT_HEADS)
POOL_WINDOWS = (2, 4, 8, 16)
POOL_WIDTH = D_MODEL - HY_WIDTH - ATT_WIDTH
POOL_GROUP = POOL_WIDTH // len(POOL_WINDOWS)
IN_WIDTH = 3 * HY_WIDTH + 3 * ATT_WIDTH + POOL_WIDTH
ROPE_THETA = 500000.0
ROPE_DIM = ATT_HEAD_DIM // 4
Q_BLOCK = 128
HY_EMB = 33
HY_BANDS = (HY_EMB - 1) // 2
HY_FILTER_HIDDEN = 64
HY_DECAY_TARGET = 1e-2
HY_FAST_DECAY = 0.3
HY_SLOW_DECAY = 1.5
HY_SHIFT = 0.0
N_GROUPS = 4
EXPERTS_PER_GROUP = 4
N_EXPERTS = N_GROUPS * EXPERTS_PER_GROUP
TOP_K_IN_GROUP = 2
D_EXPERT = 256
PLE_DIM = 256
LN_EPS = 1e-5
RMS_EPS = 1e-5
DN_ALPHA = (2 * DEPTH) ** 0.25
DN_BETA = (8 * DEPTH) ** -0.25

kernel_name = 'hybrid_hyena_diffattn_pool_hmoe_encoder'


def layer_norm(x, g, b):
    xf = x.astype(jnp.float32)
    mu = jnp.mean(xf, axis=-1, keepdims=True)
    var = jnp.mean(jnp.square(xf - mu), axis=-1, keepdims=True)
    y = (xf - mu) * lax.rsqrt(var + LN_EPS) * g.astype(jnp.float32) + b.astype(jnp.float32)
    return y.astype(x.dtype)


def rms_norm(x, g):
    xf = x.astype(jnp.float32)
    y = xf * lax.rsqrt(jnp.mean(jnp.square(xf), axis=-1, keepdims=True) + RMS_EPS) * g.astype(jnp.float32)
    return y.astype(x.dtype)


def rope_tables(seq_len):
    pos = jnp.arange(seq_len, dtype=jnp.float32)
    inv_freq = jnp.power(ROPE_THETA, -jnp.arange(0, ROPE_DIM, 2, dtype=jnp.float32) / ROPE_DIM)
    ang = pos[:, None] * inv_freq[None, :]
    ang = jnp.concatenate([ang, ang], axis=-1)
    return jnp.cos(ang), jnp.sin(ang)


def apply_partial_rope(x, cos, sin):
    xf = x.astype(jnp.float32)
    xr, xp = xf[..., :ROPE_DIM], xf[..., ROPE_DIM:]
    half = ROPE_DIM // 2
    rot = jnp.concatenate([-xr[..., half:], xr[..., :half]], axis=-1)
    c = cos[None, :, None, None, :]
    s = sin[None, :, None, None, :]
    return jnp.concatenate([xr * c + rot * s, xp], axis=-1).astype(x.dtype)


def short_conv_centred(u, w, b):
    up = jnp.pad(u, ((0, 0), (1, 1), (0, 0)))
    return up[:, :-2] * w[0] + up[:, 1:-1] * w[1] + up[:, 2:] * w[2] + b


def hyena_filters(seq_len, fw1, fb1, freq1, fw2, fb2, freq2, fw3):
    f32 = jnp.float32
    t = jnp.linspace(0.0, 1.0, seq_len, dtype=f32)[:, None]
    wpos = 2.0 * math.pi * jnp.arange(seq_len, dtype=f32)[:, None] / seq_len
    bands = jnp.linspace(1e-4, HY_BANDS - 1, HY_BANDS, dtype=f32)[None, :]
    z = jnp.concatenate([t, jnp.cos(wpos * bands), -jnp.sin(wpos * bands)], axis=-1)
    hdn = jnp.sin(freq1.astype(f32) * (z @ fw1.astype(f32) + fb1.astype(f32)))
    hdn = jnp.sin(freq2.astype(f32) * (hdn @ fw2.astype(f32) + fb2.astype(f32)))
    filt = hdn @ fw3.astype(f32)
    max_decay = math.log(HY_DECAY_TARGET) / HY_FAST_DECAY
    min_decay = math.log(HY_DECAY_TARGET) / HY_SLOW_DECAY
    deltas = jnp.linspace(min_decay, max_decay, HY_WIDTH, dtype=f32)[None, :]
    window = jnp.exp(-t * jnp.abs(deltas)) + HY_SHIFT
    h_fwd = filt[:, :HY_WIDTH] * window
    h_bwd = filt[:, HY_WIDTH:] * window
    k = jnp.concatenate([h_fwd, jnp.zeros((1, HY_WIDTH), f32), h_bwd[:0:-1]], axis=0)
    return k * lax.rsqrt(jnp.sum(jnp.square(k), axis=0, keepdims=True) + 1e-6)


def hyena_mixer(u, conv_w, conv_b, fw1, fb1, freq1, fw2, fb2, freq2, fw3, bias_d):
    B, L, _ = u.shape
    uc = short_conv_centred(u, conv_w, conv_b)
    x0, x1, v = jnp.split(uc, 3, axis=-1)
    v = (v * x1).astype(jnp.float32)
    k = hyena_filters(L, fw1, fb1, freq1, fw2, fb2, freq2, fw3)
    vf = jnp.fft.rfft(v, n=2 * L, axis=1)
    kf = jnp.fft.rfft(k, axis=0)
    y = jnp.fft.irfft(vf * kf[None], n=2 * L, axis=1)[:, :L]
    y = y + v * bias_d.astype(jnp.float32)
    return (y * x0.astype(jnp.float32)).astype(u.dtype)


def diff_attention(q, k, v, lq1, lk1, lq2, lk2, subln_g, lam_init, cos, sin):
    B, S, _ = q.shape
    q = q.reshape(B, S, ATT_HEADS, 2, ATT_HEAD_DIM)
    k = k.reshape(B, S, ATT_HEADS, 2, ATT_HEAD_DIM)
    v = v.reshape(B, S, ATT_HEADS, 2 * ATT_HEAD_DIM)
    q = apply_partial_rope(q, cos, sin) * (ATT_HEAD_DIM ** -0.5)
    k = apply_partial_rope(k, cos, sin)
    f32 = jnp.float32
    lam = (jnp.exp(jnp.sum(lq1.astype(f32) * lk1.astype(f32)))
           - jnp.exp(jnp.sum(lq2.astype(f32) * lk2.astype(f32))) + lam_init)
    nb = S // Q_BLOCK
    qb = q.reshape(B, nb, Q_BLOCK, ATT_HEADS, 2, ATT_HEAD_DIM).transpose(1, 0, 2, 3, 4, 5)

    def one_block(qblk):
        s = jnp.einsum('bqhcd,bkhcd->bhcqk', qblk, k, preferred_element_type=f32)
        pr = jax.nn.softmax(s, axis=-1)
        a = pr[:, :, 0] - lam * pr[:, :, 1]
        return jnp.einsum('bhqk,bkhe->bqhe', a.astype(v.dtype), v)

    o = lax.map(one_block, qb)
    o = o.transpose(1, 0, 2, 3, 4).reshape(B, S, ATT_HEADS, 2 * ATT_HEAD_DIM)
    o = rms_norm(o, subln_g) * (1.0 - lam_init)
    return o.reshape(B, S, ATT_HEADS * 2 * ATT_HEAD_DIM)


def pool_mixer(u, w, b, scale):
    B, S, _ = u.shape
    G = len(POOL_WINDOWS)
    f32 = jnp.float32
    uf = u.astype(f32).reshape(B, S, G, POOL_GROUP)
    csum = jnp.concatenate([jnp.zeros((B, 1, G, POOL_GROUP), f32), jnp.cumsum(uf, axis=1)], axis=1)
    pos = jnp.arange(S)
    pooled = []
    for g, win in enumerate(POOL_WINDOWS):
        lo = jnp.clip(pos - win // 2, 0, S - 1)
        hi = jnp.clip(pos + win // 2 - 1, 0, S - 1)
        cg = csum[:, :, g]
        cnt = (hi - lo + 1).astype(f32)[None, :, None]
        pooled.append((cg[:, hi + 1] - cg[:, lo]) / cnt)
    y = jnp.stack(pooled, axis=2) - uf
    y = jnp.einsum('bsgc,gcd->bsgd', y, w.astype(f32)) + b.astype(f32)
    return (y.reshape(B, S, POOL_WIDTH) * scale.astype(f32)).astype(u.dtype)


def hier_moe(h, wgc, bgc, wgf, bgf, w1, w3, w2):
    B, S, D = h.shape
    f32 = jnp.float32
    t = h.reshape(B * S, D)
    T = t.shape[0]
    pg = jax.nn.softmax(jnp.dot(t, wgc, preferred_element_type=f32) + bgc.astype(f32), axis=-1)
    gw, gi = lax.top_k(pg, 1)
    fl = (jnp.dot(t, wgf, preferred_element_type=f32) + bgf.astype(f32)).reshape(T, N_GROUPS, EXPERTS_PER_GROUP)
    idx = jnp.broadcast_to(gi[:, :, None], (T, 1, EXPERTS_PER_GROUP))
    fs = jnp.take_along_axis(fl, idx, axis=1)[:, 0]
    tv, ti = lax.top_k(jax.nn.softmax(fs, axis=-1), TOP_K_IN_GROUP)
    tv = tv / jnp.sum(tv, axis=-1, keepdims=True)
    eid = gi * EXPERTS_PER_GROUP + ti
    wts = gw * tv
    gates = jnp.sum(jax.nn.one_hot(eid, N_EXPERTS, dtype=f32) * wts[..., None], axis=1)
    a = jnp.einsum('td,edf->tef', t, w1)
    c = jnp.einsum('td,edf->tef', t, w3)
    act = jax.nn.silu(a) * c * gates[:, :, None].astype(t.dtype)
    y = jnp.einsum('tef,efd->td', act, w2)
    return y.reshape(B, S, D)


def setup_inputs(seed: int = 0) -> dict:
    key = jax.random.key(seed)
    ks = iter(jax.random.split(key, 48))
    f32 = jnp.float32

    def nrm(shape, scale):
        return scale * jax.random.normal(next(ks), shape, f32)

    def gain(shape):
        return 1.0 + 0.05 * jax.random.normal(next(ks), shape, f32)

    L = DEPTH
    return {
        'x': nrm((BATCH, SEQ, D_MODEL), 1.0),
        'p': nrm((DEPTH, BATCH, SEQ, PLE_DIM), 1.0),
        'ln0_g': gain((D_MODEL,)),
        'ln0_b': nrm((D_MODEL,), 0.02),
        'w_in': nrm((L, D_MODEL, IN_WIDTH), D_MODEL ** -0.5),
        'hy_conv_w': nrm((L, 3, 3 * HY_WIDTH), 3 ** -0.5),
        'hy_conv_b': nrm((L, 3 * HY_WIDTH), 0.02),
        'hy_fw1': nrm((L, HY_EMB, HY_FILTER_HIDDEN), HY_EMB ** -0.5),
        'hy_fb1': nrm((L, HY_FILTER_HIDDEN), 0.02),
        'hy_freq1': gain((L, HY_FILTER_HIDDEN)),
        'hy_fw2': nrm((L, HY_FILTER_HIDDEN, HY_FILTER_HIDDEN), HY_FILTER_HIDDEN ** -0.5),
        'hy_fb2': nrm((L, HY_FILTER_HIDDEN), 0.02),
        'hy_freq2': gain((L, HY_FILTER_HIDDEN)),
        'hy_fw3': nrm((L, HY_FILTER_HIDDEN, 2 * HY_WIDTH), HY_FILTER_HIDDEN ** -0.5),
        'hy_bias': nrm((L, HY_WIDTH), 1.0),
        'att_lq1': nrm((L, ATT_HEAD_DIM), 0.1),
        'att_lk1': nrm((L, ATT_HEAD_DIM), 0.1),
        'att_lq2': nrm((L, ATT_HEAD_DIM), 0.1),
        'att_lk2': nrm((L, ATT_HEAD_DIM), 0.1),
        'att_subln_g': gain((L, 2 * ATT_HEAD_DIM)),
        'pool_w': nrm((L, len(POOL_WINDOWS), POOL_GROUP, POOL_GROUP), POOL_GROUP ** -0.5),
        'pool_b': nrm((L, len(POOL_WINDOWS), POOL_GROUP), 0.02),
        'pool_scale': gain((L, POOL_WIDTH)),
        'w_out': nrm((L, D_MODEL, D_MODEL), DN_BETA * D_MODEL ** -0.5),
        'ln1_g': gain((L, D_MODEL)),
        'ln1_b': nrm((L, D_MODEL), 0.02),
        'moe_wgc': nrm((L, D_MODEL, N_GROUPS), D_MODEL ** -0.5),
        'moe_bgc': nrm((L, N_GROUPS), 0.01),
        'moe_wgf': nrm((L, D_MODEL, N_EXPERTS), D_MODEL ** -0.5),
        'moe_bgf': nrm((L, N_EXPERTS), 0.01),
        'moe_w1': nrm((L, N_EXPERTS, D_MODEL, D_EXPERT), D_MODEL ** -0.5),
        'moe_w3': nrm((L, N_EXPERTS, D_MODEL, D_EXPERT), D_MODEL ** -0.5),
        'moe_w2': nrm((L, N_EXPERTS, D_EXPERT, D_MODEL), DN_BETA * D_EXPERT ** -0.5),
        'ple_wg': nrm((L, D_MODEL, D_MODEL), D_MODEL ** -0.5),
        'ple_bg': nrm((L, D_MODEL), 0.02),
        'ple_wp': nrm((L, PLE_DIM, D_MODEL), DN_BETA * PLE_DIM ** -0.5),
        'ln2_g': gain((L, D_MODEL)),
        'ln2_b': nrm((L, D_MODEL), 0.02),
    }


def reference(x, p, ln0_g, ln0_b, w_in, hy_conv_w, hy_conv_b, hy_fw1, hy_fb1, hy_freq1,
              hy_fw2, hy_fb2, hy_freq2, hy_fw3, hy_bias, att_lq1, att_lk1, att_lq2, att_lk2,
              att_subln_g, pool_w, pool_b, pool_scale, w_out, ln1_g, ln1_b, moe_wgc, moe_bgc,
              moe_wgf, moe_bgf, moe_w1, moe_w3, moe_w2, ple_wg, ple_bg, ple_wp, ln2_g, ln2_b):
    S = x.shape[1]
    h = layer_norm(x, ln0_g, ln0_b)
    cos, sin = rope_tables(S)
    o_q = 3 * HY_WIDTH
    o_k = o_q + ATT_WIDTH
    o_v = o_k + ATT_WIDTH
    o_p = o_v + ATT_WIDTH
    for i in range(DEPTH):
        lam_init = 0.8 - 0.6 * math.exp(-0.3 * i)
        u = h @ w_in[i]
        y_hy = hyena_mixer(u[..., :o_q], hy_conv_w[i], hy_conv_b[i], hy_fw1[i], hy_fb1[i],
                           hy_freq1[i], hy_fw2[i], hy_fb2[i], hy_freq2[i], hy_fw3[i], hy_bias[i])
        y_att = diff_attention(u[..., o_q:o_k], u[..., o_k:o_v], u[..., o_v:o_p],
                               att_lq1[i], att_lk1[i], att_lq2[i], att_lk2[i], att_subln_g[i],
                               lam_init, cos, sin)
        y_pool = pool_mixer(u[..., o_p:], pool_w[i], pool_b[i], pool_scale[i])
        mix = jnp.concatenate([y_hy, y_att, y_pool], axis=-1) @ w_out[i]
        h = layer_norm(DN_ALPHA * h + mix, ln1_g[i], ln1_b[i])
        y_moe = hier_moe(h, moe_wgc[i], moe_bgc[i], moe_wgf[i], moe_bgf[i],
                         moe_w1[i], moe_w3[i], moe_w2[i])
        y_ple = jax.nn.sigmoid(h @ ple_wg[i] + ple_bg[i]) * (p[i] @ ple_wp[i])
        h = layer_norm(DN_ALPHA * h + y_moe + y_ple, ln2_g[i], ln2_b[i])
    return h
```

```python
import math
import numpy as np
import ml_dtypes
from contextlib import ExitStack
import concourse.bass as bass
import concourse.mybir as mybir
from concourse.bass_utils import run_bass_kernel_spmd

F32 = mybir.dt.float32
BF16 = mybir.dt.bfloat16
AF = mybir.ActivationFunctionType
ALU = mybir.AluOpType
AX = mybir.AxisListType

ENGS = ("pe", "act", "dve", "pool", "sp")
NDS = 24
NDS_HW = 16

T = 4096
D = 1024
NT = 32
DEPTH = 4
ALPHA = (2 * DEPTH) ** 0.25
O_Q, O_K, O_V, O_P = 768, 1280, 1792, 2304
LN_EPS = 1e-5
NFC = 33
PI = math.pi


class _Rec:
    def __getattr__(self, name):
        def f(*a, **k):
            self.call = (name, a, k)
        return f


class Sched:
    def __init__(self, nc):
        self.nc = nc
        self.q = {e: [] for e in ENGS}
        self.last_w = {}
        self.readers = {}
        self.ndma = 0
        self.ndma_sw = 0
        self.dma_hist = [[] for _ in range(NDS)]
        self.pending = {}

    def _deps(self, eng, r, w):
        deps = set()
        for k in r:
            t = self.last_w.get(k)
            if t is not None:
                deps.add(t)
        for k in w:
            t = self.last_w.get(k)
            if t is not None:
                deps.add(t)
            for t in self.readers.get(k, ()):
                deps.add(t)
        p = self.pending.pop(eng, None)
        if p:
            deps |= p
        return deps

    def _commit(self, tok, r, w):
        for k in r:
            lst = self.readers.setdefault(k, [])
            if tok[0] == "c":
                for j in range(len(lst)):
                    if lst[j][0] == "c" and lst[j][1] == tok[1]:
                        lst[j] = tok
                        break
                else:
                    lst.append(tok)
            else:
                lst.append(tok)
        for k in w:
            self.last_w[k] = tok
            self.readers[k] = []

    def op(self, eng, fn, r=(), w=(), after=()):
        rec = _Rec()
        fn(rec)
        name_, a_, k_ = rec.call
        fn = lambda e, name_=name_, a_=a_, k_=k_: getattr(e, name_)(*a_, **k_)
        deps = self._deps(eng, r, w)
        deps.update(after)
        tok = ("c", eng, len(self.q[eng]))
        self.q[eng].append(dict(fn=fn, deps=deps, tok=tok, dma=None))
        self._commit(tok, r, w)
        return tok

    def dma(self, eng, out, in_, r=(), w=(), after=(), **kw):
        deps = self._deps(eng, r, w)
        deps.update(after)
        if eng == "pool":
            slot = NDS_HW + self.ndma_sw % (NDS - NDS_HW)
            self.ndma_sw += 1
        else:
            slot = self.ndma % NDS_HW
            self.ndma += 1
        hist = self.dma_hist[slot]
        if hist:
            deps.add(hist[-1])
        tok = ("d", slot, len(hist) + 1)
        hist.append(tok)
        fn = lambda e, out=out, in_=in_, kw=kw: e.dma_start(out=out, in_=in_, **kw)
        self.q[eng].append(dict(fn=fn, deps=deps, tok=tok, dma=slot))
        self._commit(tok, r, w)
        return tok

    def barrier(self):
        toks = set()
        for e in ENGS:
            for ins in reversed(self.q[e]):
                if ins["dma"] is None:
                    toks.add(ins["tok"])
                    break
        for h in self.dma_hist:
            if h:
                toks.add(h[-1])
        for e in ENGS:
            self.pending.setdefault(e, set()).update(toks)
        self.last_w = {}
        self.readers = {}

    def emit(self, final_wait_eng="sp", final_tokens=()):
        nc = self.nc
        needed = set()
        for e in ENGS:
            for ins in self.q[e]:
                for d in ins["deps"]:
                    if d[0] == "c" and not (e == "pe" and d[1] == "pe"):
                        needed.add(d)
        cnt = {}
        for e in ENGS:
            c = 0
            for ins in self.q[e]:
                if ins["tok"] in needed:
                    c += 1
                    cnt[ins["tok"]] = c
        with ExitStack() as es:
            csem = {e: es.enter_context(nc.semaphore("cs_" + e)) for e in ENGS}
            dsem = [es.enter_context(nc.semaphore("ds_%d" % i)) for i in range(NDS)]
            block = es.enter_context(nc.Block())

            def resolve(t):
                if t[0] == "c":
                    return ("c", t[1]), csem[t[1]], cnt[t]
                return ("d", t[1]), dsem[t[1]], 16 * t[2]

            def run(ename, eobj):
                waited = {}
                for ins in self.q[ename]:
                    need = {}
                    for d in ins["deps"]:
                        if d[0] == "c" and d[1] == ename and ename == "pe":
                            continue
                        key, sem, val = resolve(d)
                        if waited.get(key, 0) >= val:
                            continue
                        if need.get(key, (None, 0))[1] < val:
                            need[key] = (sem, val)
                    for key, (sem, val) in need.items():
                        eobj.wait_ge(sem, val)
                        waited[key] = val
                    i = ins["fn"](eobj)
                    if ins["dma"] is not None:
                        i.then_inc(dsem[ins["dma"]], 16)
                    elif ins["tok"] in needed:
                        i.then_inc(csem[ename], 1)
                if ename == final_wait_eng:
                    for t in final_tokens:
                        key, sem, val = resolve(t)
                        if waited.get(key, 0) < val:
                            eobj.wait_ge(sem, val)
                            waited[key] = val

            @block.tensor
            def _(e):
                run("pe", e)

            @block.scalar
            def _(e):
                run("act", e)

            @block.vector
            def _(e):
                run("dve", e)

            @block.gpsimd
            def _(e):
                run("pool", e)

            @block.sync
            def _(e):
                run("sp", e)


_CONST = {}


def host_consts():
    if _CONST:
        return _CONST
    c = {}
    c["ident"] = np.eye(128, dtype=np.float32)
    rot = np.zeros((128, 128), np.float32)
    for base in (0, 64):
        for d in range(8):
            rot[base + d + 8, base + d] = -1.0
            rot[base + d, base + d + 8] = 1.0
    c["rotm"] = rot
    pos = np.arange(T, dtype=np.float32)
    inv_freq = np.power(np.float32(500000.0), -np.arange(0, 16, 2, dtype=np.float32) / np.float32(16)).astype(np.float32)
    ang = (pos[:, None] * inv_freq[None, :]).astype(np.float32)
    ang = np.concatenate([ang, ang], axis=-1)
    ct = np.ones((128, T), np.float32)
    stb = np.zeros((128, T), np.float32)
    for base in (0, 64):
        ct[base:base + 16] = np.cos(ang).T
        stb[base:base + 16] = np.sin(ang).T
    c["cos_tab"] = ct
    c["sin_tab"] = stb
    idx = np.arange(NFC * 128, dtype=np.int64)
    prod = (idx[:, None] * idx[None, :]) % 8192
    valid = (idx[:, None] <= 4096) & (idx[None, :] <= 4096)
    angd = prod.astype(np.float64) * (2.0 * np.pi / 8192.0)
    tabs = []
    for fn in (np.cos, np.sin):
        m = np.where(valid, fn(angd), 0.0).astype(np.float32)
        m4 = m.reshape(NFC, 128, NFC, 128).transpose(2, 1, 0, 3)
        tabs.append(np.ascontiguousarray(m4).reshape(NFC, 128, NFC * 128))
    c["dft"] = np.stack(tabs, 0).astype(ml_dtypes.bfloat16)
    del prod, angd, valid
    t = np.linspace(0.0, 1.0, T, dtype=np.float32)[:, None]
    wpos = (np.float32(2.0 * math.pi) * np.arange(T, dtype=np.float32)[:, None] / np.float32(T)).astype(np.float32)
    bands = np.linspace(1e-4, 15, 16, dtype=np.float32)[None, :]
    z = np.concatenate([t, np.cos(wpos * bands), -np.sin(wpos * bands)], axis=-1).astype(np.float32)
    c["zT"] = np.ascontiguousarray(z.T)
    tt = np.linspace(0.0, 1.0, T, dtype=np.float32)
    c["negt"] = np.ascontiguousarray(-tt.reshape(NT, 128).T)
    max_decay = math.log(1e-2) / 0.3
    min_decay = math.log(1e-2) / 1.5
    c["absd"] = np.abs(np.linspace(min_decay, max_decay, 256, dtype=np.float32)).astype(np.float32)
    f = np.arange(NFC * 128)
    wf = np.where(f <= 4096, 2.0, 0.0)
    wf[0] = 1.0
    wf[4096] = 1.0
    c["wfT"] = np.ascontiguousarray((wf / 8192.0).astype(np.float32).reshape(NFC, 128).T)
    rc = np.zeros((2, 128, 16), np.float32)
    wins = (2, 4, 8, 16)
    for cp in range(2):
        for half in range(2):
            w = wins[2 * cp + half]
            for j in range(16):
                tpos = j if j < 8 else T - 16 + j
                lo = max(tpos - w // 2, 0)
                hi = min(tpos + w // 2 - 1, T - 1)
                rc[cp, half * 64:(half + 1) * 64, j] = 1.0 / float(hi - lo + 1)
    c["poolrc"] = rc
    _CONST.update(c)
    return _CONST


WEIGHT_SPECS = [
    ("ln0_g", [1024]), ("ln0_b", [1024]), ("w_in", [4, 1024, 2560]), ("hy_conv_w", [4, 3, 768]),
    ("hy_conv_b", [4, 768]), ("hy_fw1", [4, 33, 64]), ("hy_fb1", [4, 64]), ("hy_freq1", [4, 64]),
    ("hy_fw2", [4, 64, 64]), ("hy_fb2", [4, 64]), ("hy_freq2", [4, 64]), ("hy_fw3", [4, 64, 512]),
    ("hy_bias", [4, 256]), ("att_lq1", [4, 64]), ("att_lk1", [4, 64]), ("att_lq2", [4, 64]),
    ("att_lk2", [4, 64]), ("att_subln_g", [4, 128]), ("pool_w", [4, 4, 64, 64]), ("pool_b", [4, 4, 64]),
    ("pool_scale", [4, 256]), ("w_out", [4, 1024, 1024]), ("ln1_g", [4, 1024]), ("ln1_b", [4, 1024]),
    ("moe_wgc", [4, 1024, 4]), ("moe_bgc", [4, 4]), ("moe_wgf", [4, 1024, 16]), ("moe_bgf", [4, 16]),
    ("moe_w1", [4, 16, 1024, 256]), ("moe_w3", [4, 16, 1024, 256]), ("moe_w2", [4, 16, 256, 1024]),
    ("ple_wg", [4, 1024, 1024]), ("ple_bg", [4, 1024]), ("ple_wp", [4, 256, 1024]),
    ("ln2_g", [4, 1024]), ("ln2_b", [4, 1024]),
]
CONST_SPECS = [
    ("ident", [128, 128], F32), ("rotm", [128, 128], F32), ("cos_tab", [128, T], F32), ("sin_tab", [128, T], F32),
    ("dft", [2, NFC, 128, NFC * 128], BF16), ("zT", [33, T], F32), ("negt", [128, NT], F32), ("absd", [256], F32),
    ("wfT", [128, NFC], F32), ("poolrc", [2, 128, 16], F32),
]


class Arena:
    def __init__(self, ap, nbytes):
        self.ap = ap
        self.nbytes = nbytes
        self.top = 0
        self.marks = []

    def alloc(self, shape, dt):
        esz = 2 if dt == BF16 else 4
        n = 1
        for s in shape[1:]:
            n *= s
        nb = (n * esz + 31) // 32 * 32
        off = self.top
        self.top += nb
        assert self.top <= self.nbytes, ("arena overflow", self.top)
        a = self.ap[:, off // 4:(off + nb) // 4]
        if dt == BF16:
            a = a.bitcast(BF16)
        a = a[:, 0:n]
        names = " ".join("d%d" % i for i in range(len(shape) - 1))
        if len(shape) > 2:
            kw = {"d%d" % i: shape[i + 1] for i in range(len(shape) - 1)}
            a = a.rearrange("p (%s) -> p %s" % (names, names), **kw)
        if shape[0] < 128:
            a = a[0:shape[0]]
        return a

    def push(self):
        self.marks.append(self.top)

    def pop(self):
        self.top = self.marks.pop()


class Rot:
    def __init__(self, name, items):
        self.name = name
        self.items = items
        self.i = 0

    def next(self):
        j = self.i % len(self.items)
        self.i += 1
        return self.items[j], (self.name, j)


def col(ap1d):
    return ap1d.rearrange("(p o) -> p o", o=1)


def build(n_layers=DEPTH, stop_after=None, debug=False, skip=()):
    nc = bass.Bass("TRN2", target_bir_lowering=False)
    dr = {}
    dr["x"] = nc.dram_tensor("x", [T, D], F32, kind="ExternalInput").ap()
    NLD = n_layers if debug else DEPTH
    dr["p"] = nc.dram_tensor("p", [NLD, T, 256], F32, kind="ExternalInput").ap()
    for n, s in WEIGHT_SPECS:
        s = list(s)
        if s[0] == DEPTH and n not in ("ln0_g", "ln0_b"):
            s[0] = NLD
        dr[n] = nc.dram_tensor(n, s, F32, kind="ExternalInput").ap()
    for n, s, dt in CONST_SPECS:
        dr[n] = nc.dram_tensor(n, s, dt, kind="ExternalInput").ap()
    out = nc.dram_tensor("out", [T, D], F32, kind="ExternalOutput").ap()
    skind = dict(kind="ExternalOutput") if debug else {}
    H32 = nc.dram_tensor("H32", [T, D], F32, **skind).ap()
    MIXT = nc.dram_tensor("MIXT", [8, 128, T], BF16, **skind).ap()

    es = ExitStack()
    arena_t = es.enter_context(nc.sbuf_tensor("arena", [128, 204 * 256], F32))
    PS = [es.enter_context(nc.psum_tensor("ps%d" % i, [128, 512], F32)) for i in range(8)]
    S = Sched(nc)
    A = Arena(arena_t[:], 204 * 1024)
    final_tokens = []

    hT = A.alloc([128, 8, T], BF16)
    ident = A.alloc([128, 128], F32)
    ones_f = A.alloc([128, 128], F32)
    zeros_b = A.alloc([128, 512], BF16)
    gates = A.alloc([128, NT, 16], F32)
    S.dma("sp", ident, dr["ident"], w=["ident"])
    S.op("dve", lambda e: e.memset(ones_f, 1.0), w=["ones_f"])
    S.op("dve", lambda e: e.memset(zeros_b, 0.0), w=["zeros_b"])

    def hTk(ks, tiles):
        return [("hT", k, i) for k in ks for i in tiles]

    K8 = list(range(8))

    def load_w(dst, src, key, eng="pool"):
        return S.dma(eng, dst, src.rearrange("(k p) n -> p k n", p=128), w=[key])

    def make_ln_bufs():
        b = {}
        b["st"] = Rot("ln_st", [A.alloc([128, 2, 6], F32) for _ in range(2)])
        b["mv"] = Rot("ln_mv", [A.alloc([128, 2], F32) for _ in range(2)])
        b["rs"] = Rot("ln_rs", [A.alloc([128, 1], F32) for _ in range(2)])
        b["hn"] = Rot("ln_hn", [A.alloc([128, D], F32) for _ in range(2)])
        b["bank"] = Rot("ps", [(PS[6], 6), (PS[7], 7)])
        return b

    def ln_tile(bufs, rt, rt_keys, i, g_rep, b_rep, gb_key, dst, dst_key, gating=None):
        st, stk = bufs["st"].next()
        mv, mvk = bufs["mv"].next()
        rs, rsk = bufs["rs"].next()
        hn, hnk = bufs["hn"].next()
        for hh in range(2):
            S.op("dve", lambda e, hh=hh: e.bn_stats(st[:, hh, :], rt[:, hh * 512:(hh + 1) * 512]), r=list(rt_keys), w=[stk + (hh,)])
        S.op("dve", lambda e: e.bn_aggr(mv, st.rearrange("p a b -> p (a b)")), r=[stk + (0,), stk + (1,)], w=[mvk])
        S.op("dve", lambda e: e.tensor_scalar(rs, mv[:, 1:2], LN_EPS, None, ALU.add), r=[mvk], w=[rsk])
        S.op("act", lambda e: e.activation(rs, rs, AF.Sqrt), r=[rsk], w=[rsk])
        S.op("dve", lambda e: e.reciprocal(rs, rs), r=[rsk], w=[rsk])
        S.op("dve", lambda e: e.tensor_scalar(hn, rt, mv[:, 0:1], rs[:, 0:1], ALU.subtract, ALU.mult), r=list(rt_keys) + [mvk, rsk], w=[hnk])
        S.op("dve", lambda e: e.tensor_tensor(hn, hn, g_rep, ALU.mult), r=[hnk, gb_key], w=[hnk])
        S.op("dve", lambda e: e.tensor_tensor(hn, hn, b_rep, ALU.add), r=[hnk, gb_key], w=[hnk])
        tok = S.dma("sp", dst[i * 128:(i + 1) * 128, :], hn, r=[hnk], w=[(dst_key, i)])
        for half in range(2):
            (bank, bi), _ = bufs["bank"].next()
            for j in range(4):
                k = half * 4 + j
                S.op("pe", lambda e, bank=bank, j=j, k=k: e.transpose(bank[:, j * 128:(j + 1) * 128], hn[:, k * 128:(k + 1) * 128], ident),
                     r=[hnk, "ident"], w=[("ps", bi)])
            S.op("act", lambda e, bank=bank, half=half: e.copy(hT[:, half * 4:(half + 1) * 4, i * 128:(i + 1) * 128],
                                                                bank[:, 0:512].rearrange("p (a b) -> p a b", a=4)),
                 r=[("ps", bi)], w=hTk(range(half * 4, half * 4 + 4), [i]))
            if gating is not None:
                if half == 0:
                    gating["cur"] = gating["hTf"].next()
                hTf, hTfk = gating["cur"]
                S.op("act", lambda e, bank=bank, half=half, hTf=hTf: e.copy(hTf[:, half * 4:(half + 1) * 4, :],
                                                                                   bank[:, 0:512].rearrange("p (a b) -> p a b", a=4)),
                     r=[("ps", bi)], w=[hTfk + (half,)])
        if gating is not None:
            hTf, hTfk = gating["cur"]
            gb, gbi = gating["bank"]
            for k in range(8):
                S.op("pe", lambda e, k=k, hTf=hTf: e.matmul(gb[:, 0:20], hTf[:, k, :], gating["wg"][:, k, :], start=(k == 0), stop=(k == 7)),
                     r=[hTfk + (k // 4,), "wg_f"], w=[("ps", gbi)])
            S.op("dve", lambda e: e.tensor_tensor(gating["logits"][:, i, :], gb[:, 0:20], gating["bg"], ALU.add),
                 r=[("ps", gbi), "bg_rep"], w=[("logits", i)])
        return tok

    dbg_list = []

    def dbg_dump(name, ap, shape, dt, rkeys=()):
        if not debug:
            return
        t = nc.dram_tensor("dbg_" + name, shape, dt, kind="ExternalOutput").ap()
        S.dma("sp", t, ap, r=list(rkeys))

    def dbg_stop(layer, phase):
        return stop_after is not None and stop_after == (layer, phase)

    A.push()
    g_rep = A.alloc([128, D], F32)
    b_rep = A.alloc([128, D], F32)
    S.dma("sp", g_rep, dr["ln0_g"].partition_broadcast(128), w=["gb0"])
    S.dma("sp", b_rep, dr["ln0_b"].partition_broadcast(128), w=["gb0"])
    lnb = make_ln_bufs()
    xt = Rot("xt", [A.alloc([128, D], F32) for _ in range(2)])
    for i in range(NT):
        x_t, xk = xt.next()
        S.dma("act", x_t, dr["x"][i * 128:(i + 1) * 128, :], w=[xk])
        ln_tile(lnb, x_t, [xk], i, g_rep, b_rep, "gb0", H32, "H32")
    A.pop()
    S.barrier()

    done = False
    for l in range(n_layers):
        lam_init = 0.8 - 0.6 * math.exp(-0.3 * l)
        last = (l == DEPTH - 1)
        for _ph in ([0] if "A" not in skip else []):
            A.push()
            PA = A.top
            VK = A.alloc([128, NT, 768], BF16)
            A.alloc([128, 16], F32)
            A.push()
            zT = A.alloc([33, T], F32)
            S.dma("sp", zT, dr["zT"], w=["zT"])
            fw1 = A.alloc([33, 64], F32)
            fw2 = A.alloc([64, 64], F32)
            fw3 = A.alloc([64, 512], F32)
            S.dma("sp", fw1, dr["hy_fw1"][l], w=["fw1"])
            S.dma("sp", fw2, dr["hy_fw2"][l], w=["fw2"])
            S.dma("sp", fw3, dr["hy_fw3"][l], w=["fw3"])
            fcol = A.alloc([64, 8], F32)
            S.dma("sp", fcol[:, 0:1], col(dr["hy_fb1"][l]), w=["fcol"])
            S.dma("sp", fcol[:, 1:2], col(dr["hy_freq1"][l]), w=["fcol"])
            S.dma("sp", fcol[:, 2:3], col(dr["hy_fb2"][l]), w=["fcol"])
            S.dma("sp", fcol[:, 3:4], col(dr["hy_freq2"][l]), w=["fcol"])
            S.op("dve", lambda e: e.tensor_tensor(fcol[:, 4:5], fcol[:, 0:1], fcol[:, 1:2], ALU.mult), r=["fcol"], w=["fcol"])
            S.op("dve", lambda e: e.tensor_tensor(fcol[:, 5:6], fcol[:, 2:3], fcol[:, 3:4], ALU.mult), r=["fcol"], w=["fcol"])
            hdn1 = A.alloc([64, T], F32)
            hdn2 = A.alloc([64, T], F32)
            arg = Rot("arg", [A.alloc([64, 512], F32) for _ in range(2)])
            m1b = Rot("m1b", [A.alloc([64, 512], F32) for _ in range(2)])
            m2b = Rot("m2b", [A.alloc([64, 512], F32) for _ in range(2)])
            absd = A.alloc([128, 256], F32)
            S.dma("sp", absd, dr["absd"].partition_broadcast(128), w=["absd"])
            negt = A.alloc([128, NT], F32)
            S.dma("sp", negt, dr["negt"], w=["negt"])
            hb = A.alloc([1, 256], F32)
            S.dma("sp", hb, dr["hy_bias"][l].rearrange("(o c) -> o c", o=1), w=["hb"])

            def sin_layer(ps, pk, fq, bs, dst, dkey):
                a, ak = arg.next()
                m1, m1k = m1b.next()
                m2, m2k = m2b.next()
                S.op("dve", lambda e: e.tensor_scalar(a, ps, fcol[:, fq:fq + 1], fcol[:, bs:bs + 1], ALU.mult, ALU.add), r=[pk, "fcol"], w=[ak])
                S.op("dve", lambda e: e.tensor_scalar(m1, a, -PI, 2 * PI, ALU.is_lt, ALU.mult), r=[ak], w=[m1k])
                S.op("dve", lambda e: e.tensor_scalar(m2, a, PI, 2 * PI, ALU.is_gt, ALU.mult), r=[ak], w=[m2k])
                S.op("dve", lambda e: e.tensor_tensor(a, a, m1, ALU.add), r=[ak, m1k], w=[ak])
                S.op("dve", lambda e: e.tensor_tensor(a, a, m2, ALU.subtract), r=[ak, m2k], w=[ak])
                S.op("act", lambda e: e.activation(dst, a, AF.Sin), r=[ak], w=[dkey])

            for tc in range(8):
                sl = slice(tc * 512, (tc + 1) * 512)
                S.op("pe", lambda e, sl=sl: e.matmul(PS[0][0:64, :], fw1, zT[:, sl], start=True, stop=True), r=["fw1", "zT"], w=[("ps", 0)])
                sin_layer(PS[0][0:64, :], ("ps", 0), 1, 4, hdn1[:, sl], ("hdn1", tc))
                S.op("pe", lambda e, sl=sl: e.matmul(PS[1][0:64, :], fw2, hdn1[:, sl], start=True, stop=True), r=["fw2", ("hdn1", tc)], w=[("ps", 1)])
                sin_layer(PS[1][0:64, :], ("ps", 1), 3, 5, hdn2[:, sl], ("hdn2", tc))

            win = Rot("win", [A.alloc([128, 256], F32) for _ in range(2)])
            kw = Rot("kw", [A.alloc([128, 512], F32) for _ in range(2)])
            sq = Rot("sq", [A.alloc([128, 512], F32) for _ in range(2)])
            nrm = A.alloc([128, 256], F32)
            kbank = Rot("ps", [(PS[2], 2), (PS[3], 3)])

            def filt_tile(i):
                (pb, pbi), _ = kbank.next()
                S.op("pe", lambda e: e.matmul(pb[:, :], hdn2[:, i * 128:(i + 1) * 128], fw3, start=True, stop=True),
                     r=[("hdn2", i // 4), "fw3"], w=[("ps", pbi)])
                wn, wnk = win.next()
                S.op("act", lambda e: e.activation(wn, absd, AF.Exp, scale=negt[:, i:i + 1]), r=["absd", "negt"], w=[wnk])
                k_, kk = kw.next()
                for hh in range(2):
                    S.op("dve", lambda e, hh=hh: e.tensor_tensor(k_[:, hh * 256:(hh + 1) * 256], pb[:, hh * 256:(hh + 1) * 256], wn, ALU.mult),
                         r=[("ps", pbi), wnk], w=[kk])
                if i == 0:
                    S.op("dve", lambda e: e.memset(k_[0:1, 256:512], 0.0), w=[kk])
                return k_, kk

            for i in range(NT):
                k_, kk = filt_tile(i)
                s_, sk = sq.next()
                S.op("act", lambda e, k_=k_, s_=s_: e.activation(s_, k_, AF.Square), r=[kk], w=[sk])
                S.op("pe", lambda e, s_=s_, i=i: e.matmul(PS[4][:, :], ones_f, s_, start=(i == 0), stop=(i == NT - 1)), r=["ones_f", sk], w=[("ps", 4)])
            S.op("dve", lambda e: e.tensor_copy(nrm, PS[4][:, 0:256]), r=[("ps", 4)], w=["nrm"])
            S.op("dve", lambda e: e.tensor_tensor(nrm, nrm, PS[4][:, 256:512], ALU.add), r=[("ps", 4), "nrm"], w=["nrm"])
            S.op("dve", lambda e: e.tensor_scalar(nrm, nrm, 1e-6, None, ALU.add), r=["nrm"], w=["nrm"])
            S.op("act", lambda e: e.activation(nrm, nrm, AF.Sqrt), r=["nrm"], w=["nrm"])
            S.op("dve", lambda e: e.reciprocal(nrm, nrm), r=["nrm"], w=["nrm"])
            for i in range(NT):
                k_, kk = filt_tile(i)
                s_, sk = sq.next()
                S.op("dve", lambda e, k_=k_, s_=s_: e.tensor_tensor(s_[:, 0:256], k_[:, 0:256], k_[:, 256:512], ALU.add), r=[kk], w=[sk])
                S.op("dve", lambda e, k_=k_, s_=s_: e.tensor_tensor(s_[:, 256:512], k_[:, 256:512], k_[:, 0:256], ALU.subtract), r=[kk], w=[sk])
                for hh in range(2):
                    S.op("dve", lambda e, s_=s_, hh=hh: e.tensor_tensor(s_[:, hh * 256:(hh + 1) * 256], s_[:, hh * 256:(hh + 1) * 256], nrm, ALU.mult),
                         r=[sk, "nrm"], w=[sk])
                if i == 0:
                    S.op("dve", lambda e, s_=s_: e.tensor_tensor(s_[0:1, 0:256], s_[0:1, 0:256], hb, ALU.add), r=[sk, "hb"], w=[sk])
                    S.op("dve", lambda e, s_=s_: e.tensor_tensor(s_[0:1, 256:512], s_[0:1, 256:512], hb, ALU.subtract), r=[sk, "hb"], w=[sk])
                S.op("act", lambda e, s_=s_, i=i: e.copy(VK[:, i, 0:256], s_[:, 0:256]), r=[sk], w=[("VKk", i)])
                S.op("act", lambda e, s_=s_, i=i: e.copy(VK[:, i, 512:768], s_[:, 256:512]), r=[sk], w=[("VKk", i)])
            A.pop()
            S.barrier()
            A.push()
            ub = A.alloc([128, T + 2], F32)
            c1 = A.alloc([128, T], F32)
            vp = A.alloc([128, T], F32)
            wch = Rot("wch", [A.alloc([128, 8, 128], BF16) for _ in range(2)])
            cw = Rot("cw", [A.alloc([128, 4], F32) for _ in range(2)])
            S.op("dve", lambda e: e.memset(ub[:, 0:1], 0.0), w=["ub_h"])
            S.op("dve", lambda e: e.memset(ub[:, T + 1:T + 2], 0.0), w=["ub_h"])
            pbank = Rot("ps", [(PS[0], 0), (PS[1], 1)])
            tbank = Rot("ps", [(PS[2], 2), (PS[3], 3)])
            for cc in range(2):
                for sname, coff in (("x1", 256), ("v", 512)):
                    c0 = coff + cc * 128
                    w_, wk = wch.next()
                    load_w(w_, dr["w_in"][l][:, c0:c0 + 128], wk)
                    cw_, cwk = cw.next()
                    for j in range(3):
                        S.dma("sp", cw_[:, j:j + 1], col(dr["hy_conv_w"][l, j, c0:c0 + 128]), w=[cwk])
                    S.dma("sp", cw_[:, 3:4], col(dr["hy_conv_b"][l, c0:c0 + 128]), w=[cwk])
                    for tc in range(8):
                        (pb, pbi), _ = pbank.next()
                        for k in range(8):
                            S.op("pe", lambda e, pb=pb, k=k, tc=tc, w_=w_: e.matmul(pb[:, :], w_[:, k, :], hT[:, k, tc * 512:(tc + 1) * 512], start=(k == 0), stop=(k == 7)),
                                 r=[wk] + hTk([k], range(tc * 4, tc * 4 + 4)), w=[("ps", pbi)])
                        S.op("act", lambda e, pb=pb, tc=tc: e.copy(ub[:, 1 + tc * 512:1 + (tc + 1) * 512], pb[:, :]), r=[("ps", pbi)], w=[("ub", tc)])
                    ubk = [("ub", tc) for tc in range(8)] + ["ub_h"]
                    dst = {"x1": c1, "v": vp}[sname]
                    dk = {"x1": "c1", "v": "vp"}[sname]
                    S.op("dve", lambda e, dst=dst, cw_=cw_: e.tensor_scalar(dst, ub[:, 0:T], cw_[:, 0:1], cw_[:, 3:4], ALU.mult, ALU.add), r=ubk + [cwk], w=[dk])
                    S.op("dve", lambda e, dst=dst, cw_=cw_: e.scalar_tensor_tensor(dst, ub[:, 1:T + 1], cw_[:, 1:2], dst, ALU.mult, ALU.add), r=ubk + [cwk, dk], w=[dk])
                    S.op("dve", lambda e, dst=dst, cw_=cw_: e.scalar_tensor_tensor(dst, ub[:, 2:T + 2], cw_[:, 2:3], dst, ALU.mult, ALU.add), r=ubk + [cwk, dk], w=[dk])
                    if sname == "v":
                        S.op("dve", lambda e: e.tensor_tensor(vp, vp, c1, ALU.mult), r=["vp", "c1"], w=["vp"])
                        for i4 in range(8):
                            (tb, tbi), _ = tbank.next()
                            for j in range(4):
                                i = i4 * 4 + j
                                S.op("pe", lambda e, tb=tb, j=j, i=i: e.transpose(tb[:, j * 128:(j + 1) * 128], vp[:, i * 128:(i + 1) * 128], ident),
                                     r=["vp", "ident"], w=[("ps", tbi)])
                            S.op("act", lambda e, tb=tb, i4=i4, cc=cc: e.copy(VK[:, i4 * 4:(i4 + 1) * 4, 256 + cc * 128:256 + (cc + 1) * 128],
                                                                             tb[:, 0:512].rearrange("p (a b) -> p a b", a=4)),
                                 r=[("ps", tbi)], w=[("VKv", cc, i4)])
            S.barrier()
            if l == 0:
                pass
                pass
                pass
                dbg_dump("cw", cw_, [128, 4], F32)
                pass
                S.barrier()
            A.pop()
            if dbg_stop(l, "A2"):
                break
            A.push()
            Zc = A.alloc([128, NFC, 256], BF16)
            Zs = A.alloc([128, NFC, 256], BF16)
            tabC = Rot("tabC", [A.alloc([128, NFC * 128], BF16) for _ in range(2)])
            tabS = Rot("tabS", [A.alloc([128, NFC * 128], BF16) for _ in range(2)])
            wfT = A.alloc([128, NFC], F32)
            S.dma("sp", wfT, dr["wfT"], w=["wfT"])
            T3 = A.top
            csb = Rot("csb", [A.alloc([128, 512], F32) for _ in range(2)])
            ssb = Rot("ssb", [A.alloc([128, 512], F32) for _ in range(2)])
            tm = Rot("tm", [A.alloc([128, 4, 256], F32) for _ in range(2)])
            cbank = Rot("ps", [(PS[0], 0), (PS[1], 1)])
            sbank = Rot("ps", [(PS[2], 2), (PS[3], 3)])
            for j in range(NFC):
                tc_, tck = tabC.next()
                ts_, tsk = tabS.next()
                S.dma("sp", tc_[:, 0:NT * 128], dr["dft"][0, j][:, 0:NT * 128], w=[tck])
                S.dma("act", ts_[:, 0:NT * 128], dr["dft"][1, j][:, 0:NT * 128], w=[tsk])
                (pc, pci), _ = cbank.next()
                (psn, psi), _ = sbank.next()
                for i in range(NT):
                    S.op("pe", lambda e, pc=pc, tc_=tc_, i=i: e.matmul(pc[:, :], tc_[:, i * 128:(i + 1) * 128], VK[:, i, 0:512], start=(i == 0), stop=(i == NT - 1)),
                         r=[tck], w=[("ps", pci)])
                for i in range(NT):
                    S.op("pe", lambda e, psn=psn, ts_=ts_, i=i: e.matmul(psn[:, :], ts_[:, i * 128:(i + 1) * 128], VK[:, i, 256:768], start=(i == 0), stop=(i == NT - 1)),
                         r=[tsk], w=[("ps", psi)])
                c_, ck = csb.next()
                s_, sk = ssb.next()
                t_, tk = tm.next()
                S.op("dve", lambda e, c_=c_, pc=pc, j=j: e.tensor_scalar(c_[:, 0:256], pc[:, 0:256], wfT[:, j:j + 1], None, ALU.mult), r=[("ps", pci), "wfT"], w=[ck])
                S.op("act", lambda e, c_=c_, pc=pc: e.copy(c_[:, 256:512], pc[:, 256:512]), r=[("ps", pci)], w=[ck])
                S.op("act", lambda e, s_=s_, psn=psn: e.copy(s_[:, 0:256], psn[:, 0:256]), r=[("ps", psi)], w=[sk])
                S.op("dve", lambda e, s_=s_, psn=psn, j=j: e.tensor_scalar(s_[:, 256:512], psn[:, 256:512], wfT[:, j:j + 1], None, ALU.mult), r=[("ps", psi), "wfT"], w=[sk])
                S.op("dve", lambda e, t_=t_, c_=c_: e.tensor_tensor(t_[:, 0, :], c_[:, 256:512], c_[:, 0:256], ALU.mult), r=[ck], w=[tk + (0,)])
                S.op("dve", lambda e, t_=t_, s_=s_: e.tensor_tensor(t_[:, 1, :], s_[:, 0:256], s_[:, 256:512], ALU.mult), r=[sk], w=[tk + (1,)])
                S.op("dve", lambda e, t_=t_, j=j: e.tensor_tensor(Zc[:, j, :], t_[:, 0, :], t_[:, 1, :], ALU.add), r=[tk + (0,), tk + (1,)], w=[("Z", j)])
                S.op("dve", lambda e, t_=t_, s_=s_, c_=c_: e.tensor_tensor(t_[:, 2, :], s_[:, 0:256], c_[:, 0:256], ALU.mult), r=[sk, ck], w=[tk + (2,)])
                S.op("dve", lambda e, t_=t_, s_=s_, c_=c_: e.tensor_tensor(t_[:, 3, :], c_[:, 256:512], s_[:, 256:512], ALU.mult), r=[sk, ck], w=[tk + (3,)])
                S.op("dve", lambda e, t_=t_, j=j: e.tensor_tensor(Zs[:, j, :], t_[:, 2, :], t_[:, 3, :], ALU.subtract), r=[tk + (2,), tk + (3,)], w=[("Z", j)])
            S.barrier()
            if l == 0:
                pass
                pass
                S.barrier()
            A.top = T3
            mst = A.alloc([128, 2, T], BF16)
            ysb = Rot("ysb", [A.alloc([128, 256], F32) for _ in range(2)])
            w_ = A.alloc([128, 8, 128], BF16)
            cw_ = A.alloc([128, 4], F32)
            TOPR = A.top
            A.top = PA
            ub = A.alloc([128, T + 2], F32)
            c1 = A.alloc([128, T], F32)
            x0c = A.alloc([128, 2, T], BF16)
            assert A.top <= T3 and A.top <= PA + 49152 + 64
            A.top = TOPR
            S.op("dve", lambda e: e.memset(ub[:, 0:1], 0.0), w=["ub_h"])
            S.op("dve", lambda e: e.memset(ub[:, T + 1:T + 2], 0.0), w=["ub_h"])
            pbank = Rot("ps", [(PS[0], 0), (PS[1], 1)])
            for cc in range(2):
                c0 = cc * 128
                load_w(w_, dr["w_in"][l][:, c0:c0 + 128], "wchx")
                for j in range(3):
                    S.dma("sp", cw_[:, j:j + 1], col(dr["hy_conv_w"][l, j, c0:c0 + 128]), w=["cwx"])
                S.dma("sp", cw_[:, 3:4], col(dr["hy_conv_b"][l, c0:c0 + 128]), w=["cwx"])
                for tc in range(8):
                    (pb, pbi), _ = pbank.next()
                    for k in range(8):
                        S.op("pe", lambda e, pb=pb, k=k, tc=tc: e.matmul(pb[:, :], w_[:, k, :], hT[:, k, tc * 512:(tc + 1) * 512], start=(k == 0), stop=(k == 7)),
                             r=["wchx"] + hTk([k], range(tc * 4, tc * 4 + 4)), w=[("ps", pbi)])
                    S.op("act", lambda e, pb=pb, tc=tc: e.copy(ub[:, 1 + tc * 512:1 + (tc + 1) * 512], pb[:, :]), r=[("ps", pbi)], w=[("ub", tc)])
                ubk = [("ub", tc) for tc in range(8)] + ["ub_h"]
                S.op("dve", lambda e: e.tensor_scalar(c1, ub[:, 0:T], cw_[:, 0:1], cw_[:, 3:4], ALU.mult, ALU.add), r=ubk + ["cwx"], w=["c1"])
                S.op("dve", lambda e: e.scalar_tensor_tensor(c1, ub[:, 1:T + 1], cw_[:, 1:2], c1, ALU.mult, ALU.add), r=ubk + ["cwx", "c1"], w=["c1"])
                S.op("dve", lambda e, cc=cc: e.scalar_tensor_tensor(x0c[:, cc, :], ub[:, 2:T + 2], cw_[:, 2:3], c1, ALU.mult, ALU.add), r=ubk + ["cwx", "c1"], w=[("x0c", cc)])
            ybank = Rot("ps", [(PS[4], 4), (PS[5], 5)])
            t2bank = Rot("ps", [(PS[6], 6), (PS[7], 7)])
            Zkeys = [("Z", j) for j in range(NFC)]
            for i in range(NT):
                tc_, tck = tabC.next()
                ts_, tsk = tabS.next()
                S.dma("sp", tc_, dr["dft"][0, i], w=[tck])
                S.dma("act", ts_, dr["dft"][1, i], w=[tsk])
                (py, pyi), _ = ybank.next()
                for jj in range(NFC):
                    S.op("pe", lambda e, py=py, tc_=tc_, jj=jj: e.matmul(py[:, 0:256], tc_[:, jj * 128:(jj + 1) * 128], Zc[:, jj, :], start=(jj == 0), stop=False),
                         r=[tck] + (Zkeys if jj == 0 else []), w=[("ps", pyi)])
                    S.op("pe", lambda e, py=py, ts_=ts_, jj=jj: e.matmul(py[:, 0:256], ts_[:, jj * 128:(jj + 1) * 128], Zs[:, jj, :], start=False, stop=(jj == NFC - 1)),
                         r=[tsk], w=[("ps", pyi)])
                y_, yk = ysb.next()
                S.op("act", lambda e, y_=y_, py=py: e.copy(y_, py[:, 0:256]), r=[("ps", pyi)], w=[yk])
                (tb, tbi), _ = t2bank.next()
                for cc in range(2):
                    S.op("pe", lambda e, tb=tb, y_=y_, cc=cc: e.transpose(tb[:, cc * 128:(cc + 1) * 128], y_[:, cc * 128:(cc + 1) * 128], ident),
                         r=[yk, "ident"], w=[("ps", tbi)])
                S.op("dve", lambda e, tb=tb, i=i: e.tensor_tensor(mst[:, :, i * 128:(i + 1) * 128], tb[:, 0:256].rearrange("p (a b) -> p a b", a=2),
                                                                  x0c[:, :, i * 128:(i + 1) * 128], ALU.mult),
                     r=[("ps", tbi), ("x0c", 0), ("x0c", 1)], w=[("mst", i)])
            for cc in range(2):
                S.dma("sp", MIXT[cc], mst[:, cc, :], r=[("mst", i) for i in range(NT)], w=[("MIXT", cc)])
            A.pop()
            A.pop()
            S.barrier()
        if dbg_stop(l, "A"):
            break

        for _ph in ([0] if "B" not in skip else []):
            A.push()
            cos_t = A.alloc([128, T], F32)
            sin_t = A.alloc([128, T], F32)
            S.dma("sp", cos_t, dr["cos_tab"], w=["cos_t"])
            S.dma("act", sin_t, dr["sin_tab"], w=["sin_t"])
            rotm = A.alloc([128, 128], F32)
            S.dma("sp", rotm, dr["rotm"], w=["rotm"])
            Va = A.alloc([128, NT, 4, 130], BF16)
            qT = A.alloc([128, T], BF16)
            kT = A.alloc([128, T], BF16)
            wv = A.alloc([128, 8, 512], BF16)
            wqk = Rot("wqk", [A.alloc([128, 8, 128], BF16) for _ in range(2)])
            tmpf = Rot("tmpf", [A.alloc([128, 512], F32) for _ in range(2)])
            r1t = Rot("r1t", [A.alloc([128, 512], F32) for _ in range(2)])
            r2t = Rot("r2t", [A.alloc([128, 512], F32) for _ in range(2)])
            Pb = Rot("Pb", [A.alloc([128, 512], BF16) for _ in range(4)])
            mst = A.alloc([128, T], BF16)
            osb = Rot("osb", [A.alloc([128, 128], F32) for _ in range(2)])
            t2b = Rot("t2b", [A.alloc([128, 128], F32) for _ in range(2)])
            junk = A.alloc([128, 128], F32)
            sm = Rot("sm", [A.alloc([128, 8], F32) for _ in range(2)])
            gsub = A.alloc([128, 128], F32)
            lqk = A.alloc([128, 4, 64], F32)
            lam_t = A.alloc([128, 8], F32)
            for j, nm in enumerate(("att_lq1", "att_lk1", "att_lq2", "att_lk2")):
                S.dma("sp", lqk[:, j, :], dr[nm][l].partition_broadcast(128), w=["lqk"])
            S.op("dve", lambda e: e.tensor_tensor(lqk[:, 0, :], lqk[:, 0, :], lqk[:, 1, :], ALU.mult), r=["lqk"], w=["lqk"])
            S.op("dve", lambda e: e.tensor_tensor(lqk[:, 2, :], lqk[:, 2, :], lqk[:, 3, :], ALU.mult), r=["lqk"], w=["lqk"])
            S.op("dve", lambda e: e.tensor_reduce(lam_t[:, 0:1], lqk[:, 0, :], AX.X, ALU.add), r=["lqk"], w=["lam"])
            S.op("dve", lambda e: e.tensor_reduce(lam_t[:, 1:2], lqk[:, 2, :], AX.X, ALU.add), r=["lqk"], w=["lam"])
            S.op("act", lambda e: e.activation(lam_t[:, 2:4], lam_t[:, 0:2], AF.Exp), r=["lam"], w=["lam"])
            S.op("dve", lambda e: e.tensor_tensor(lam_t[:, 4:5], lam_t[:, 3:4], lam_t[:, 2:3], ALU.subtract), r=["lam"], w=["lam"])
            S.op("dve", lambda e: e.tensor_scalar(lam_t[:, 5:6], lam_t[:, 4:5], -lam_init, None, ALU.add), r=["lam"], w=["lam"])
            neglam = lam_t[:, 5:6]
            S.dma("sp", gsub, dr["att_subln_g"][l].partition_broadcast(128), w=["gsub"])
            S.op("dve", lambda e: e.tensor_scalar(gsub, gsub, 1.0 - lam_init, None, ALU.mult), r=["gsub"], w=["gsub"])
            S.op("dve", lambda e: e.memset(Va[:, :, :, 128:130], 1.0), w=["Va1"])
            S.dma("pool", wv, dr["w_in"][l][:, O_V:O_V + 512].rearrange("(k p) n -> p k n", p=128), w=["wv"])
            vbank = Rot("ps", [(PS[6], 6), (PS[7], 7)])
            for i in range(NT):
                (pb, pbi), _ = vbank.next()
                for k in range(8):
                    S.op("pe", lambda e, pb=pb, k=k, i=i: e.matmul(pb[:, :], hT[:, k, i * 128:(i + 1) * 128], wv[:, k, :], start=(k == 0), stop=(k == 7)),
                         r=["wv"] + hTk([k], [i]), w=[("ps", pbi)])
                S.op("act", lambda e, pb=pb, i=i: e.copy(Va[:, i, :, 0:128], pb[:, :].rearrange("p (a b) -> p a b", a=4)), r=[("ps", pbi)], w=[("Va", i)])
            Vkeys = [("Va", i) for i in range(NT)] + ["Va1"]
            accb = [(PS[2], 2), (PS[3], 3), (PS[4], 4), (PS[5], 5)]
            sbank = Rot("ps", [(PS[0], 0), (PS[1], 1)])
            for hd in range(4):
                for which, dstT, dkey, coff in (("q", qT, "qT", O_Q), ("k", kT, "kT", O_K)):
                    w_, wk = wqk.next()
                    load_w(w_, dr["w_in"][l][:, coff + hd * 128:coff + (hd + 1) * 128], wk)
                    for tc in range(8):
                        sl = slice(tc * 512, (tc + 1) * 512)
                        (pb, pbi), _ = vbank.next()
                        for k in range(8):
                            S.op("pe", lambda e, pb=pb, k=k, sl=sl, w_=w_: e.matmul(pb[:, :], w_[:, k, :], hT[:, k, sl], start=(k == 0), stop=(k == 7)),
                                 r=[wk] + hTk([k], range(tc * 4, tc * 4 + 4)), w=[("ps", pbi)])
                        tf, tfk = tmpf.next()
                        S.op("act", lambda e, tf=tf, pb=pb: e.copy(tf, pb[:, :]), r=[("ps", pbi)], w=[tfk])
                        S.op("act", lambda e, pb=pb, dstT=dstT, sl=sl: e.copy(dstT[:, sl], pb[:, :]), r=[("ps", pbi)], w=[(dkey, tc)])
                        (pr, pri), _ = vbank.next()
                        S.op("pe", lambda e, pr=pr, tf=tf: e.matmul(pr[:, :], rotm, tf, start=True, stop=True), r=["rotm", tfk], w=[("ps", pri)])
                        a1, a1k = r1t.next()
                        a2, a2k = r2t.next()
                        for base in (0, 64):
                            ps_ = slice(base, base + 16)
                            S.op("dve", lambda e, a1=a1, tf=tf, ps_=ps_, sl=sl: e.tensor_tensor(a1[ps_, :], tf[ps_, :], cos_t[ps_, sl], ALU.mult), r=[tfk, "cos_t"], w=[a1k])
                            S.op("dve", lambda e, a2=a2, pr=pr, ps_=ps_, sl=sl: e.tensor_tensor(a2[ps_, :], pr[ps_, :], sin_t[ps_, sl], ALU.mult), r=[("ps", pri), "sin_t"], w=[a2k])
                            S.op("dve", lambda e, a1=a1, a2=a2, ps_=ps_, sl=sl, dstT=dstT: e.tensor_tensor(dstT[ps_, sl], a1[ps_, :], a2[ps_, :], ALU.add), r=[a1k, a2k], w=[(dkey, tc)])
                kkeys = [("kT", tc) for tc in range(8)]
                for qc in range(8):
                    qsl = slice(qc * 512, (qc + 1) * 512)
                    for (ab, abi) in accb:
                        S.op("pe", lambda e, ab=ab: e.matmul(ab[:, :], zeros_b[:, 0:128], zeros_b[:, :], start=True, stop=False, skip_group_check=True),
                             r=["zeros_b"], w=[("ps", abi)])
                    for kc in range(NT):
                        for mp in range(2):
                            (sb_, sbi), _ = sbank.next()
                            rows = slice(mp * 64, (mp + 1) * 64)
                            S.op("pe", lambda e, sb_=sb_, rows=rows, kc=kc, qsl=qsl: e.matmul(sb_[:, :], kT[rows, kc * 128:(kc + 1) * 128], qT[rows, qsl], start=True, stop=True),
                                 r=[("kT", kc // 4), ("qT", qc)], w=[("ps", sbi)])
                            P_, Pk = Pb.next()
                            S.op("act", lambda e, P_=P_, sb_=sb_: e.activation(P_, sb_[:, :], AF.Exp, scale=0.125), r=[("ps", sbi)], w=[Pk])
                            for sub in range(4):
                                ab, abi = accb[mp * 2 + sub // 2]
                                co = (sub % 2) * 256
                                S.op("pe", lambda e, ab=ab, co=co, P_=P_, sub=sub, kc=kc, hd=hd: e.matmul(ab[:, co:co + 130], P_[:, sub * 128:(sub + 1) * 128], Va[:, kc, hd, 0:130],
                                                                                                            start=False, stop=(kc == NT - 1), skip_group_check=True),
                                     r=[Pk, ("Va", kc), "Va1"], w=[("ps", abi)])
                    for sub in range(4):
                        i = qc * 4 + sub
                        co = (sub % 2) * 256
                        a1b, a1i = accb[sub // 2]
                        a2b, a2i = accb[2 + sub // 2]
                        s_, sk = sm.next()
                        o_, ok = osb.next()
                        t2, t2k = t2b.next()
                        S.op("dve", lambda e, s_=s_, a1b=a1b, co=co: e.reciprocal(s_[:, 0:1], a1b[:, co + 128:co + 129]), r=[("ps", a1i)], w=[sk])
                        S.op("dve", lambda e, s_=s_, a2b=a2b, co=co: e.reciprocal(s_[:, 1:2], a2b[:, co + 128:co + 129]), r=[("ps", a2i)], w=[sk])
                        S.op("dve", lambda e, s_=s_: e.tensor_tensor(s_[:, 2:3], s_[:, 1:2], neglam, ALU.mult), r=[sk, "lam"], w=[sk])
                        S.op("dve", lambda e, t2=t2, a2b=a2b, co=co, s_=s_: e.tensor_scalar(t2, a2b[:, co:co + 128], s_[:, 2:3], None, ALU.mult), r=[("ps", a2i), sk], w=[t2k])
                        S.op("dve", lambda e, o_=o_, a1b=a1b, co=co, s_=s_, t2=t2: e.scalar_tensor_tensor(o_, a1b[:, co:co + 128], s_[:, 0:1], t2, ALU.mult, ALU.add),
                             r=[("ps", a1i), sk, t2k], w=[ok])
                        S.op("act", lambda e, o_=o_, s_=s_: e.activation(junk, o_, AF.Square, accum_out=s_[:, 3:4]), r=[ok], w=[sk, "junk"])
                        S.op("dve", lambda e, s_=s_: e.tensor_scalar(s_[:, 4:5], s_[:, 3:4], 1.0 / 128.0, 1e-5, ALU.mult, ALU.add), r=[sk], w=[sk])
                        S.op("act", lambda e, s_=s_: e.activation(s_[:, 5:6], s_[:, 4:5], AF.Sqrt), r=[sk], w=[sk])
                        S.op("dve", lambda e, s_=s_: e.reciprocal(s_[:, 6:7], s_[:, 5:6]), r=[sk], w=[sk])
                        S.op("dve", lambda e, o_=o_, s_=s_: e.scalar_tensor_tensor(o_, o_, s_[:, 6:7], gsub, ALU.mult, ALU.mult), r=[ok, sk, "gsub"], w=[ok])
                        (tb, tbi), _ = vbank.next()
                        S.op("pe", lambda e, tb=tb, o_=o_: e.transpose(tb[:, 0:128], o_, ident), r=[ok, "ident"], w=[("ps", tbi)])
                        S.op("act", lambda e, tb=tb, i=i: e.copy(mst[:, i * 128:(i + 1) * 128], tb[:, 0:128]), r=[("ps", tbi)], w=[("mstB", i)])
                S.dma("sp", MIXT[2 + hd], mst, r=[("mstB", i) for i in range(NT)], w=[("MIXT", 2 + hd)])
            A.pop()
            S.barrier()
        if dbg_stop(l, "B"):
            break

        for _ph in ([0] if "C" not in skip else []):
            A.push()
            LE = T + 32
            ub = A.alloc([128, LE], F32)
            wa = A.alloc([128, LE], F32)
            wb = A.alloc([128, LE], F32)
            yb = A.alloc([128, T], F32)
            bd = A.alloc([128, 128], F32)
            pcol = A.alloc([128, 2], F32)
            prc = A.alloc([128, 16], F32)
            etmp = A.alloc([128, 16], F32)
            mst = A.alloc([128, T], BF16)
            wch = Rot("wch", [A.alloc([128, 8, 128], BF16) for _ in range(2)])
            pbank = Rot("ps", [(PS[0], 0), (PS[1], 1)])
            obank = Rot("ps", [(PS[2], 2), (PS[3], 3)])
            wins = (2, 4, 8, 16)
            for cp in range(2):
                c0 = O_P + cp * 128
                w_, wk = wch.next()
                load_w(w_, dr["w_in"][l][:, c0:c0 + 128], wk)
                S.op("dve", lambda e: e.memset(ub, 0.0), w=["pub"])
                S.op("dve", lambda e: e.memset(wa, 0.0), w=["pwa"])
                S.op("dve", lambda e: e.memset(wb, 0.0), w=["pwb"])
                S.op("dve", lambda e: e.memset(bd, 0.0), w=["bd"])
                S.dma("sp", bd[0:64, 0:64], dr["pool_w"][l, 2 * cp], w=["bd"])
                S.dma("sp", bd[64:128, 64:128], dr["pool_w"][l, 2 * cp + 1], w=["bd"])
                S.dma("sp", pcol[:, 0:1], col(dr["pool_b"][l].rearrange("g c -> (g c)")[cp * 128:(cp + 1) * 128]), w=["pcol"])
                S.dma("sp", pcol[:, 1:2], col(dr["pool_scale"][l, cp * 128:(cp + 1) * 128]), w=["pcol"])
                S.dma("sp", prc, dr["poolrc"][cp], w=["prc"])
                for tc in range(8):
                    (pb, pbi), _ = pbank.next()
                    for k in range(8):
                        S.op("pe", lambda e, pb=pb, k=k, tc=tc, w_=w_: e.matmul(pb[:, :], w_[:, k, :], hT[:, k, tc * 512:(tc + 1) * 512], start=(k == 0), stop=(k == 7)),
                             r=[wk] + hTk([k], range(tc * 4, tc * 4 + 4)), w=[("ps", pbi)])
                    S.op("act", lambda e, pb=pb, tc=tc: e.copy(ub[:, 16 + tc * 512:16 + (tc + 1) * 512], pb[:, :]), r=[("ps", pbi)], w=["pub"])
                R0, R1 = 8, LE - 8
                n = R1 - R0
                S.op("dve", lambda e: e.tensor_tensor(wa[:, R0:R1], ub[:, R0 - 1:R1 - 1], ub[:, R0:R1], ALU.add), r=["pub"], w=["pwa"])
                S.op("dve", lambda e: e.tensor_tensor(wb[:, R0:R1], wa[:, R0 - 1:R1 - 1], wa[:, R0 + 1:R1 + 1], ALU.add), r=["pwa"], w=["pwb"])
                if cp == 0:
                    srcs = (wa, "pwa"), (wb, "pwb")
                else:
                    S.op("dve", lambda e: e.tensor_tensor(wa[:, R0:R1], wb[:, R0 - 2:R1 - 2], wb[:, R0 + 2:R1 + 2], ALU.add), r=["pwb"], w=["pwa"])
                    S.op("dve", lambda e: e.tensor_tensor(wb[:, R0:R1], wa[:, R0 - 4:R1 - 4], wa[:, R0 + 4:R1 + 4], ALU.add), r=["pwa"], w=["pwb"])
                    srcs = (wa, "pwa"), (wb, "pwb")
                for half in range(2):
                    rows = slice(half * 64, (half + 1) * 64)
                    src, srck = srcs[half]
                    wsz = wins[2 * cp + half]
                    S.op("dve", lambda e, rows=rows, src=src, wsz=wsz: e.scalar_tensor_tensor(yb[rows, :], src[rows, 16:16 + T], 1.0 / wsz, ub[rows, 16:16 + T], ALU.mult, ALU.subtract),
                         r=[srck, "pub"], w=["yb"])
                    for (e0, t0) in ((0, 0), (8, T - 8)):
                        S.op("dve", lambda e, rows=rows, src=src, e0=e0, t0=t0: e.tensor_tensor(etmp[rows, e0:e0 + 8], src[rows, 16 + t0:16 + t0 + 8], prc[rows, e0:e0 + 8], ALU.mult),
                             r=[srck, "prc"], w=["etmp"])
                        S.op("dve", lambda e, rows=rows, e0=e0, t0=t0: e.tensor_tensor(yb[rows, t0:t0 + 8], etmp[rows, e0:e0 + 8], ub[rows, 16 + t0:16 + t0 + 8], ALU.subtract),
                             r=["etmp", "pub", "yb"], w=["yb"])
                for tc in range(8):
                    (ob, obi), _ = obank.next()
                    S.op("pe", lambda e, ob=ob, tc=tc: e.matmul(ob[:, :], bd, yb[:, tc * 512:(tc + 1) * 512], start=True, stop=True), r=["bd", "yb"], w=[("ps", obi)])
                    S.op("dve", lambda e, ob=ob, tc=tc: e.tensor_scalar(mst[:, tc * 512:(tc + 1) * 512], ob[:, :], pcol[:, 0:1], pcol[:, 1:2], ALU.add, ALU.mult),
                         r=[("ps", obi), "pcol"], w=["mstC"])
                S.dma("sp", MIXT[6 + cp], mst, r=["mstC"], w=[("MIXT", 6 + cp)])
            A.pop()
            S.barrier()
        if dbg_stop(l, "C"):
            break

        A.push()
        wo = A.alloc([128, 8, D], BF16)
        for hh in range(2):
            S.dma("pool", wo[:, :, hh * 512:(hh + 1) * 512], dr["w_out"][l][:, hh * 512:(hh + 1) * 512].rearrange("(k p) n -> p k n", p=128), w=["wo"])
        g_rep = A.alloc([128, D], F32)
        b_rep = A.alloc([128, D], F32)
        S.dma("sp", g_rep, dr["ln1_g"][l].partition_broadcast(128), w=["gb1"])
        S.dma("sp", b_rep, dr["ln1_b"][l].partition_broadcast(128), w=["gb1"])
        wg_f = A.alloc([128, 8, 20], F32)
        S.dma("sp", wg_f[:, :, 0:4], dr["moe_wgc"][l].rearrange("(k p) n -> p k n", p=128), w=["wg_f"])
        S.dma("sp", wg_f[:, :, 4:20], dr["moe_wgf"][l].rearrange("(k p) n -> p k n", p=128), w=["wg_f"])
        bg_rep = A.alloc([128, 20], F32)
        S.dma("sp", bg_rep[:, 0:4], dr["moe_bgc"][l].partition_broadcast(128), w=["bg_rep"])
        S.dma("sp", bg_rep[:, 4:20], dr["moe_bgf"][l].partition_broadcast(128), w=["bg_rep"])
        logits = A.alloc([128, NT, 20], F32)
        lnb = make_ln_bufs()
        gating = dict(hTf=Rot("hTf", [A.alloc([128, 8, 128], F32) for _ in range(2)]), bank=(PS[4], 4), wg=wg_f, bg=bg_rep, logits=logits, cur=None)
        import os as _os
        DBG_D = _os.environ.get("DBG_D", "").split(",")
        if "nogate" in DBG_D:
            gating = None
        mt = Rot("mt", [A.alloc([128, 8, 128], BF16) for _ in range(2)])
        ht = Rot("ht", [A.alloc([128, D], F32) for _ in range(2)])
        rt = Rot("rt", [A.alloc([128, D], F32) for _ in range(2)])
        obank = Rot("ps", [(PS[0], 0), (PS[1], 1), (PS[2], 2), (PS[3], 3)])
        MIXr = MIXT.rearrange("c p t -> p c t")
        for i in range(NT):
            m_, mk = mt.next()
            h_, hk = ht.next()
            r_, rk = rt.next()
            if "nomixdma" not in DBG_D:
                S.dma("sp", m_, MIXr[:, :, i * 128:(i + 1) * 128], r=[("MIXT", c) for c in range(8)], w=[mk])
            S.dma("act", h_, H32[i * 128:(i + 1) * 128, :], r=[("H32", i)], w=[hk])
            for half in range(2):
                (ob, obi), _ = obank.next()
                if "nomm" in DBG_D:
                    S.op("dve", lambda e, h_=h_, r_=r_, half=half: e.tensor_scalar(r_[:, half * 512:(half + 1) * 512], h_[:, half * 512:(half + 1) * 512], ALPHA, None, ALU.mult), r=[hk], w=[rk])
                    continue
                for k in range(8):
                    S.op("pe", lambda e, ob=ob, m_=m_, k=k, half=half: e.matmul(ob[:, :], m_[:, k, :], wo[:, k, half * 512:(half + 1) * 512], start=(k == 0), stop=(k == 7)),
                         r=[mk, "wo"], w=[("ps", obi)])
                if half == 0:
                    S.op("act", lambda e, h_=h_: e.mul(h_, h_, ALPHA), r=[hk], w=[hk])
                S.op("dve", lambda e, ob=ob, h_=h_, r_=r_, half=half: e.tensor_tensor(r_[:, half * 512:(half + 1) * 512], ob[:, :], h_[:, half * 512:(half + 1) * 512], ALU.add),
                     r=[("ps", obi), hk], w=[rk])
            ln_tile(lnb, r_, [rk], i, g_rep, b_rep, "gb1", H32, "H32", gating=gating)
        if "nogate" in DBG_D or "noroute" in DBG_D:
            A.pop()
            S.barrier()
            break
        rA = A.alloc([128, NT, 16], F32)
        rB = A.alloc([128, NT, 16], F32)
        rC = A.alloc([128, NT, 16], F32)
        r1 = A.alloc([128, NT, 8], F32)
        lkeys = [("logits", i) for i in range(NT)]
        lc = logits[:, :, 0:4]
        lf = logits[:, :, 4:20]
        BIG = 1.0e30
        mc = r1[:, :, 0]
        S.op("dve", lambda e: e.tensor_reduce(mc, lc, AX.X, ALU.max), r=lkeys, w=["r1"])
        ec = rA[:, :, 0:4]
        S.op("dve", lambda e: e.tensor_tensor(ec, lc, mc.unsqueeze(2).broadcast_to([128, NT, 4]), ALU.subtract), r=lkeys + ["r1"], w=["rA"])
        S.op("act", lambda e: e.activation(ec, ec, AF.Exp), r=["rA"], w=["rA"])
        S.op("dve", lambda e: e.tensor_reduce(r1[:, :, 1], ec, AX.X, ALU.add), r=["rA"], w=["r1"])
        S.op("dve", lambda e: e.reciprocal(r1[:, :, 2], r1[:, :, 1]), r=["r1"], w=["r1"])
        oh = rA[:, :, 4:8]
        S.op("dve", lambda e: e.tensor_tensor(oh, lc, mc.unsqueeze(2).broadcast_to([128, NT, 4]), ALU.is_equal), r=lkeys + ["r1", "rA"], w=["rA"])
        pen = rA[:, :, 8:12]
        S.op("dve", lambda e: e.tensor_scalar(pen, oh, 1.0, BIG, ALU.subtract, ALU.mult), r=["rA"], w=["rA"])
        lfm = rB
        S.op("dve", lambda e: e.tensor_tensor(lfm.rearrange("p t (g j) -> p t g j", g=4), lf.rearrange("p t (g j) -> p t g j", g=4),
                                              pen.unsqueeze(3).broadcast_to([128, NT, 4, 4]), ALU.add), r=lkeys + ["rA"], w=["rB"])
        S.op("dve", lambda e: e.tensor_reduce(r1[:, :, 3], lfm, AX.X, ALU.max), r=["rB"], w=["r1"])
        eq1 = rC
        S.op("dve", lambda e: e.tensor_tensor(eq1, lfm, r1[:, :, 3].unsqueeze(2).broadcast_to([128, NT, 16]), ALU.is_equal), r=["rB", "r1"], w=["rC"])
        S.op("dve", lambda e: e.scalar_tensor_tensor(lfm, eq1, -BIG, lfm, ALU.mult, ALU.add), r=["rB", "rC"], w=["rB"])
        S.op("dve", lambda e: e.tensor_reduce(r1[:, :, 4], lfm, AX.X, ALU.max), r=["rB"], w=["r1"])
        eq2 = rA
        S.op("dve", lambda e: e.tensor_tensor(eq2, lfm, r1[:, :, 4].unsqueeze(2).broadcast_to([128, NT, 16]), ALU.is_equal), r=["rB", "r1", "rA"], w=["rA"])
        S.op("dve", lambda e: e.tensor_tensor(r1[:, :, 5], r1[:, :, 4], r1[:, :, 3], ALU.subtract), r=["r1"], w=["r1"])
        S.op("act", lambda e: e.activation(r1[:, :, 5], r1[:, :, 5], AF.Exp), r=["r1"], w=["r1"])
        S.op("dve", lambda e: e.tensor_scalar(r1[:, :, 6], r1[:, :, 5], 1.0, None, ALU.add), r=["r1"], w=["r1"])
        S.op("dve", lambda e: e.reciprocal(r1[:, :, 6], r1[:, :, 6]), r=["r1"], w=["r1"])
        S.op("dve", lambda e: e.tensor_tensor(r1[:, :, 7], r1[:, :, 5], r1[:, :, 6], ALU.mult), r=["r1"], w=["r1"])
        S.op("dve", lambda e: e.tensor_tensor(r1[:, :, 6], r1[:, :, 6], r1[:, :, 2], ALU.mult), r=["r1"], w=["r1"])
        S.op("dve", lambda e: e.tensor_tensor(r1[:, :, 7], r1[:, :, 7], r1[:, :, 2], ALU.mult), r=["r1"], w=["r1"])
        S.op("dve", lambda e: e.tensor_tensor(eq1, eq1, r1[:, :, 6].unsqueeze(2).broadcast_to([128, NT, 16]), ALU.mult), r=["rC", "r1"], w=["rC"])
        S.op("dve", lambda e: e.tensor_tensor(eq2, eq2, r1[:, :, 7].unsqueeze(2).broadcast_to([128, NT, 16]), ALU.mult), r=["rA", "r1"], w=["rA"])
        S.op("dve", lambda e: e.tensor_tensor(gates, eq1, eq2, ALU.add), r=["rC", "rA"], w=["gates"])
        A.pop()
        S.barrier()
        if dbg_stop(l, "D"):
            break

        A.push()
        wgp = A.alloc([128, 8, D], BF16)
        wpp = A.alloc([128, 2, D], BF16)
        for hh in range(2):
            S.dma("pool", wgp[:, :, hh * 512:(hh + 1) * 512], dr["ple_wg"][l][:, hh * 512:(hh + 1) * 512].rearrange("(k p) n -> p k n", p=128), w=["wgp"])
        S.dma("pool", wpp, dr["ple_wp"][l].rearrange("(k p) n -> p k n", p=128), w=["wpp"])
        bgp = A.alloc([128, D], F32)
        S.dma("sp", bgp, dr["ple_bg"][l].partition_broadcast(128), w=["bgp"])
        g_rep = A.alloc([128, D], F32)
        b_rep = A.alloc([128, D], F32)
        S.dma("sp", g_rep, dr["ln2_g"][l].partition_broadcast(128), w=["gb2"])
        S.dma("sp", b_rep, dr["ln2_b"][l].partition_broadcast(128), w=["gb2"])
        lnb = make_ln_bufs()
        yacc = A.alloc([128, 8, D], F32)
        w13 = Rot("w13", [A.alloc([128, 2, 8, 256], BF16) for _ in range(2)])
        w2r = Rot("w2r", [A.alloc([128, 2, D], BF16) for _ in range(2)])
        actT = Rot("actT", [A.alloc([128, 2, 1024], BF16) for _ in range(2)])
        sa = Rot("sa", [A.alloc([128, 512], F32) for _ in range(3)])
        pt = Rot("pt", [A.alloc([128, 256], F32) for _ in range(2)])
        pT = Rot("pT", [A.alloc([128, 2, 128], BF16) for _ in range(2)])
        ht = Rot("htE", [A.alloc([128, D], F32) for _ in range(2)])
        sg = Rot("sg", [A.alloc([128, 512], F32) for _ in range(2)])
        b01 = Rot("ps", [(PS[0], 0), (PS[1], 1)])
        b23 = Rot("ps", [(PS[2], 2), (PS[3], 3)])
        b45 = Rot("ps", [(PS[4], 4), (PS[5], 5)])
        dstD = out if last else H32
        for tcb in range(4):
            tiles = range(tcb * 8, tcb * 8 + 8)
            for ii, i in enumerate(tiles):
                p_, pk = pt.next()
                S.dma("sp", p_, dr["p"][l, i * 128:(i + 1) * 128, :], w=[pk])
                h_, hk = ht.next()
                S.dma("act", h_, H32[i * 128:(i + 1) * 128, :], r=[("H32", i)], w=[hk])
                (tb, tbi), _ = b45.next()
                for kk in range(2):
                    S.op("pe", lambda e, tb=tb, p_=p_, kk=kk: e.transpose(tb[:, kk * 128:(kk + 1) * 128], p_[:, kk * 128:(kk + 1) * 128], ident), r=[pk, "ident"], w=[("ps", tbi)])
                pT_, pTk = pT.next()
                S.op("act", lambda e, pT_=pT_, tb=tb: e.copy(pT_, tb[:, 0:256].rearrange("p (a b) -> p a b", a=2)), r=[("ps", tbi)], w=[pTk])
                for half in range(2):
                    hs = slice(half * 512, (half + 1) * 512)
                    (pg, pgi), _ = b01.next()
                    (pp, ppi), _ = b23.next()
                    for k in range(8):
                        S.op("pe", lambda e, pg=pg, k=k, i=i, hs=hs: e.matmul(pg[:, :], hT[:, k, i * 128:(i + 1) * 128], wgp[:, k, hs], start=(k == 0), stop=(k == 7)),
                             r=["wgp"] + hTk([k], [i]), w=[("ps", pgi)])
                    for kk in range(2):
                        S.op("pe", lambda e, pp=pp, kk=kk, pT_=pT_, hs=hs: e.matmul(pp[:, :], pT_[:, kk, :], wpp[:, kk, hs], start=(kk == 0), stop=(kk == 1)),
                             r=["wpp", pTk], w=[("ps", ppi)])
                    s_, sk = sg.next()
                    S.op("dve", lambda e, s_=s_, pg=pg, hs=hs: e.tensor_tensor(s_, pg[:, :], bgp[:, hs], ALU.add), r=[("ps", pgi), "bgp"], w=[sk])
                    S.op("act", lambda e, s_=s_: e.activation(s_, s_, AF.Sigmoid), r=[sk], w=[sk])
                    S.op("dve", lambda e, s_=s_, pp=pp: e.tensor_tensor(s_, s_, pp[:, :], ALU.mult), r=[sk, ("ps", ppi)], w=[sk])
                    S.op("dve", lambda e, s_=s_, h_=h_, ii=ii, hs=hs: e.scalar_tensor_tensor(yacc[:, ii, hs], h_[:, hs], ALPHA, s_, ALU.mult, ALU.add),
                         r=[sk, hk], w=[("yacc", ii, half)])
            for ex in range(16):
                w13_, w13k = w13.next()
                w2_, w2k = w2r.next()
                S.dma("pool", w13_[:, 0], dr["moe_w1"][l, ex].rearrange("(k p) n -> p k n", p=128), w=[w13k + (0,)])
                S.dma("pool", w13_[:, 1], dr["moe_w3"][l, ex].rearrange("(k p) n -> p k n", p=128), w=[w13k + (1,)])
                S.dma("pool", w2_, dr["moe_w2"][l, ex].rearrange("(k p) n -> p k n", p=128), w=[w2k])
                aT, aTk = actT.next()
                for sub in range(2):
                    tsl = slice(tcb * 1024 + sub * 512, tcb * 1024 + (sub + 1) * 512)
                    tl = range(tcb * 8 + sub * 4, tcb * 8 + sub * 4 + 4)
                    for fc in range(2):
                        (pa, pai), _ = b01.next()
                        (pc, pci), _ = b23.next()
                        for k in range(8):
                            S.op("pe", lambda e, pa=pa, k=k, fc=fc, tsl=tsl, w13_=w13_: e.matmul(pa[:, :], w13_[:, 0, k, fc * 128:(fc + 1) * 128], hT[:, k, tsl], start=(k == 0), stop=(k == 7)),
                                 r=[w13k + (0,)] + hTk([k], tl), w=[("ps", pai)])
                        for k in range(8):
                            S.op("pe", lambda e, pc=pc, k=k, fc=fc, tsl=tsl, w13_=w13_: e.matmul(pc[:, :], w13_[:, 1, k, fc * 128:(fc + 1) * 128], hT[:, k, tsl], start=(k == 0), stop=(k == 7)),
                                 r=[w13k + (1,)] + hTk([k], tl), w=[("ps", pci)])
                        s_, sk = sa.next()
                        S.op("act", lambda e, s_=s_, pa=pa: e.activation(s_, pa[:, :], AF.Silu), r=[("ps", pai)], w=[sk])
                        S.op("dve", lambda e, s_=s_, pc=pc, aT=aT, fc=fc, sub=sub: e.tensor_tensor(aT[:, fc, sub * 512:(sub + 1) * 512], s_, pc[:, :], ALU.mult),
                             r=[sk, ("ps", pci)], w=[aTk + (sub,)])
                for ii, i in enumerate(tiles):
                    for half in range(2):
                        hs = slice(half * 512, (half + 1) * 512)
                        (py, pyi), _ = b45.next()
                        for fc in range(2):
                            S.op("pe", lambda e, py=py, aT=aT, fc=fc, ii=ii, hs=hs, w2_=w2_: e.matmul(py[:, :], aT[:, fc, ii * 128:(ii + 1) * 128], w2_[:, fc, hs], start=(fc == 0), stop=(fc == 1)),
                                 r=[aTk + (ii // 4,), w2k], w=[("ps", pyi)])
                        S.op("dve", lambda e, py=py, ii=ii, hs=hs, i=i, ex=ex: e.scalar_tensor_tensor(yacc[:, ii, hs], py[:, :], gates[:, i, ex:ex + 1], yacc[:, ii, hs], ALU.mult, ALU.add),
                             r=[("ps", pyi), ("yacc", ii, half)], w=[("yacc", ii, half)])
            for ii, i in enumerate(tiles):
                tok = ln_tile(lnb, yacc[:, ii, :], [("yacc", ii, 0), ("yacc", ii, 1)], i, g_rep, b_rep, "gb2", dstD, "OUT" if last else "H32")
                if last:
                    final_tokens.append(tok)
        A.pop()
        S.barrier()
        if dbg_stop(l, "E"):
            break

    for h in S.dma_hist:
        if h:
            final_tokens.append(h[-1])
    S.emit(final_tokens=final_tokens)
    es.close()
    return nc


_NC_CACHE = {}


def make_in_maps(inputs, nld=DEPTH):
    c = host_consts()
    x = np.ascontiguousarray(np.asarray(inputs["x"], dtype=np.float32))
    p = np.asarray(inputs["p"], dtype=np.float32)[:nld]
    shared = {}
    for n, sh in WEIGHT_SPECS:
        a = np.asarray(inputs[n], dtype=np.float32)
        if sh[0] == DEPTH and n not in ("ln0_g", "ln0_b"):
            a = a[:nld]
        shared[n] = np.ascontiguousarray(a)
    for n, _, _ in CONST_SPECS:
        shared[n] = c[n]
    in_maps = []
    for core in range(8):
        b = core % 4
        m = dict(shared)
        m["x"] = x[b]
        m["p"] = np.ascontiguousarray(p[:, b])
        in_maps.append(m)
    return in_maps


def kernel(**inputs):
    if "nc" not in _NC_CACHE:
        _NC_CACHE["nc"] = build()
    nc = _NC_CACHE["nc"]
    in_maps = make_in_maps(inputs)
    res = run_bass_kernel_spmd(nc, in_maps, core_ids=list(range(8)))
    outs = [np.asarray(res.results[b]["out"], dtype=np.float32) for b in range(4)]
    return np.stack(outs, 0)
```

```python
import math
import numpy as np
import ml_dtypes
from contextlib import ExitStack
import concourse.bass as bass
import concourse.mybir as mybir
from concourse.bass_utils import run_bass_kernel_spmd

F32 = mybir.dt.float32
BF16 = mybir.dt.bfloat16
AF = mybir.ActivationFunctionType
ALU = mybir.AluOpType
AX = mybir.AxisListType

ENGS = ("pe", "act", "dve", "pool", "sp")
NDS = 24
NDS_HW = 16

T = 4096
D = 1024
NT = 32
DEPTH = 4
ALPHA = (2 * DEPTH) ** 0.25
O_Q, O_K, O_V, O_P = 768, 1280, 1792, 2304
LN_EPS = 1e-5
NFC = 33
PI = math.pi


class _Rec:
    def __getattr__(self, name):
        def f(*a, **k):
            self.call = (name, a, k)
        return f


class Sched:
    def __init__(self, nc):
        self.nc = nc
        self.q = {e: [] for e in ENGS}
        self.last_w = {}
        self.readers = {}
        self.ndma = 0
        self.ndma_sw = 0
        self.dma_hist = [[] for _ in range(NDS)]
        self.pending = {}

    def _deps(self, eng, r, w):
        deps = set()
        for k in r:
            t = self.last_w.get(k)
            if t is not None:
                deps.add(t)
        for k in w:
            t = self.last_w.get(k)
            if t is not None:
                deps.add(t)
            for t in self.readers.get(k, ()):
                deps.add(t)
        p = self.pending.pop(eng, None)
        if p:
            deps |= p
        return deps

    def _commit(self, tok, r, w):
        for k in r:
            lst = self.readers.setdefault(k, [])
            if tok[0] == "c":
                for j in range(len(lst)):
                    if lst[j][0] == "c" and lst[j][1] == tok[1]:
                        lst[j] = tok
                        break
                else:
                    lst.append(tok)
            else:
                lst.append(tok)
        for k in w:
            self.last_w[k] = tok
            self.readers[k] = []

    def op(self, eng, fn, r=(), w=(), after=()):
        rec = _Rec()
        fn(rec)
        name_, a_, k_ = rec.call
        fn = lambda e, name_=name_, a_=a_, k_=k_: getattr(e, name_)(*a_, **k_)
        deps = self._deps(eng, r, w)
        deps.update(after)
        tok = ("c", eng, len(self.q[eng]))
        self.q[eng].append(dict(fn=fn, deps=deps, tok=tok, dma=None))
        self._commit(tok, r, w)
        return tok

    def dma(self, eng, out, in_, r=(), w=(), after=(), **kw):
        deps = self._deps(eng, r, w)
        deps.update(after)
        if eng == "pool":
            slot = NDS_HW + self.ndma_sw % (NDS - NDS_HW)
            self.ndma_sw += 1
        else:
            slot = self.ndma % NDS_HW
            self.ndma += 1
        hist = self.dma_hist[slot]
        if hist:
            deps.add(hist[-1])
        tok = ("d", slot, len(hist) + 1)
        hist.append(tok)
        fn = lambda e, out=out, in_=in_, kw=kw: e.dma_start(out=out, in_=in_, **kw)
        self.q[eng].append(dict(fn=fn, deps=deps, tok=tok, dma=slot))
        self._commit(tok, r, w)
        return tok

    def barrier(self):
        toks = set()
        for e in ENGS:
            for ins in reversed(self.q[e]):
                if ins["dma"] is None:
                    toks.add(ins["tok"])
                    break
        for h in self.dma_hist:
            if h:
                toks.add(h[-1])
        for e in ENGS:
            self.pending.setdefault(e, set()).update(toks)
        self.last_w = {}
        self.readers = {}

    def emit(self, final_wait_eng="sp", final_tokens=()):
        nc = self.nc
        needed = set()
        for e in ENGS:
            for ins in self.q[e]:
                for d in ins["deps"]:
                    if d[0] == "c" and not (e == "pe" and d[1] == "pe"):
                        needed.add(d)
        cnt = {}
        for e in ENGS:
            c = 0
            for ins in self.q[e]:
                if ins["tok"] in needed:
                    c += 1
                    cnt[ins["tok"]] = c
        with ExitStack() as es:
            csem = {e: es.enter_context(nc.semaphore("cs_" + e)) for e in ENGS}
            dsem = [es.enter_context(nc.semaphore("ds_%d" % i)) for i in range(NDS)]
            block = es.enter_context(nc.Block())

            def resolve(t):
                if t[0] == "c":
                    return ("c", t[1]), csem[t[1]], cnt[t]
                return ("d", t[1]), dsem[t[1]], 16 * t[2]

            def run(ename, eobj):
                waited = {}
                for ins in self.q[ename]:
                    need = {}
                    for d in ins["deps"]:
                        if d[0] == "c" and d[1] == ename and ename == "pe":
                            continue
                        key, sem, val = resolve(d)
                        if waited.get(key, 0) >= val:
                            continue
                        if need.get(key, (None, 0))[1] < val:
                            need[key] = (sem, val)
                    for key, (sem, val) in need.items():
                        eobj.wait_ge(sem, val)
                        waited[key] = val
                    i = ins["fn"](eobj)
                    if ins["dma"] is not None:
                        i.then_inc(dsem[ins["dma"]], 16)
                    elif ins["tok"] in needed:
                        i.then_inc(csem[ename], 1)
                if ename == final_wait_eng:
                    for t in final_tokens:
                        key, sem, val = resolve(t)
                        if waited.get(key, 0) < val:
                            eobj.wait_ge(sem, val)
                            waited[key] = val

            @block.tensor
            def _(e):
                run("pe", e)

            @block.scalar
            def _(e):
                run("act", e)

            @block.vector
            def _(e):
                run("dve", e)

            @block.gpsimd
            def _(e):
                run("pool", e)

            @block.sync
            def _(e):
                run("sp", e)


_CONST = {}


def host_consts():
    if _CONST:
        return _CONST
    c = {}
    c["ident"] = np.eye(128, dtype=np.float32)
    rot = np.zeros((128, 128), np.float32)
    for base in (0, 64):
        for d in range(8):
            rot[base + d + 8, base + d] = -1.0
            rot[base + d, base + d + 8] = 1.0
    c["rotm"] = rot
    pos = np.arange(T, dtype=np.float32)
    inv_freq = np.power(np.float32(500000.0), -np.arange(0, 16, 2, dtype=np.float32) / np.float32(16)).astype(np.float32)
    ang = (pos[:, None] * inv_freq[None, :]).astype(np.float32)
    ang = np.concatenate([ang, ang], axis=-1)
    ct = np.ones((128, T), np.float32)
    stb = np.zeros((128, T), np.float32)
    for base in (0, 64):
        ct[base:base + 16] = np.cos(ang).T
        stb[base:base + 16] = np.sin(ang).T
    c["cos_tab"] = ct
    c["sin_tab"] = stb
    idx = np.arange(NFC * 128, dtype=np.int64)
    prod = (idx[:, None] * idx[None, :]) % 8192
    valid = (idx[:, None] <= 4096) & (idx[None, :] <= 4096)
    angd = prod.astype(np.float64) * (2.0 * np.pi / 8192.0)
    tabs = []
    for fn in (np.cos, np.sin):
        m = np.where(valid, fn(angd), 0.0).astype(np.float32)
        m4 = m.reshape(NFC, 128, NFC, 128).transpose(2, 1, 0, 3)
        tabs.append(np.ascontiguousarray(m4).reshape(NFC, 128, NFC * 128))
    c["dft"] = np.stack(tabs, 0).astype(ml_dtypes.bfloat16)
    del prod, angd, valid
    t = np.linspace(0.0, 1.0, T, dtype=np.float32)[:, None]
    wpos = (np.float32(2.0 * math.pi) * np.arange(T, dtype=np.float32)[:, None] / np.float32(T)).astype(np.float32)
    bands = np.linspace(1e-4, 15, 16, dtype=np.float32)[None, :]
    z = np.concatenate([t, np.cos(wpos * bands), -np.sin(wpos * bands)], axis=-1).astype(np.float32)
    c["zT"] = np.ascontiguousarray(z.T)
    tt = np.linspace(0.0, 1.0, T, dtype=np.float32)
    c["negt"] = np.ascontiguousarray(-tt.reshape(NT, 128).T)
    max_decay = math.log(1e-2) / 0.3
    min_decay = math.log(1e-2) / 1.5
    c["absd"] = np.abs(np.linspace(min_decay, max_decay, 256, dtype=np.float32)).astype(np.float32)
    f = np.arange(NFC * 128)
    wf = np.where(f <= 4096, 2.0, 0.0)
    wf[0] = 1.0
    wf[4096] = 1.0
    c["wfT"] = np.ascontiguousarray((wf / 8192.0).astype(np.float32).reshape(NFC, 128).T)
    rc = np.zeros((2, 128, 16), np.float32)
    wins = (2, 4, 8, 16)
    for cp in range(2):
        for half in range(2):
            w = wins[2 * cp + half]
            for j in range(16):
                tpos = j if j < 8 else T - 16 + j
                lo = max(tpos - w // 2, 0)
                hi = min(tpos + w // 2 - 1, T - 1)
                rc[cp, half * 64:(half + 1) * 64, j] = 1.0 / float(hi - lo + 1)
    c["poolrc"] = rc
    _CONST.update(c)
    return _CONST


WEIGHT_SPECS = [
    ("ln0_g", [1024]), ("ln0_b", [1024]), ("w_in", [4, 1024, 2560]), ("hy_conv_w", [4, 3, 768]),
    ("hy_conv_b", [4, 768]), ("hy_fw1", [4, 33, 64]), ("hy_fb1", [4, 64]), ("hy_freq1", [4, 64]),
    ("hy_fw2", [4, 64, 64]), ("hy_fb2", [4, 64]), ("hy_freq2", [4, 64]), ("hy_fw3", [4, 64, 512]),
    ("hy_bias", [4, 256]), ("att_lq1", [4, 64]), ("att_lk1", [4, 64]), ("att_lq2", [4, 64]),
    ("att_lk2", [4, 64]), ("att_subln_g", [4, 128]), ("pool_w", [4, 4, 64, 64]), ("pool_b", [4, 4, 64]),
    ("pool_scale", [4, 256]), ("w_out", [4, 1024, 1024]), ("ln1_g", [4, 1024]), ("ln1_b", [4, 1024]),
    ("moe_wgc", [4, 1024, 4]), ("moe_bgc", [4, 4]), ("moe_wgf", [4, 1024, 16]), ("moe_bgf", [4, 16]),
    ("moe_w1", [4, 16, 1024, 256]), ("moe_w3", [4, 16, 1024, 256]), ("moe_w2", [4, 16, 256, 1024]),
    ("ple_wg", [4, 1024, 1024]), ("ple_bg", [4, 1024]), ("ple_wp", [4, 256, 1024]),
    ("ln2_g", [4, 1024]), ("ln2_b", [4, 1024]),
]
CONST_SPECS = [
    ("ident", [128, 128], F32), ("rotm", [128, 128], F32), ("cos_tab", [128, T], F32), ("sin_tab", [128, T], F32),
    ("dft", [2, NFC, 128, NFC * 128], BF16), ("zT", [33, T], F32), ("negt", [128, NT], F32), ("absd", [256], F32),
    ("wfT", [128, NFC], F32), ("poolrc", [2, 128, 16], F32),
]


class Arena:
    def __init__(self, ap, nbytes):
        self.ap = ap
        self.nbytes = nbytes
        self.top = 0
        self.marks = []

    def alloc(self, shape, dt):
        esz = 2 if dt == BF16 else 4
        n = 1
        for s in shape[1:]:
            n *= s
        nb = (n * esz + 31) // 32 * 32
        off = self.top
        self.top += nb
        assert self.top <= self.nbytes, ("arena overflow", self.top)
        a = self.ap[:, off // 4:(off + nb) // 4]
        if dt == BF16:
            a = a.bitcast(BF16)
        a = a[:, 0:n]
        names = " ".join("d%d" % i for i in range(len(shape) - 1))
        if len(shape) > 2:
            kw = {"d%d" % i: shape[i + 1] for i in range(len(shape) - 1)}
            a = a.rearrange("p (%s) -> p %s" % (names, names), **kw)
        if shape[0] < 128:
            a = a[0:shape[0]]
        return a

    def push(self):
        self.marks.append(self.top)

    def pop(self):
        self.top = self.marks.pop()


class Rot:
    def __init__(self, name, items):
        self.name = name
        self.items = items
        self.i = 0

    def next(self):
        j = self.i % len(self.items)
        self.i += 1
        return self.items[j], (self.name, j)


def col(ap1d):
    return ap1d.rearrange("(p o) -> p o", o=1)


def build(n_layers=DEPTH, stop_after=None, debug=False, skip=()):
    nc = bass.Bass("TRN2", target_bir_lowering=False)
    dr = {}
    dr["x"] = nc.dram_tensor("x", [T, D], F32, kind="ExternalInput").ap()
    NLD = n_layers if debug else DEPTH
    dr["p"] = nc.dram_tensor("p", [NLD, T, 256], F32, kind="ExternalInput").ap()
    for n, s in WEIGHT_SPECS:
        s = list(s)
        if s[0] == DEPTH and n not in ("ln0_g", "ln0_b"):
            s[0] = NLD
        dr[n] = nc.dram_tensor(n, s, F32, kind="ExternalInput").ap()
    for n, s, dt in CONST_SPECS:
        dr[n] = nc.dram_tensor(n, s, dt, kind="ExternalInput").ap()
    out = nc.dram_tensor("out", [T, D], F32, kind="ExternalOutput").ap()
    skind = dict(kind="ExternalOutput") if debug else {}
    H32 = nc.dram_tensor("H32", [T, D], F32, **skind).ap()
    MIXT = nc.dram_tensor("MIXT", [8, 128, T], BF16, **skind).ap()

    es = ExitStack()
    arena_t = es.enter_context(nc.sbuf_tensor("arena", [128, 204 * 256], F32))
    PS = [es.enter_context(nc.psum_tensor("ps%d" % i, [128, 512], F32)) for i in range(8)]
    S = Sched(nc)
    A = Arena(arena_t[:], 204 * 1024)
    final_tokens = []

    hT = A.alloc([128, 8, T], BF16)
    ident = A.alloc([128, 128], F32)
    ones_f = A.alloc([128, 128], F32)
    zeros_b = A.alloc([128, 512], BF16)
    gates = A.alloc([128, NT, 16], F32)
    S.dma("sp", ident, dr["ident"], w=["ident"])
    S.op("dve", lambda e: e.memset(ones_f, 1.0), w=["ones_f"])
    S.op("dve", lambda e: e.memset(zeros_b, 0.0), w=["zeros_b"])

    def hTk(ks, tiles):
        return [("hT", k, i) for k in ks for i in tiles]

    K8 = list(range(8))

    def load_w(dst, src, key, eng="pool"):
        return S.dma(eng, dst, src.rearrange("(k p) n -> p k n", p=128), w=[key])

    def make_ln_bufs():
        b = {}
        b["st"] = Rot("ln_st", [A.alloc([128, 2, 6], F32) for _ in range(2)])
        b["mv"] = Rot("ln_mv", [A.alloc([128, 2], F32) for _ in range(2)])
        b["rs"] = Rot("ln_rs", [A.alloc([128, 1], F32) for _ in range(2)])
        b["hn"] = Rot("ln_hn", [A.alloc([128, D], F32) for _ in range(2)])
        b["bank"] = Rot("ps", [(PS[6], 6), (PS[7], 7)])
        return b

    def ln_tile(bufs, rt, rt_keys, i, g_rep, b_rep, gb_key, dst, dst_key, gating=None):
        st, stk = bufs["st"].next()
        mv, mvk = bufs["mv"].next()
        rs, rsk = bufs["rs"].next()
        hn, hnk = bufs["hn"].next()
        for hh in range(2):
            S.op("dve", lambda e, hh=hh: e.bn_stats(st[:, hh, :], rt[:, hh * 512:(hh + 1) * 512]), r=list(rt_keys), w=[stk + (hh,)])
        S.op("dve", lambda e: e.bn_aggr(mv, st.rearrange("p a b -> p (a b)")), r=[stk + (0,), stk + (1,)], w=[mvk])
        S.op("dve", lambda e: e.tensor_scalar(rs, mv[:, 1:2], LN_EPS, None, ALU.add), r=[mvk], w=[rsk])
        S.op("act", lambda e: e.activation(rs, rs, AF.Sqrt), r=[rsk], w=[rsk])
        S.op("dve", lambda e: e.reciprocal(rs, rs), r=[rsk], w=[rsk])
        S.op("dve", lambda e: e.tensor_scalar(hn, rt, mv[:, 0:1], rs[:, 0:1], ALU.subtract, ALU.mult), r=list(rt_keys) + [mvk, rsk], w=[hnk])
        S.op("dve", lambda e: e.tensor_tensor(hn, hn, g_rep, ALU.mult), r=[hnk, gb_key], w=[hnk])
        S.op("dve", lambda e: e.tensor_tensor(hn, hn, b_rep, ALU.add), r=[hnk, gb_key], w=[hnk])
        tok = S.dma("sp", dst[i * 128:(i + 1) * 128, :], hn, r=[hnk], w=[(dst_key, i)])
        for half in range(2):
            (bank, bi), _ = bufs["bank"].next()
            for j in range(4):
                k = half * 4 + j
                S.op("pe", lambda e, bank=bank, j=j, k=k: e.transpose(bank[:, j * 128:(j + 1) * 128], hn[:, k * 128:(k + 1) * 128], ident),
                     r=[hnk, "ident"], w=[("ps", bi)])
            S.op("act", lambda e, bank=bank, half=half: e.copy(hT[:, half * 4:(half + 1) * 4, i * 128:(i + 1) * 128],
                                                                bank[:, 0:512].rearrange("p (a b) -> p a b", a=4)),
                 r=[("ps", bi)], w=hTk(range(half * 4, half * 4 + 4), [i]))
            if gating is not None:
                if half == 0:
                    gating["cur"] = gating["hTf"].next()
                hTf, hTfk = gating["cur"]
                S.op("act", lambda e, bank=bank, half=half, hTf=hTf: e.copy(hTf[:, half * 4:(half + 1) * 4, :],
                                                                                   bank[:, 0:512].rearrange("p (a b) -> p a b", a=4)),
                     r=[("ps", bi)], w=[hTfk + (half,)])
        if gating is not None:
            hTf, hTfk = gating["cur"]
            gb, gbi = gating["bank"]
            for k in range(8):
                S.op("pe", lambda e, k=k, hTf=hTf: e.matmul(gb[:, 0:20], hTf[:, k, :], gating["wg"][:, k, :], start=(k == 0), stop=(k == 7)),
                     r=[hTfk + (k // 4,), "wg_f"], w=[("ps", gbi)])
            S.op("dve", lambda e: e.tensor_tensor(gating["logits"][:, i, :], gb[:, 0:20], gating["bg"], ALU.add),
                 r=[("ps", gbi), "bg_rep"], w=[("logits", i)])
        return tok

    dbg_list = []

    def dbg_dump(name, ap, shape, dt, rkeys=()):
        if not debug:
            return
        t = nc.dram_tensor("dbg_" + name, shape, dt, kind="ExternalOutput").ap()
        S.dma("sp", t, ap, r=list(rkeys))

    def dbg_stop(layer, phase):
        return stop_after is not None and stop_after == (layer, phase)

    A.push()
    g_rep = A.alloc([128, D], F32)
    b_rep = A.alloc([128, D], F32)
    S.dma("sp", g_rep, dr["ln0_g"].partition_broadcast(128), w=["gb0"])
    S.dma("sp", b_rep, dr["ln0_b"].partition_broadcast(128), w=["gb0"])
    lnb = make_ln_bufs()
    xt = Rot("xt", [A.alloc([128, D], F32) for _ in range(2)])
    for i in range(NT):
        x_t, xk = xt.next()
        S.dma("act", x_t, dr["x"][i * 128:(i + 1) * 128, :], w=[xk])
        ln_tile(lnb, x_t, [xk], i, g_rep, b_rep, "gb0", H32, "H32")
    A.pop()
    S.barrier()

    done = False
    for l in range(n_layers):
        lam_init = 0.8 - 0.6 * math.exp(-0.3 * l)
        last = (l == DEPTH - 1)
        for _ph in ([0] if "A" not in skip else []):
            A.push()
            PA = A.top
            VK = A.alloc([128, NT, 768], BF16)
            A.alloc([128, 16], F32)
            A.push()
            zT = A.alloc([33, T], F32)
            S.dma("sp", zT, dr["zT"], w=["zT"])
            fw1 = A.alloc([33, 64], F32)
            fw2 = A.alloc([64, 64], F32)
            fw3 = A.alloc([64, 512], F32)
            S.dma("sp", fw1, dr["hy_fw1"][l], w=["fw1"])
            S.dma("sp", fw2, dr["hy_fw2"][l], w=["fw2"])
            S.dma("sp", fw3, dr["hy_fw3"][l], w=["fw3"])
            fcol = A.alloc([64, 8], F32)
            S.dma("sp", fcol[:, 0:1], col(dr["hy_fb1"][l]), w=["fcol"])
            S.dma("sp", fcol[:, 1:2], col(dr["hy_freq1"][l]), w=["fcol"])
            S.dma("sp", fcol[:, 2:3], col(dr["hy_fb2"][l]), w=["fcol"])
            S.dma("sp", fcol[:, 3:4], col(dr["hy_freq2"][l]), w=["fcol"])
            S.op("dve", lambda e: e.tensor_tensor(fcol[:, 4:5], fcol[:, 0:1], fcol[:, 1:2], ALU.mult), r=["fcol"], w=["fcol"])
            S.op("dve", lambda e: e.tensor_tensor(fcol[:, 5:6], fcol[:, 2:3], fcol[:, 3:4], ALU.mult), r=["fcol"], w=["fcol"])
            hdn1 = A.alloc([64, T], F32)
            hdn2 = A.alloc([64, T], F32)
            arg = Rot("arg", [A.alloc([64, 512], F32) for _ in range(2)])
            m1b = Rot("m1b", [A.alloc([64, 512], F32) for _ in range(2)])
            m2b = Rot("m2b", [A.alloc([64, 512], F32) for _ in range(2)])
            absd = A.alloc([128, 256], F32)
            S.dma("sp", absd, dr["absd"].partition_broadcast(128), w=["absd"])
            negt = A.alloc([128, NT], F32)
            S.dma("sp", negt, dr["negt"], w=["negt"])
            hb = A.alloc([1, 256], F32)
            S.dma("sp", hb, dr["hy_bias"][l].rearrange("(o c) -> o c", o=1), w=["hb"])

            def sin_layer(ps, pk, fq, bs, dst, dkey):
                a, ak = arg.next()
                m1, m1k = m1b.next()
                m2, m2k = m2b.next()
                S.op("dve", lambda e: e.tensor_scalar(a, ps, fcol[:, fq:fq + 1], fcol[:, bs:bs + 1], ALU.mult, ALU.add), r=[pk, "fcol"], w=[ak])
                S.op("dve", lambda e: e.tensor_scalar(m1, a, -PI, 2 * PI, ALU.is_lt, ALU.mult), r=[ak], w=[m1k])
                S.op("dve", lambda e: e.tensor_scalar(m2, a, PI, 2 * PI, ALU.is_gt, ALU.mult), r=[ak], w=[m2k])
                S.op("dve", lambda e: e.tensor_tensor(a, a, m1, ALU.add), r=[ak, m1k], w=[ak])
                S.op("dve", lambda e: e.tensor_tensor(a, a, m2, ALU.subtract), r=[ak, m2k], w=[ak])
                S.op("act", lambda e: e.activation(dst, a, AF.Sin), r=[ak], w=[dkey])

            for tc in range(8):
                sl = slice(tc * 512, (tc + 1) * 512)
                S.op("pe", lambda e, sl=sl: e.matmul(PS[0][0:64, :], fw1, zT[:, sl], start=True, stop=True), r=["fw1", "zT"], w=[("ps", 0)])
                sin_layer(PS[0][0:64, :], ("ps", 0), 1, 4, hdn1[:, sl], ("hdn1", tc))
                S.op("pe", lambda e, sl=sl: e.matmul(PS[1][0:64, :], fw2, hdn1[:, sl], start=True, stop=True), r=["fw2", ("hdn1", tc)], w=[("ps", 1)])
                sin_layer(PS[1][0:64, :], ("ps", 1), 3, 5, hdn2[:, sl], ("hdn2", tc))

            win = Rot("win", [A.alloc([128, 256], F32) for _ in range(2)])
            kw = Rot("kw", [A.alloc([128, 512], F32) for _ in range(2)])
            sq = Rot("sq", [A.alloc([128, 512], F32) for _ in range(2)])
            nrm = A.alloc([128, 256], F32)
            kbank = Rot("ps", [(PS[2], 2), (PS[3], 3)])

            def filt_tile(i):
                (pb, pbi), _ = kbank.next()
                S.op("pe", lambda e: e.matmul(pb[:, :], hdn2[:, i * 128:(i + 1) * 128], fw3, start=True, stop=True),
                     r=[("hdn2", i // 4), "fw3"], w=[("ps", pbi)])
                wn, wnk = win.next()
                S.op("act", lambda e: e.activation(wn, absd, AF.Exp, scale=negt[:, i:i + 1]), r=["absd", "negt"], w=[wnk])
                k_, kk = kw.next()
                for hh in range(2):
                    S.op("dve", lambda e, hh=hh: e.tensor_tensor(k_[:, hh * 256:(hh + 1) * 256], pb[:, hh * 256:(hh + 1) * 256], wn, ALU.mult),
                         r=[("ps", pbi), wnk], w=[kk])
                if i == 0:
                    S.op("dve", lambda e: e.memset(k_[0:1, 256:512], 0.0), w=[kk])
                return k_, kk

            for i in range(NT):
                k_, kk = filt_tile(i)
                s_, sk = sq.next()
                S.op("act", lambda e, k_=k_, s_=s_: e.activation(s_, k_, AF.Square), r=[kk], w=[sk])
                S.op("pe", lambda e, s_=s_, i=i: e.matmul(PS[4][:, :], ones_f, s_, start=(i == 0), stop=(i == NT - 1)), r=["ones_f", sk], w=[("ps", 4)])
            S.op("dve", lambda e: e.tensor_copy(nrm, PS[4][:, 0:256]), r=[("ps", 4)], w=["nrm"])
            S.op("dve", lambda e: e.tensor_tensor(nrm, nrm, PS[4][:, 256:512], ALU.add), r=[("ps", 4), "nrm"], w=["nrm"])
            S.op("dve", lambda e: e.tensor_scalar(nrm, nrm, 1e-6, None, ALU.add), r=["nrm"], w=["nrm"])
            S.op("act", lambda e: e.activation(nrm, nrm, AF.Sqrt), r=["nrm"], w=["nrm"])
            S.op("dve", lambda e: e.reciprocal(nrm, nrm), r=["nrm"], w=["nrm"])
            for i in range(NT):
                k_, kk = filt_tile(i)
                s_, sk = sq.next()
                S.op("dve", lambda e, k_=k_, s_=s_: e.tensor_tensor(s_[:, 0:256], k_[:, 0:256], k_[:, 256:512], ALU.add), r=[kk], w=[sk])
                S.op("dve", lambda e, k_=k_, s_=s_: e.tensor_tensor(s_[:, 256:512], k_[:, 256:512], k_[:, 0:256], ALU.subtract), r=[kk], w=[sk])
                for hh in range(2):
                    S.op("dve", lambda e, s_=s_, hh=hh: e.tensor_tensor(s_[:, hh * 256:(hh + 1) * 256], s_[:, hh * 256:(hh + 1) * 256], nrm, ALU.mult),
                         r=[sk, "nrm"], w=[sk])
                if i == 0:
                    S.op("dve", lambda e, s_=s_: e.tensor_tensor(s_[0:1, 0:256], s_[0:1, 0:256], hb, ALU.add), r=[sk, "hb"], w=[sk])
                    S.op("dve", lambda e, s_=s_: e.tensor_tensor(s_[0:1, 256:512], s_[0:1, 256:512], hb, ALU.subtract), r=[sk, "hb"], w=[sk])
                S.op("act", lambda e, s_=s_, i=i: e.copy(VK[:, i, 0:256], s_[:, 0:256]), r=[sk], w=[("VKk", i)])
                S.op("act", lambda e, s_=s_, i=i: e.copy(VK[:, i, 512:768], s_[:, 256:512]), r=[sk], w=[("VKk", i)])
            A.pop()
            S.barrier()
            A.push()
            ub = A.alloc([128, T + 2], F32)
            c1 = A.alloc([128, T], F32)
            vp = A.alloc([128, T], F32)
            wch = Rot("wch", [A.alloc([128, 8, 128], BF16) for _ in range(2)])
            cw = Rot("cw", [A.alloc([128, 4], F32) for _ in range(2)])
            S.op("dve", lambda e: e.memset(ub[:, 0:1], 0.0), w=["ub_h"])
            S.op("dve", lambda e: e.memset(ub[:, T + 1:T + 2], 0.0), w=["ub_h"])
            pbank = Rot("ps", [(PS[0], 0), (PS[1], 1)])
            tbank = Rot("ps", [(PS[2], 2), (PS[3], 3)])
            for cc in range(2):
                for sname, coff in (("x1", 256), ("v", 512)):
                    c0 = coff + cc * 128
                    w_, wk = wch.next()
                    load_w(w_, dr["w_in"][l][:, c0:c0 + 128], wk)
                    cw_, cwk = cw.next()
                    for j in range(3):
                        S.dma("sp", cw_[:, j:j + 1], col(dr["hy_conv_w"][l, j, c0:c0 + 128]), w=[cwk])
                    S.dma("sp", cw_[:, 3:4], col(dr["hy_conv_b"][l, c0:c0 + 128]), w=[cwk])
                    for tc in range(8):
                        (pb, pbi), _ = pbank.next()
                        for k in range(8):
                            S.op("pe", lambda e, pb=pb, k=k, tc=tc, w_=w_: e.matmul(pb[:, :], w_[:, k, :], hT[:, k, tc * 512:(tc + 1) * 512], start=(k == 0), stop=(k == 7)),
                                 r=[wk] + hTk([k], range(tc * 4, tc * 4 + 4)), w=[("ps", pbi)])
                        S.op("act", lambda e, pb=pb, tc=tc: e.copy(ub[:, 1 + tc * 512:1 + (tc + 1) * 512], pb[:, :]), r=[("ps", pbi)], w=[("ub", tc)])
                    ubk = [("ub", tc) for tc in range(8)] + ["ub_h"]
                    dst = {"x1": c1, "v": vp}[sname]
                    dk = {"x1": "c1", "v": "vp"}[sname]
                    S.op("dve", lambda e, dst=dst, cw_=cw_: e.tensor_scalar(dst, ub[:, 0:T], cw_[:, 0:1], cw_[:, 3:4], ALU.mult, ALU.add), r=ubk + [cwk], w=[dk])
                    S.op("dve", lambda e, dst=dst, cw_=cw_: e.scalar_tensor_tensor(dst, ub[:, 1:T + 1], cw_[:, 1:2], dst, ALU.mult, ALU.add), r=ubk + [cwk, dk], w=[dk])
                    S.op("dve", lambda e, dst=dst, cw_=cw_: e.scalar_tensor_tensor(dst, ub[:, 2:T + 2], cw_[:, 2:3], dst, ALU.mult, ALU.add), r=ubk + [cwk, dk], w=[dk])
                    if sname == "v":
                        S.op("dve", lambda e: e.tensor_tensor(vp, vp, c1, ALU.mult), r=["vp", "c1"], w=["vp"])
                        for i4 in range(8):
                            (tb, tbi), _ = tbank.next()
                            for j in range(4):
                                i = i4 * 4 + j
                                S.op("pe", lambda e, tb=tb, j=j, i=i: e.transpose(tb[:, j * 128:(j + 1) * 128], vp[:, i * 128:(i + 1) * 128], ident),
                                     r=["vp", "ident"], w=[("ps", tbi)])
                            S.op("act", lambda e, tb=tb, i4=i4, cc=cc: e.copy(VK[:, i4 * 4:(i4 + 1) * 4, 256 + cc * 128:256 + (cc + 1) * 128],
                                                                             tb[:, 0:512].rearrange("p (a b) -> p a b", a=4)),
                                 r=[("ps", tbi)], w=[("VKv", cc, i4)])
            S.barrier()
            if l == 0:
                pass
                pass
                pass
                dbg_dump("cw", cw_, [128, 4], F32)
                pass
                S.barrier()
            A.pop()
            if dbg_stop(l, "A2"):
                break
            A.push()
            Zc = A.alloc([128, NFC, 256], BF16)
            Zs = A.alloc([128, NFC, 256], BF16)
            tabC = Rot("tabC", [A.alloc([128, NFC * 128], BF16) for _ in range(2)])
            tabS = Rot("tabS", [A.alloc([128, NFC * 128], BF16) for _ in range(2)])
            wfT = A.alloc([128, NFC], F32)
            S.dma("sp", wfT, dr["wfT"], w=["wfT"])
            T3 = A.top
            csb = Rot("csb", [A.alloc([128, 512], F32) for _ in range(2)])
            ssb = Rot("ssb", [A.alloc([128, 512], F32) for _ in range(2)])
            tm = Rot("tm", [A.alloc([128, 4, 256], F32) for _ in range(2)])
            cbank = Rot("ps", [(PS[0], 0), (PS[1], 1)])
            sbank = Rot("ps", [(PS[2], 2), (PS[3], 3)])
            for j in range(NFC):
                tc_, tck = tabC.next()
                ts_, tsk = tabS.next()
                S.dma("sp", tc_[:, 0:NT * 128], dr["dft"][0, j][:, 0:NT * 128], w=[tck])
                S.dma("act", ts_[:, 0:NT * 128], dr["dft"][1, j][:, 0:NT * 128], w=[tsk])
                (pc, pci), _ = cbank.next()
                (psn, psi), _ = sbank.next()
                for i in range(NT):
                    S.op("pe", lambda e, pc=pc, tc_=tc_, i=i: e.matmul(pc[:, :], tc_[:, i * 128:(i + 1) * 128], VK[:, i, 0:512], start=(i == 0), stop=(i == NT - 1)),
                         r=[tck], w=[("ps", pci)])
                for i in range(NT):
                    S.op("pe", lambda e, psn=psn, ts_=ts_, i=i: e.matmul(psn[:, :], ts_[:, i * 128:(i + 1) * 128], VK[:, i, 256:768], start=(i == 0), stop=(i == NT - 1)),
                         r=[tsk], w=[("ps", psi)])
                c_, ck = csb.next()
                s_, sk = ssb.next()
                t_, tk = tm.next()
                S.op("dve", lambda e, c_=c_, pc=pc, j=j: e.tensor_scalar(c_[:, 0:256], pc[:, 0:256], wfT[:, j:j + 1], None, ALU.mult), r=[("ps", pci), "wfT"], w=[ck])
                S.op("act", lambda e, c_=c_, pc=pc: e.copy(c_[:, 256:512], pc[:, 256:512]), r=[("ps", pci)], w=[ck])
                S.op("act", lambda e, s_=s_, psn=psn: e.copy(s_[:, 0:256], psn[:, 0:256]), r=[("ps", psi)], w=[sk])
                S.op("dve", lambda e, s_=s_, psn=psn, j=j: e.tensor_scalar(s_[:, 256:512], psn[:, 256:512], wfT[:, j:j + 1], None, ALU.mult), r=[("ps", psi), "wfT"], w=[sk])
                S.op("dve", lambda e, t_=t_, c_=c_: e.tensor_tensor(t_[:, 0, :], c_[:, 256:512], c_[:, 0:256], ALU.mult), r=[ck], w=[tk + (0,)])
                S.op("dve", lambda e, t_=t_, s_=s_: e.tensor_tensor(t_[:, 1, :], s_[:, 0:256], s_[:, 256:512], ALU.mult), r=[sk], w=[tk + (1,)])
                S.op("dve", lambda e, t_=t_, j=j: e.tensor_tensor(Zc[:, j, :], t_[:, 0, :], t_[:, 1, :], ALU.add), r=[tk + (0,), tk + (1,)], w=[("Z", j)])
                S.op("dve", lambda e, t_=t_, s_=s_, c_=c_: e.tensor_tensor(t_[:, 2, :], s_[:, 0:256], c_[:, 0:256], ALU.mult), r=[sk, ck], w=[tk + (2,)])
                S.op("dve", lambda e, t_=t_, s_=s_, c_=c_: e.tensor_tensor(t_[:, 3, :], c_[:, 256:512], s_[:, 256:512], ALU.mult), r=[sk, ck], w=[tk + (3,)])
                S.op("dve", lambda e, t_=t_, j=j: e.tensor_tensor(Zs[:, j, :], t_[:, 2, :], t_[:, 3, :], ALU.subtract), r=[tk + (2,), tk + (3,)], w=[("Z", j)])
            S.barrier()
            if l == 0:
                pass
                pass
                S.barrier()
            A.top = T3
            mst = A.alloc([128, 2, T], BF16)
            ysb = Rot("ysb", [A.alloc([128, 256], F32) for _ in range(2)])
            w_ = A.alloc([128, 8, 128], BF16)
            cw_ = A.alloc([128, 4], F32)
            TOPR = A.top
            A.top = PA
            ub = A.alloc([128, T + 2], F32)
            c1 = A.alloc([128, T], F32)
            x0c = A.alloc([128, 2, T], BF16)
            assert A.top <= T3 and A.top <= PA + 49152 + 64
            A.top = TOPR
            S.op("dve", lambda e: e.memset(ub[:, 0:1], 0.0), w=["ub_h"])
            S.op("dve", lambda e: e.memset(ub[:, T + 1:T + 2], 0.0), w=["ub_h"])
            pbank = Rot("ps", [(PS[0], 0), (PS[1], 1)])
            for cc in range(2):
                c0 = cc * 128
                load_w(w_, dr["w_in"][l][:, c0:c0 + 128], "wchx")
                for j in range(3):
                    S.dma("sp", cw_[:, j:j + 1], col(dr["hy_conv_w"][l, j, c0:c0 + 128]), w=["cwx"])
                S.dma("sp", cw_[:, 3:4], col(dr["hy_conv_b"][l, c0:c0 + 128]), w=["cwx"])
                for tc in range(8):
                    (pb, pbi), _ = pbank.next()
                    for k in range(8):
                        S.op("pe", lambda e, pb=pb, k=k, tc=tc: e.matmul(pb[:, :], w_[:, k, :], hT[:, k, tc * 512:(tc + 1) * 512], start=(k == 0), stop=(k == 7)),
                             r=["wchx"] + hTk([k], range(tc * 4, tc * 4 + 4)), w=[("ps", pbi)])
                    S.op("act", lambda e, pb=pb, tc=tc: e.copy(ub[:, 1 + tc * 512:1 + (tc + 1) * 512], pb[:, :]), r=[("ps", pbi)], w=[("ub", tc)])
                ubk = [("ub", tc) for tc in range(8)] + ["ub_h"]
                S.op("dve", lambda e: e.tensor_scalar(c1, ub[:, 0:T], cw_[:, 0:1], cw_[:, 3:4], ALU.mult, ALU.add), r=ubk + ["cwx"], w=["c1"])
                S.op("dve", lambda e: e.scalar_tensor_tensor(c1, ub[:, 1:T + 1], cw_[:, 1:2], c1, ALU.mult, ALU.add), r=ubk + ["cwx", "c1"], w=["c1"])
                S.op("dve", lambda e, cc=cc: e.scalar_tensor_tensor(x0c[:, cc, :], ub[:, 2:T + 2], cw_[:, 2:3], c1, ALU.mult, ALU.add), r=ubk + ["cwx", "c1"], w=[("x0c", cc)])
            ybank = Rot("ps", [(PS[4], 4), (PS[5], 5)])
            t2bank = Rot("ps", [(PS[6], 6), (PS[7], 7)])
            Zkeys = [("Z", j) for j in range(NFC)]
            for i in range(NT):
                tc_, tck = tabC.next()
                ts_, tsk = tabS.next()
                S.dma("sp", tc_, dr["dft"][0, i], w=[tck])
                S.dma("act", ts_, dr["dft"][1, i], w=[tsk])
                (py, pyi), _ = ybank.next()
                for jj in range(NFC):
                    S.op("pe", lambda e, py=py, tc_=tc_, jj=jj: e.matmul(py[:, 0:256], tc_[:, jj * 128:(jj + 1) * 128], Zc[:, jj, :], start=(jj == 0), stop=False),
                         r=[tck] + (Zkeys if jj == 0 else []), w=[("ps", pyi)])
                    S.op("pe", lambda e, py=py, ts_=ts_, jj=jj: e.matmul(py[:, 0:256], ts_[:, jj * 128:(jj + 1) * 128], Zs[:, jj, :], start=False, stop=(jj == NFC - 1)),
                         r=[tsk], w=[("ps", pyi)])
                y_, yk = ysb.next()
                S.op("act", lambda e, y_=y_, py=py: e.copy(y_, py[:, 0:256]), r=[("ps", pyi)], w=[yk])
                (tb, tbi), _ = t2bank.next()
                for cc in range(2):
                    S.op("pe", lambda e, tb=tb, y_=y_, cc=cc: e.transpose(tb[:, cc * 128:(cc + 1) * 128], y_[:, cc * 128:(cc + 1) * 128], ident),
                         r=[yk, "ident"], w=[("ps", tbi)])
                S.op("dve", lambda e, tb=tb, i=i: e.tensor_tensor(mst[:, :, i * 128:(i + 1) * 128], tb[:, 0:256].rearrange("p (a b) -> p a b", a=2),
                                                                  x0c[:, :, i * 128:(i + 1) * 128], ALU.mult),
                     r=[("ps", tbi), ("x0c", 0), ("x0c", 1)], w=[("mst", i)])
            for cc in range(2):
                S.dma("sp", MIXT[cc], mst[:, cc, :], r=[("mst", i) for i in range(NT)], w=[("MIXT", cc)])
            A.pop()
            A.pop()
            S.barrier()
        if dbg_stop(l, "A"):
            break

        for _ph in ([0] if "B" not in skip else []):
            A.push()
            cos_t = A.alloc([128, T], F32)
            sin_t = A.alloc([128, T], F32)
            S.dma("sp", cos_t, dr["cos_tab"], w=["cos_t"])
            S.dma("act", sin_t, dr["sin_tab"], w=["sin_t"])
            rotm = A.alloc([128, 128], F32)
            S.dma("sp", rotm, dr["rotm"], w=["rotm"])
            Va = A.alloc([128, NT, 4, 130], BF16)
            qa = A.alloc([128, T], BF16)
            qb = A.alloc([128, T], BF16)
            kT = A.alloc([128, T], BF16)
            S.op("dve", lambda e: e.memset(qa[64:128, :], 0.0), w=["qz"])
            S.op("dve", lambda e: e.memset(qb[0:64, :], 0.0), w=["qz"])
            wv = A.alloc([128, 8, 512], BF16)
            wqk = Rot("wqk", [A.alloc([128, 8, 128], BF16) for _ in range(2)])
            tmpf = Rot("tmpf", [A.alloc([128, 512], F32) for _ in range(2)])
            r1t = Rot("r1t", [A.alloc([128, 512], F32) for _ in range(2)])
            r2t = Rot("r2t", [A.alloc([128, 512], F32) for _ in range(2)])
            Pb = Rot("Pb", [A.alloc([128, 512], BF16) for _ in range(4)])
            mst = A.alloc([128, T], BF16)
            osb = Rot("osb", [A.alloc([128, 128], F32) for _ in range(2)])
            t2b = Rot("t2b", [A.alloc([128, 128], F32) for _ in range(2)])
            junk = A.alloc([128, 128], F32)
            sm = Rot("sm", [A.alloc([128, 8], F32) for _ in range(2)])
            gsub = A.alloc([128, 128], F32)
            lqk = A.alloc([128, 4, 64], F32)
            lam_t = A.alloc([128, 8], F32)
            for j, nm in enumerate(("att_lq1", "att_lk1", "att_lq2", "att_lk2")):
                S.dma("sp", lqk[:, j, :], dr[nm][l].partition_broadcast(128), w=["lqk"])
            S.op("dve", lambda e: e.tensor_tensor(lqk[:, 0, :], lqk[:, 0, :], lqk[:, 1, :], ALU.mult), r=["lqk"], w=["lqk"])
            S.op("dve", lambda e: e.tensor_tensor(lqk[:, 2, :], lqk[:, 2, :], lqk[:, 3, :], ALU.mult), r=["lqk"], w=["lqk"])
            S.op("dve", lambda e: e.tensor_reduce(lam_t[:, 0:1], lqk[:, 0, :], AX.X, ALU.add), r=["lqk"], w=["lam"])
            S.op("dve", lambda e: e.tensor_reduce(lam_t[:, 1:2], lqk[:, 2, :], AX.X, ALU.add), r=["lqk"], w=["lam"])
            S.op("act", lambda e: e.activation(lam_t[:, 2:4], lam_t[:, 0:2], AF.Exp), r=["lam"], w=["lam"])
            S.op("dve", lambda e: e.tensor_tensor(lam_t[:, 4:5], lam_t[:, 3:4], lam_t[:, 2:3], ALU.subtract), r=["lam"], w=["lam"])
            S.op("dve", lambda e: e.tensor_scalar(lam_t[:, 5:6], lam_t[:, 4:5], -lam_init, None, ALU.add), r=["lam"], w=["lam"])
            neglam = lam_t[:, 5:6]
            S.dma("sp", gsub, dr["att_subln_g"][l].partition_broadcast(128), w=["gsub"])
            S.op("dve", lambda e: e.tensor_scalar(gsub, gsub, 1.0 - lam_init, None, ALU.mult), r=["gsub"], w=["gsub"])
            S.op("dve", lambda e: e.memset(Va[:, :, :, 128:130], 1.0), w=["Va1"])
            S.dma("pool", wv, dr["w_in"][l][:, O_V:O_V + 512].rearrange("(k p) n -> p k n", p=128), w=["wv"])
            vbank = Rot("ps", [(PS[6], 6), (PS[7], 7)])
            for i in range(NT):
                (pb, pbi), _ = vbank.next()
                for k in range(8):
                    S.op("pe", lambda e, pb=pb, k=k, i=i: e.matmul(pb[:, :], hT[:, k, i * 128:(i + 1) * 128], wv[:, k, :], start=(k == 0), stop=(k == 7)),
                         r=["wv"] + hTk([k], [i]), w=[("ps", pbi)])
                S.op("act", lambda e, pb=pb, i=i: e.copy(Va[:, i, :, 0:128], pb[:, :].rearrange("p (a b) -> p a b", a=4)), r=[("ps", pbi)], w=[("Va", i)])
            Vkeys = [("Va", i) for i in range(NT)] + ["Va1"]
            accb = [(PS[2], 2), (PS[3], 3), (PS[4], 4), (PS[5], 5)]
            sbank = Rot("ps", [(PS[0], 0), (PS[1], 1)])
            for hd in range(4):
                for which, dstT, dkey, coff in (("q", None, "qT", O_Q), ("k", kT, "kT", O_K)):
                    w_, wk = wqk.next()
                    load_w(w_, dr["w_in"][l][:, coff + hd * 128:coff + (hd + 1) * 128], wk)
                    for tc in range(8):
                        sl = slice(tc * 512, (tc + 1) * 512)
                        (pb, pbi), _ = vbank.next()
                        for k in range(8):
                            S.op("pe", lambda e, pb=pb, k=k, sl=sl, w_=w_: e.matmul(pb[:, :], w_[:, k, :], hT[:, k, sl], start=(k == 0), stop=(k == 7)),
                                 r=[wk] + hTk([k], range(tc * 4, tc * 4 + 4)), w=[("ps", pbi)])
                        tf, tfk = tmpf.next()
                        S.op("act", lambda e, tf=tf, pb=pb: e.copy(tf, pb[:, :]), r=[("ps", pbi)], w=[tfk])
                        if which == "k":
                            S.op("act", lambda e, pb=pb, dstT=dstT, sl=sl: e.copy(dstT[:, sl], pb[:, :]), r=[("ps", pbi)], w=[(dkey, tc)])
                        else:
                            S.op("act", lambda e, pb=pb, sl=sl: e.copy(qa[0:64, sl], pb[0:64, :]), r=[("ps", pbi), "qz"], w=[(dkey, tc)])
                            S.op("act", lambda e, pb=pb, sl=sl: e.copy(qb[64:128, sl], pb[64:128, :]), r=[("ps", pbi), "qz"], w=[(dkey, tc)])
                        (pr, pri), _ = vbank.next()
                        S.op("pe", lambda e, pr=pr, tf=tf: e.matmul(pr[:, :], rotm, tf, start=True, stop=True), r=["rotm", tfk], w=[("ps", pri)])
                        a1, a1k = r1t.next()
                        a2, a2k = r2t.next()
                        for base in (0, 64):
                            ps_ = slice(base, base + 16)
                            S.op("dve", lambda e, a1=a1, tf=tf, ps_=ps_, sl=sl: e.tensor_tensor(a1[ps_, :], tf[ps_, :], cos_t[ps_, sl], ALU.mult), r=[tfk, "cos_t"], w=[a1k])
                            S.op("dve", lambda e, a2=a2, pr=pr, ps_=ps_, sl=sl: e.tensor_tensor(a2[ps_, :], pr[ps_, :], sin_t[ps_, sl], ALU.mult), r=[("ps", pri), "sin_t"], w=[a2k])
                            dT = dstT if which == "k" else (qa if base == 0 else qb)
                            S.op("dve", lambda e, a1=a1, a2=a2, ps_=ps_, sl=sl, dT=dT: e.tensor_tensor(dT[ps_, sl], a1[ps_, :], a2[ps_, :], ALU.add), r=[a1k, a2k], w=[(dkey, tc)])
                kkeys = [("kT", tc) for tc in range(8)]
                for qc in range(8):
                    qsl = slice(qc * 512, (qc + 1) * 512)
                    for (ab, abi) in accb:
                        S.op("pe", lambda e, ab=ab: e.matmul(ab[:, :], zeros_b[:, 0:128], zeros_b[:, :], start=True, stop=False, skip_group_check=True),
                             r=["zeros_b"], w=[("ps", abi)])
                    its = [(kc, mp) for kc in range(NT) for mp in range(2)]

                    def emit_score(kc, mp):
                        (sb_, sbi), _ = sbank.next()
                        qq = qa if mp == 0 else qb
                        S.op("pe", lambda e, sb_=sb_, kc=kc, qsl=qsl, qq=qq: e.matmul(sb_[:, :], kT[:, kc * 128:(kc + 1) * 128], qq[:, qsl], start=True, stop=True),
                             r=[("kT", kc // 4), ("qT", qc), "qz"], w=[("ps", sbi)])
                        return sb_, sbi

                    nxt = emit_score(*its[0])
                    for n_, (kc, mp) in enumerate(its):
                        sb_, sbi = nxt
                        if n_ + 1 < len(its):
                            nxt = emit_score(*its[n_ + 1])
                        if True:
                            P_, Pk = Pb.next()
                            S.op("act", lambda e, P_=P_, sb_=sb_: e.activation(P_, sb_[:, :], AF.Exp, scale=0.125), r=[("ps", sbi)], w=[Pk])
                            for sub in range(4):
                                ab, abi = accb[mp * 2 + sub // 2]
                                co = (sub % 2) * 256
                                S.op("pe", lambda e, ab=ab, co=co, P_=P_, sub=sub, kc=kc, hd=hd: e.matmul(ab[:, co:co + 130], P_[:, sub * 128:(sub + 1) * 128], Va[:, kc, hd, 0:130],
                                                                                                            start=False, stop=(kc == NT - 1), skip_group_check=True),
                                     r=[Pk, ("Va", kc), "Va1"], w=[("ps", abi)])
                    for sub in range(4):
                        i = qc * 4 + sub
                        co = (sub % 2) * 256
                        a1b, a1i = accb[sub // 2]
                        a2b, a2i = accb[2 + sub // 2]
                        s_, sk = sm.next()
                        o_, ok = osb.next()
                        t2, t2k = t2b.next()
                        S.op("dve", lambda e, s_=s_, a1b=a1b, co=co: e.reciprocal(s_[:, 0:1], a1b[:, co + 128:co + 129]), r=[("ps", a1i)], w=[sk])
                        S.op("dve", lambda e, s_=s_, a2b=a2b, co=co: e.reciprocal(s_[:, 1:2], a2b[:, co + 128:co + 129]), r=[("ps", a2i)], w=[sk])
                        S.op("dve", lambda e, s_=s_: e.tensor_tensor(s_[:, 2:3], s_[:, 1:2], neglam, ALU.mult), r=[sk, "lam"], w=[sk])
                        S.op("dve", lambda e, t2=t2, a2b=a2b, co=co, s_=s_: e.tensor_scalar(t2, a2b[:, co:co + 128], s_[:, 2:3], None, ALU.mult), r=[("ps", a2i), sk], w=[t2k])
                        S.op("dve", lambda e, o_=o_, a1b=a1b, co=co, s_=s_, t2=t2: e.scalar_tensor_tensor(o_, a1b[:, co:co + 128], s_[:, 0:1], t2, ALU.mult, ALU.add),
                             r=[("ps", a1i), sk, t2k], w=[ok])
                        S.op("act", lambda e, o_=o_, s_=s_: e.activation(junk, o_, AF.Square, accum_out=s_[:, 3:4]), r=[ok], w=[sk, "junk"])
                        S.op("dve", lambda e, s_=s_: e.tensor_scalar(s_[:, 4:5], s_[:, 3:4], 1.0 / 128.0, 1e-5, ALU.mult, ALU.add), r=[sk], w=[sk])
                        S.op("act", lambda e, s_=s_: e.activation(s_[:, 5:6], s_[:, 4:5], AF.Sqrt), r=[sk], w=[sk])
                        S.op("dve", lambda e, s_=s_: e.reciprocal(s_[:, 6:7], s_[:, 5:6]), r=[sk], w=[sk])
                        S.op("dve", lambda e, o_=o_, s_=s_: e.scalar_tensor_tensor(o_, o_, s_[:, 6:7], gsub, ALU.mult, ALU.mult), r=[ok, sk, "gsub"], w=[ok])
                        (tb, tbi), _ = vbank.next()
                        S.op("pe", lambda e, tb=tb, o_=o_: e.transpose(tb[:, 0:128], o_, ident), r=[ok, "ident"], w=[("ps", tbi)])
                        S.op("act", lambda e, tb=tb, i=i: e.copy(mst[:, i * 128:(i + 1) * 128], tb[:, 0:128]), r=[("ps", tbi)], w=[("mstB", i)])
                S.dma("sp", MIXT[2 + hd], mst, r=[("mstB", i) for i in range(NT)], w=[("MIXT", 2 + hd)])
            A.pop()
            S.barrier()
        if dbg_stop(l, "B"):
            break

        for _ph in ([0] if "C" not in skip else []):
            A.push()
            LE = T + 32
            ub = A.alloc([128, LE], F32)
            wa = A.alloc([128, LE], F32)
            wb = A.alloc([128, LE], F32)
            yb = A.alloc([128, T], F32)
            bd = A.alloc([128, 128], F32)
            pcol = A.alloc([128, 2], F32)
            prc = A.alloc([128, 16], F32)
            etmp = A.alloc([128, 16], F32)
            mst = A.alloc([128, T], BF16)
            wch = Rot("wch", [A.alloc([128, 8, 128], BF16) for _ in range(2)])
            pbank = Rot("ps", [(PS[0], 0), (PS[1], 1)])
            obank = Rot("ps", [(PS[2], 2), (PS[3], 3)])
            wins = (2, 4, 8, 16)
            for cp in range(2):
                c0 = O_P + cp * 128
                w_, wk = wch.next()
                load_w(w_, dr["w_in"][l][:, c0:c0 + 128], wk)
                S.op("dve", lambda e: e.memset(ub, 0.0), w=["pub"])
                S.op("dve", lambda e: e.memset(wa, 0.0), w=["pwa"])
                S.op("dve", lambda e: e.memset(wb, 0.0), w=["pwb"])
                S.op("dve", lambda e: e.memset(bd, 0.0), w=["bd"])
                S.dma("sp", bd[0:64, 0:64], dr["pool_w"][l, 2 * cp], w=["bd"])
                S.dma("sp", bd[64:128, 64:128], dr["pool_w"][l, 2 * cp + 1], w=["bd"])
                S.dma("sp", pcol[:, 0:1], col(dr["pool_b"][l].rearrange("g c -> (g c)")[cp * 128:(cp + 1) * 128]), w=["pcol"])
                S.dma("sp", pcol[:, 1:2], col(dr["pool_scale"][l, cp * 128:(cp + 1) * 128]), w=["pcol"])
                S.dma("sp", prc, dr["poolrc"][cp], w=["prc"])
                for tc in range(8):
                    (pb, pbi), _ = pbank.next()
                    for k in range(8):
                        S.op("pe", lambda e, pb=pb, k=k, tc=tc, w_=w_: e.matmul(pb[:, :], w_[:, k, :], hT[:, k, tc * 512:(tc + 1) * 512], start=(k == 0), stop=(k == 7)),
                             r=[wk] + hTk([k], range(tc * 4, tc * 4 + 4)), w=[("ps", pbi)])
                    S.op("act", lambda e, pb=pb, tc=tc: e.copy(ub[:, 16 + tc * 512:16 + (tc + 1) * 512], pb[:, :]), r=[("ps", pbi)], w=["pub"])
                R0, R1 = 8, LE - 8
                n = R1 - R0
                S.op("dve", lambda e: e.tensor_tensor(wa[:, R0:R1], ub[:, R0 - 1:R1 - 1], ub[:, R0:R1], ALU.add), r=["pub"], w=["pwa"])
                S.op("dve", lambda e: e.tensor_tensor(wb[:, R0:R1], wa[:, R0 - 1:R1 - 1], wa[:, R0 + 1:R1 + 1], ALU.add), r=["pwa"], w=["pwb"])
                if cp == 0:
                    srcs = (wa, "pwa"), (wb, "pwb")
                else:
                    S.op("dve", lambda e: e.tensor_tensor(wa[:, R0:R1], wb[:, R0 - 2:R1 - 2], wb[:, R0 + 2:R1 + 2], ALU.add), r=["pwb"], w=["pwa"])
                    S.op("dve", lambda e: e.tensor_tensor(wb[:, R0:R1], wa[:, R0 - 4:R1 - 4], wa[:, R0 + 4:R1 + 4], ALU.add), r=["pwa"], w=["pwb"])
                    srcs = (wa, "pwa"), (wb, "pwb")
                for half in range(2):
                    rows = slice(half * 64, (half + 1) * 64)
                    src, srck = srcs[half]
                    wsz = wins[2 * cp + half]
                    S.op("dve", lambda e, rows=rows, src=src, wsz=wsz: e.scalar_tensor_tensor(yb[rows, :], src[rows, 16:16 + T], 1.0 / wsz, ub[rows, 16:16 + T], ALU.mult, ALU.subtract),
                         r=[srck, "pub"], w=["yb"])
                    for (e0, t0) in ((0, 0), (8, T - 8)):
                        S.op("dve", lambda e, rows=rows, src=src, e0=e0, t0=t0: e.tensor_tensor(etmp[rows, e0:e0 + 8], src[rows, 16 + t0:16 + t0 + 8], prc[rows, e0:e0 + 8], ALU.mult),
                             r=[srck, "prc"], w=["etmp"])
                        S.op("dve", lambda e, rows=rows, e0=e0, t0=t0: e.tensor_tensor(yb[rows, t0:t0 + 8], etmp[rows, e0:e0 + 8], ub[rows, 16 + t0:16 + t0 + 8], ALU.subtract),
                             r=["etmp", "pub", "yb"], w=["yb"])
                for tc in range(8):
                    (ob, obi), _ = obank.next()
                    S.op("pe", lambda e, ob=ob, tc=tc: e.matmul(ob[:, :], bd, yb[:, tc * 512:(tc + 1) * 512], start=True, stop=True), r=["bd", "yb"], w=[("ps", obi)])
                    S.op("dve", lambda e, ob=ob, tc=tc: e.tensor_scalar(mst[:, tc * 512:(tc + 1) * 512], ob[:, :], pcol[:, 0:1], pcol[:, 1:2], ALU.add, ALU.mult),
                         r=[("ps", obi), "pcol"], w=["mstC"])
                S.dma("sp", MIXT[6 + cp], mst, r=["mstC"], w=[("MIXT", 6 + cp)])
            A.pop()
            S.barrier()
        if dbg_stop(l, "C"):
            break

        A.push()
        wo = A.alloc([128, 8, D], BF16)
        for hh in range(2):
            S.dma("pool", wo[:, :, hh * 512:(hh + 1) * 512], dr["w_out"][l][:, hh * 512:(hh + 1) * 512].rearrange("(k p) n -> p k n", p=128), w=["wo"])
        g_rep = A.alloc([128, D], F32)
        b_rep = A.alloc([128, D], F32)
        S.dma("sp", g_rep, dr["ln1_g"][l].partition_broadcast(128), w=["gb1"])
        S.dma("sp", b_rep, dr["ln1_b"][l].partition_broadcast(128), w=["gb1"])
        wg_f = A.alloc([128, 8, 20], F32)
        S.dma("sp", wg_f[:, :, 0:4], dr["moe_wgc"][l].rearrange("(k p) n -> p k n", p=128), w=["wg_f"])
        S.dma("sp", wg_f[:, :, 4:20], dr["moe_wgf"][l].rearrange("(k p) n -> p k n", p=128), w=["wg_f"])
        bg_rep = A.alloc([128, 20], F32)
        S.dma("sp", bg_rep[:, 0:4], dr["moe_bgc"][l].partition_broadcast(128), w=["bg_rep"])
        S.dma("sp", bg_rep[:, 4:20], dr["moe_bgf"][l].partition_broadcast(128), w=["bg_rep"])
        logits = A.alloc([128, NT, 20], F32)
        lnb = make_ln_bufs()
        gating = dict(hTf=Rot("hTf", [A.alloc([128, 8, 128], F32) for _ in range(2)]), bank=(PS[4], 4), wg=wg_f, bg=bg_rep, logits=logits, cur=None)
        import os as _os
        DBG_D = _os.environ.get("DBG_D", "").split(",")
        if "nogate" in DBG_D:
            gating = None
        mt = Rot("mt", [A.alloc([128, 8, 128], BF16) for _ in range(2)])
        ht = Rot("ht", [A.alloc([128, D], F32) for _ in range(2)])
        rt = Rot("rt", [A.alloc([128, D], F32) for _ in range(2)])
        obank = Rot("ps", [(PS[0], 0), (PS[1], 1), (PS[2], 2), (PS[3], 3)])
        MIXr = MIXT.rearrange("c p t -> p c t")
        for i in range(NT):
            m_, mk = mt.next()
            h_, hk = ht.next()
            r_, rk = rt.next()
            if "nomixdma" not in DBG_D:
                S.dma("sp", m_, MIXr[:, :, i * 128:(i + 1) * 128], r=[("MIXT", c) for c in range(8)], w=[mk])
            S.dma("act", h_, H32[i * 128:(i + 1) * 128, :], r=[("H32", i)], w=[hk])
            for half in range(2):
                (ob, obi), _ = obank.next()
                if "nomm" in DBG_D:
                    S.op("dve", lambda e, h_=h_, r_=r_, half=half: e.tensor_scalar(r_[:, half * 512:(half + 1) * 512], h_[:, half * 512:(half + 1) * 512], ALPHA, None, ALU.mult), r=[hk], w=[rk])
                    continue
                for k in range(8):
                    S.op("pe", lambda e, ob=ob, m_=m_, k=k, half=half: e.matmul(ob[:, :], m_[:, k, :], wo[:, k, half * 512:(half + 1) * 512], start=(k == 0), stop=(k == 7)),
                         r=[mk, "wo"], w=[("ps", obi)])
                if half == 0:
                    S.op("act", lambda e, h_=h_: e.mul(h_, h_, ALPHA), r=[hk], w=[hk])
                S.op("dve", lambda e, ob=ob, h_=h_, r_=r_, half=half: e.tensor_tensor(r_[:, half * 512:(half + 1) * 512], ob[:, :], h_[:, half * 512:(half + 1) * 512], ALU.add),
                     r=[("ps", obi), hk], w=[rk])
            ln_tile(lnb, r_, [rk], i, g_rep, b_rep, "gb1", H32, "H32", gating=gating)
        if "nogate" in DBG_D or "noroute" in DBG_D:
            A.pop()
            S.barrier()
            break
        rA = A.alloc([128, NT, 16], F32)
        rB = A.alloc([128, NT, 16], F32)
        rC = A.alloc([128, NT, 16], F32)
        r1 = A.alloc([128, NT, 8], F32)
        lkeys = [("logits", i) for i in range(NT)]
        lc = logits[:, :, 0:4]
        lf = logits[:, :, 4:20]
        BIG = 1.0e30
        mc = r1[:, :, 0]
        S.op("dve", lambda e: e.tensor_reduce(mc, lc, AX.X, ALU.max), r=lkeys, w=["r1"])
        ec = rA[:, :, 0:4]
        S.op("dve", lambda e: e.tensor_tensor(ec, lc, mc.unsqueeze(2).broadcast_to([128, NT, 4]), ALU.subtract), r=lkeys + ["r1"], w=["rA"])
        S.op("act", lambda e: e.activation(ec, ec, AF.Exp), r=["rA"], w=["rA"])
        S.op("dve", lambda e: e.tensor_reduce(r1[:, :, 1], ec, AX.X, ALU.add), r=["rA"], w=["r1"])
        S.op("dve", lambda e: e.reciprocal(r1[:, :, 2], r1[:, :, 1]), r=["r1"], w=["r1"])
        oh = rA[:, :, 4:8]
        S.op("dve", lambda e: e.tensor_tensor(oh, lc, mc.unsqueeze(2).broadcast_to([128, NT, 4]), ALU.is_equal), r=lkeys + ["r1", "rA"], w=["rA"])
        pen = rA[:, :, 8:12]
        S.op("dve", lambda e: e.tensor_scalar(pen, oh, 1.0, BIG, ALU.subtract, ALU.mult), r=["rA"], w=["rA"])
        lfm = rB
        S.op("dve", lambda e: e.tensor_tensor(lfm.rearrange("p t (g j) -> p t g j", g=4), lf.rearrange("p t (g j) -> p t g j", g=4),
                                              pen.unsqueeze(3).broadcast_to([128, NT, 4, 4]), ALU.add), r=lkeys + ["rA"], w=["rB"])
        S.op("dve", lambda e: e.tensor_reduce(r1[:, :, 3], lfm, AX.X, ALU.max), r=["rB"], w=["r1"])
        eq1 = rC
        S.op("dve", lambda e: e.tensor_tensor(eq1, lfm, r1[:, :, 3].unsqueeze(2).broadcast_to([128, NT, 16]), ALU.is_equal), r=["rB", "r1"], w=["rC"])
        S.op("dve", lambda e: e.scalar_tensor_tensor(lfm, eq1, -BIG, lfm, ALU.mult, ALU.add), r=["rB", "rC"], w=["rB"])
        S.op("dve", lambda e: e.tensor_reduce(r1[:, :, 4], lfm, AX.X, ALU.max), r=["rB"], w=["r1"])
        eq2 = rA
        S.op("dve", lambda e: e.tensor_tensor(eq2, lfm, r1[:, :, 4].unsqueeze(2).broadcast_to([128, NT, 16]), ALU.is_equal), r=["rB", "r1", "rA"], w=["rA"])
        S.op("dve", lambda e: e.tensor_tensor(r1[:, :, 5], r1[:, :, 4], r1[:, :, 3], ALU.subtract), r=["r1"], w=["r1"])
        S.op("act", lambda e: e.activation(r1[:, :, 5], r1[:, :, 5], AF.Exp), r=["r1"], w=["r1"])
        S.op("dve", lambda e: e.tensor_scalar(r1[:, :, 6], r1[:, :, 5], 1.0, None, ALU.add), r=["r1"], w=["r1"])
        S.op("dve", lambda e: e.reciprocal(r1[:, :, 6], r1[:, :, 6]), r=["r1"], w=["r1"])
        S.op("dve", lambda e: e.tensor_tensor(r1[:, :, 7], r1[:, :, 5], r1[:, :, 6], ALU.mult), r=["r1"], w=["r1"])
        S.op("dve", lambda e: e.tensor_tensor(r1[:, :, 6], r1[:, :, 6], r1[:, :, 2], ALU.mult), r=["r1"], w=["r1"])
        S.op("dve", lambda e: e.tensor_tensor(r1[:, :, 7], r1[:, :, 7], r1[:, :, 2], ALU.mult), r=["r1"], w=["r1"])
        S.op("dve", lambda e: e.tensor_tensor(eq1, eq1, r1[:, :, 6].unsqueeze(2).broadcast_to([128, NT, 16]), ALU.mult), r=["rC", "r1"], w=["rC"])
        S.op("dve", lambda e: e.tensor_tensor(eq2, eq2, r1[:, :, 7].unsqueeze(2).broadcast_to([128, NT, 16]), ALU.mult), r=["rA", "r1"], w=["rA"])
        S.op("dve", lambda e: e.tensor_tensor(gates, eq1, eq2, ALU.add), r=["rC", "rA"], w=["gates"])
        A.pop()
        S.barrier()
        if dbg_stop(l, "D"):
            break

        A.push()
        wgp = A.alloc([128, 8, D], BF16)
        wpp = A.alloc([128, 2, D], BF16)
        for hh in range(2):
            S.dma("pool", wgp[:, :, hh * 512:(hh + 1) * 512], dr["ple_wg"][l][:, hh * 512:(hh + 1) * 512].rearrange("(k p) n -> p k n", p=128), w=["wgp"])
        S.dma("pool", wpp, dr["ple_wp"][l].rearrange("(k p) n -> p k n", p=128), w=["wpp"])
        bgp = A.alloc([128, D], F32)
        S.dma("sp", bgp, dr["ple_bg"][l].partition_broadcast(128), w=["bgp"])
        g_rep = A.alloc([128, D], F32)
        b_rep = A.alloc([128, D], F32)
        S.dma("sp", g_rep, dr["ln2_g"][l].partition_broadcast(128), w=["gb2"])
        S.dma("sp", b_rep, dr["ln2_b"][l].partition_broadcast(128), w=["gb2"])
        lnb = make_ln_bufs()
        yacc = A.alloc([128, 8, D], F32)
        w13 = Rot("w13", [A.alloc([128, 2, 8, 256], BF16) for _ in range(2)])
        w2r = Rot("w2r", [A.alloc([128, 2, D], BF16) for _ in range(2)])
        actT = Rot("actT", [A.alloc([128, 2, 1024], BF16) for _ in range(2)])
        sa = Rot("sa", [A.alloc([128, 512], F32) for _ in range(3)])
        pt = Rot("pt", [A.alloc([128, 256], F32) for _ in range(2)])
        pT = Rot("pT", [A.alloc([128, 2, 128], BF16) for _ in range(2)])
        ht = Rot("htE", [A.alloc([128, D], F32) for _ in range(2)])
        sg = Rot("sg", [A.alloc([128, 512], F32) for _ in range(2)])
        b01 = Rot("ps", [(PS[0], 0), (PS[1], 1)])
        b23 = Rot("ps", [(PS[2], 2), (PS[3], 3)])
        b45 = Rot("ps", [(PS[4], 4), (PS[5], 5)])
        dstD = out if last else H32
        for tcb in range(4):
            tiles = range(tcb * 8, tcb * 8 + 8)
            for ii, i in enumerate(tiles):
                p_, pk = pt.next()
                S.dma("sp", p_, dr["p"][l, i * 128:(i + 1) * 128, :], w=[pk])
                h_, hk = ht.next()
                S.dma("act", h_, H32[i * 128:(i + 1) * 128, :], r=[("H32", i)], w=[hk])
                (tb, tbi), _ = b45.next()
                for kk in range(2):
                    S.op("pe", lambda e, tb=tb, p_=p_, kk=kk: e.transpose(tb[:, kk * 128:(kk + 1) * 128], p_[:, kk * 128:(kk + 1) * 128], ident), r=[pk, "ident"], w=[("ps", tbi)])
                pT_, pTk = pT.next()
                S.op("act", lambda e, pT_=pT_, tb=tb: e.copy(pT_, tb[:, 0:256].rearrange("p (a b) -> p a b", a=2)), r=[("ps", tbi)], w=[pTk])
                for half in range(2):
                    hs = slice(half * 512, (half + 1) * 512)
                    (pg, pgi), _ = b01.next()
                    (pp, ppi), _ = b23.next()
                    for k in range(8):
                        S.op("pe", lambda e, pg=pg, k=k, i=i, hs=hs: e.matmul(pg[:, :], hT[:, k, i * 128:(i + 1) * 128], wgp[:, k, hs], start=(k == 0), stop=(k == 7)),
                             r=["wgp"] + hTk([k], [i]), w=[("ps", pgi)])
                    for kk in range(2):
                        S.op("pe", lambda e, pp=pp, kk=kk, pT_=pT_, hs=hs: e.matmul(pp[:, :], pT_[:, kk, :], wpp[:, kk, hs], start=(kk == 0), stop=(kk == 1)),
                             r=["wpp", pTk], w=[("ps", ppi)])
                    s_, sk = sg.next()
                    S.op("dve", lambda e, s_=s_, pg=pg, hs=hs: e.tensor_tensor(s_, pg[:, :], bgp[:, hs], ALU.add), r=[("ps", pgi), "bgp"], w=[sk])
                    S.op("act", lambda e, s_=s_: e.activation(s_, s_, AF.Sigmoid), r=[sk], w=[sk])
                    S.op("dve", lambda e, s_=s_, pp=pp: e.tensor_tensor(s_, s_, pp[:, :], ALU.mult), r=[sk, ("ps", ppi)], w=[sk])
                    S.op("dve", lambda e, s_=s_, h_=h_, ii=ii, hs=hs: e.scalar_tensor_tensor(yacc[:, ii, hs], h_[:, hs], ALPHA, s_, ALU.mult, ALU.add),
                         r=[sk, hk], w=[("yacc", ii, half)])
            for ex in range(16):
                w13_, w13k = w13.next()
                w2_, w2k = w2r.next()
                S.dma("pool", w13_[:, 0], dr["moe_w1"][l, ex].rearrange("(k p) n -> p k n", p=128), w=[w13k + (0,)])
                S.dma("pool", w13_[:, 1], dr["moe_w3"][l, ex].rearrange("(k p) n -> p k n", p=128), w=[w13k + (1,)])
                S.dma("pool", w2_, dr["moe_w2"][l, ex].rearrange("(k p) n -> p k n", p=128), w=[w2k])
                aT, aTk = actT.next()
                for sub in range(2):
                    tsl = slice(tcb * 1024 + sub * 512, tcb * 1024 + (sub + 1) * 512)
                    tl = range(tcb * 8 + sub * 4, tcb * 8 + sub * 4 + 4)
                    for fc in range(2):
                        (pa, pai), _ = b01.next()
                        (pc, pci), _ = b23.next()
                        for k in range(8):
                            S.op("pe", lambda e, pa=pa, k=k, fc=fc, tsl=tsl, w13_=w13_: e.matmul(pa[:, :], w13_[:, 0, k, fc * 128:(fc + 1) * 128], hT[:, k, tsl], start=(k == 0), stop=(k == 7)),
                                 r=[w13k + (0,)] + hTk([k], tl), w=[("ps", pai)])
                        for k in range(8):
                            S.op("pe", lambda e, pc=pc, k=k, fc=fc, tsl=tsl, w13_=w13_: e.matmul(pc[:, :], w13_[:, 1, k, fc * 128:(fc + 1) * 128], hT[:, k, tsl], start=(k == 0), stop=(k == 7)),
                                 r=[w13k + (1,)] + hTk([k], tl), w=[("ps", pci)])
                        s_, sk = sa.next()
                        S.op("act", lambda e, s_=s_, pa=pa: e.activation(s_, pa[:, :], AF.Silu), r=[("ps", pai)], w=[sk])
                        S.op("dve", lambda e, s_=s_, pc=pc, aT=aT, fc=fc, sub=sub: e.tensor_tensor(aT[:, fc, sub * 512:(sub + 1) * 512], s_, pc[:, :], ALU.mult),
                             r=[sk, ("ps", pci)], w=[aTk + (sub,)])
                for ii, i in enumerate(tiles):
                    for half in range(2):
                        hs = slice(half * 512, (half + 1) * 512)
                        (py, pyi), _ = b45.next()
                        for fc in range(2):
                            S.op("pe", lambda e, py=py, aT=aT, fc=fc, ii=ii, hs=hs, w2_=w2_: e.matmul(py[:, :], aT[:, fc, ii * 128:(ii + 1) * 128], w2_[:, fc, hs], start=(fc == 0), stop=(fc == 1)),
                                 r=[aTk + (ii // 4,), w2k], w=[("ps", pyi)])
                        S.op("dve", lambda e, py=py, ii=ii, hs=hs, i=i, ex=ex: e.scalar_tensor_tensor(yacc[:, ii, hs], py[:, :], gates[:, i, ex:ex + 1], yacc[:, ii, hs], ALU.mult, ALU.add),
                             r=[("ps", pyi), ("yacc", ii, half)], w=[("yacc", ii, half)])
            for ii, i in enumerate(tiles):
                tok = ln_tile(lnb, yacc[:, ii, :], [("yacc", ii, 0), ("yacc", ii, 1)], i, g_rep, b_rep, "gb2", dstD, "OUT" if last else "H32")
                if last:
                    final_tokens.append(tok)
        A.pop()
        S.barrier()
        if dbg_stop(l, "E"):
            break

    for h in S.dma_hist:
        if h:
            final_tokens.append(h[-1])
    S.emit(final_tokens=final_tokens)
    es.close()
    return nc


_NC_CACHE = {}


def make_in_maps(inputs, nld=DEPTH):
    c = host_consts()
    x = np.ascontiguousarray(np.asarray(inputs["x"], dtype=np.float32))
    p = np.asarray(inputs["p"], dtype=np.float32)[:nld]
    shared = {}
    for n, sh in WEIGHT_SPECS:
        a = np.asarray(inputs[n], dtype=np.float32)
        if sh[0] == DEPTH and n not in ("ln0_g", "ln0_b"):
            a = a[:nld]
        shared[n] = np.ascontiguousarray(a)
    for n, _, _ in CONST_SPECS:
        shared[n] = c[n]
    in_maps = []
    for core in range(8):
        b = core % 4
        m = dict(shared)
        m["x"] = x[b]
        m["p"] = np.ascontiguousarray(p[:, b])
        in_maps.append(m)
    return in_maps


def kernel(**inputs):
    if "nc" not in _NC_CACHE:
        _NC_CACHE["nc"] = build()
    nc = _NC_CACHE["nc"]
    in_maps = make_in_maps(inputs)
    res = run_bass_kernel_spmd(nc, in_maps, core_ids=list(range(8)))
    outs = [np.asarray(res.results[b]["out"], dtype=np.float32) for b in range(4)]
    return np.stack(outs, 0)
```

```python
import math
import numpy as np
import ml_dtypes
from contextlib import ExitStack
import concourse.bass as bass
import concourse.mybir as mybir
from concourse.bass_utils import run_bass_kernel_spmd

F32 = mybir.dt.float32
BF16 = mybir.dt.bfloat16
AF = mybir.ActivationFunctionType
ALU = mybir.AluOpType
AX = mybir.AxisListType

ENGS = ("pe", "act", "dve", "pool", "sp")
NDS = 24
NDS_HW = 16

T = 4096
D = 1024
NT = 32
DEPTH = 4
ALPHA = (2 * DEPTH) ** 0.25
O_Q, O_K, O_V, O_P = 768, 1280, 1792, 2304
LN_EPS = 1e-5
NFC = 33
PI = math.pi


class _Rec:
    def __getattr__(self, name):
        def f(*a, **k):
            self.call = (name, a, k)
        return f


class Sched:
    def __init__(self, nc):
        self.nc = nc
        self.q = {e: [] for e in ENGS}
        self.last_w = {}
        self.readers = {}
        self.ndma = 0
        self.ndma_sw = 0
        self.dma_hist = [[] for _ in range(NDS)]
        self.pending = {}

    def _deps(self, eng, r, w):
        deps = set()
        for k in r:
            t = self.last_w.get(k)
            if t is not None:
                deps.add(t)
        for k in w:
            t = self.last_w.get(k)
            if t is not None:
                deps.add(t)
            for t in self.readers.get(k, ()):
                deps.add(t)
        p = self.pending.pop(eng, None)
        if p:
            deps |= p
        return deps

    def _commit(self, tok, r, w):
        for k in r:
            lst = self.readers.setdefault(k, [])
            if tok[0] == "c":
                for j in range(len(lst)):
                    if lst[j][0] == "c" and lst[j][1] == tok[1]:
                        lst[j] = tok
                        break
                else:
                    lst.append(tok)
            else:
                lst.append(tok)
        for k in w:
            self.last_w[k] = tok
            self.readers[k] = []

    def op(self, eng, fn, r=(), w=(), after=()):
        rec = _Rec()
        fn(rec)
        name_, a_, k_ = rec.call
        fn = lambda e, name_=name_, a_=a_, k_=k_: getattr(e, name_)(*a_, **k_)
        deps = self._deps(eng, r, w)
        deps.update(after)
        tok = ("c", eng, len(self.q[eng]))
        self.q[eng].append(dict(fn=fn, deps=deps, tok=tok, dma=None))
        self._commit(tok, r, w)
        return tok

    def dma(self, eng, out, in_, r=(), w=(), after=(), **kw):
        deps = self._deps(eng, r, w)
        deps.update(after)
        if eng == "pool":
            slot = NDS_HW + self.ndma_sw % (NDS - NDS_HW)
            self.ndma_sw += 1
        else:
            slot = self.ndma % NDS_HW
            self.ndma += 1
        hist = self.dma_hist[slot]
        if hist:
            deps.add(hist[-1])
        tok = ("d", slot, len(hist) + 1)
        hist.append(tok)
        fn = lambda e, out=out, in_=in_, kw=kw: e.dma_start(out=out, in_=in_, **kw)
        self.q[eng].append(dict(fn=fn, deps=deps, tok=tok, dma=slot))
        self._commit(tok, r, w)
        return tok

    def barrier(self):
        toks = set()
        for e in ENGS:
            for ins in reversed(self.q[e]):
                if ins["dma"] is None:
                    toks.add(ins["tok"])
                    break
        for h in self.dma_hist:
            if h:
                toks.add(h[-1])
        for e in ENGS:
            self.pending.setdefault(e, set()).update(toks)
        self.last_w = {}
        self.readers = {}

    def emit(self, final_wait_eng="sp", final_tokens=()):
        nc = self.nc
        needed = set()
        for e in ENGS:
            for ins in self.q[e]:
                for d in ins["deps"]:
                    if d[0] == "c" and not (e == "pe" and d[1] == "pe"):
                        needed.add(d)
        cnt = {}
        for e in ENGS:
            c = 0
            for ins in self.q[e]:
                if ins["tok"] in needed:
                    c += 1
                    cnt[ins["tok"]] = c
        with ExitStack() as es:
            csem = {e: es.enter_context(nc.semaphore("cs_" + e)) for e in ENGS}
            dsem = [es.enter_context(nc.semaphore("ds_%d" % i)) for i in range(NDS)]
            block = es.enter_context(nc.Block())

            def resolve(t):
                if t[0] == "c":
                    return ("c", t[1]), csem[t[1]], cnt[t]
                return ("d", t[1]), dsem[t[1]], 16 * t[2]

            def run(ename, eobj):
                waited = {}
                for ins in self.q[ename]:
                    need = {}
                    for d in ins["deps"]:
                        if d[0] == "c" and d[1] == ename and ename == "pe":
                            continue
                        key, sem, val = resolve(d)
                        if waited.get(key, 0) >= val:
                            continue
                        if need.get(key, (None, 0))[1] < val:
                            need[key] = (sem, val)
                    for key, (sem, val) in need.items():
                        eobj.wait_ge(sem, val)
                        waited[key] = val
                    i = ins["fn"](eobj)
                    if ins["dma"] is not None:
                        i.then_inc(dsem[ins["dma"]], 16)
                    elif ins["tok"] in needed:
                        i.then_inc(csem[ename], 1)
                if ename == final_wait_eng:
                    for t in final_tokens:
                        key, sem, val = resolve(t)
                        if waited.get(key, 0) < val:
                            eobj.wait_ge(sem, val)
                            waited[key] = val

            @block.tensor
            def _(e):
                run("pe", e)

            @block.scalar
            def _(e):
                run("act", e)

            @block.vector
            def _(e):
                run("dve", e)

            @block.gpsimd
            def _(e):
                run("pool", e)

            @block.sync
            def _(e):
                run("sp", e)


_CONST = {}


def host_consts():
    if _CONST:
        return _CONST
    c = {}
    c["ident"] = np.eye(128, dtype=np.float32)
    rot = np.zeros((128, 128), np.float32)
    for base in (0, 64):
        for d in range(8):
            rot[base + d + 8, base + d] = -1.0
            rot[base + d, base + d + 8] = 1.0
    c["rotm"] = rot
    pos = np.arange(T, dtype=np.float32)
    inv_freq = np.power(np.float32(500000.0), -np.arange(0, 16, 2, dtype=np.float32) / np.float32(16)).astype(np.float32)
    ang = (pos[:, None] * inv_freq[None, :]).astype(np.float32)
    ang = np.concatenate([ang, ang], axis=-1)
    ct = np.ones((128, T), np.float32)
    stb = np.zeros((128, T), np.float32)
    for base in (0, 64):
        ct[base:base + 16] = np.cos(ang).T
        stb[base:base + 16] = np.sin(ang).T
    c["cos_tab"] = ct
    c["sin_tab"] = stb
    idx = np.arange(NFC * 128, dtype=np.int64)
    prod = (idx[:, None] * idx[None, :]) % 8192
    valid = (idx[:, None] <= 4096) & (idx[None, :] <= 4096)
    angd = prod.astype(np.float64) * (2.0 * np.pi / 8192.0)
    tabs = []
    for fn in (np.cos, np.sin):
        m = np.where(valid, fn(angd), 0.0).astype(np.float32)
        m4 = m.reshape(NFC, 128, NFC, 128).transpose(2, 1, 0, 3)
        tabs.append(np.ascontiguousarray(m4).reshape(NFC, 128, NFC * 128))
    c["dft"] = np.stack(tabs, 0).astype(ml_dtypes.bfloat16)
    del prod, angd, valid
    t = np.linspace(0.0, 1.0, T, dtype=np.float32)[:, None]
    wpos = (np.float32(2.0 * math.pi) * np.arange(T, dtype=np.float32)[:, None] / np.float32(T)).astype(np.float32)
    bands = np.linspace(1e-4, 15, 16, dtype=np.float32)[None, :]
    z = np.concatenate([t, np.cos(wpos * bands), -np.sin(wpos * bands)], axis=-1).astype(np.float32)
    c["zT"] = np.ascontiguousarray(z.T)
    tt = np.linspace(0.0, 1.0, T, dtype=np.float32)
    c["negt"] = np.ascontiguousarray(-tt.reshape(NT, 128).T)
    max_decay = math.log(1e-2) / 0.3
    min_decay = math.log(1e-2) / 1.5
    c["absd"] = np.abs(np.linspace(min_decay, max_decay, 256, dtype=np.float32)).astype(np.float32)
    f = np.arange(NFC * 128)
    wf = np.where(f <= 4096, 2.0, 0.0)
    wf[0] = 1.0
    wf[4096] = 1.0
    c["wfT"] = np.ascontiguousarray((wf / 8192.0).astype(np.float32).reshape(NFC, 128).T)
    rc = np.zeros((2, 128, 16), np.float32)
    wins = (2, 4, 8, 16)
    for cp in range(2):
        for half in range(2):
            w = wins[2 * cp + half]
            for j in range(16):
                tpos = j if j < 8 else T - 16 + j
                lo = max(tpos - w // 2, 0)
                hi = min(tpos + w // 2 - 1, T - 1)
                rc[cp, half * 64:(half + 1) * 64, j] = 1.0 / float(hi - lo + 1)
    c["poolrc"] = rc
    _CONST.update(c)
    return _CONST


WEIGHT_SPECS = [
    ("ln0_g", [1024]), ("ln0_b", [1024]), ("w_in", [4, 1024, 2560]), ("hy_conv_w", [4, 3, 768]),
    ("hy_conv_b", [4, 768]), ("hy_fw1", [4, 33, 64]), ("hy_fb1", [4, 64]), ("hy_freq1", [4, 64]),
    ("hy_fw2", [4, 64, 64]), ("hy_fb2", [4, 64]), ("hy_freq2", [4, 64]), ("hy_fw3", [4, 64, 512]),
    ("hy_bias", [4, 256]), ("att_lq1", [4, 64]), ("att_lk1", [4, 64]), ("att_lq2", [4, 64]),
    ("att_lk2", [4, 64]), ("att_subln_g", [4, 128]), ("pool_w", [4, 4, 64, 64]), ("pool_b", [4, 4, 64]),
    ("pool_scale", [4, 256]), ("w_out", [4, 1024, 1024]), ("ln1_g", [4, 1024]), ("ln1_b", [4, 1024]),
    ("moe_wgc", [4, 1024, 4]), ("moe_bgc", [4, 4]), ("moe_wgf", [4, 1024, 16]), ("moe_bgf", [4, 16]),
    ("moe_w1", [4, 16, 1024, 256]), ("moe_w3", [4, 16, 1024, 256]), ("moe_w2", [4, 16, 256, 1024]),
    ("ple_wg", [4, 1024, 1024]), ("ple_bg", [4, 1024]), ("ple_wp", [4, 256, 1024]),
    ("ln2_g", [4, 1024]), ("ln2_b", [4, 1024]),
]
CONST_SPECS = [
    ("ident", [128, 128], F32), ("rotm", [128, 128], F32), ("cos_tab", [128, T], F32), ("sin_tab", [128, T], F32),
    ("dft", [2, NFC, 128, NFC * 128], BF16), ("zT", [33, T], F32), ("negt", [128, NT], F32), ("absd", [256], F32),
    ("wfT", [128, NFC], F32), ("poolrc", [2, 128, 16], F32),
]


class Arena:
    def __init__(self, ap, nbytes):
        self.ap = ap
        self.nbytes = nbytes
        self.top = 0
        self.marks = []

    def alloc(self, shape, dt):
        esz = 2 if dt == BF16 else 4
        n = 1
        for s in shape[1:]:
            n *= s
        nb = (n * esz + 31) // 32 * 32
        off = self.top
        self.top += nb
        assert self.top <= self.nbytes, ("arena overflow", self.top)
        a = self.ap[:, off // 4:(off + nb) // 4]
        if dt == BF16:
            a = a.bitcast(BF16)
        a = a[:, 0:n]
        names = " ".join("d%d" % i for i in range(len(shape) - 1))
        if len(shape) > 2:
            kw = {"d%d" % i: shape[i + 1] for i in range(len(shape) - 1)}
            a = a.rearrange("p (%s) -> p %s" % (names, names), **kw)
        if shape[0] < 128:
            a = a[0:shape[0]]
        return a

    def push(self):
        self.marks.append(self.top)

    def pop(self):
        self.top = self.marks.pop()


class Rot:
    def __init__(self, name, items):
        self.name = name
        self.items = items
        self.i = 0

    def next(self):
        j = self.i % len(self.items)
        self.i += 1
        return self.items[j], (self.name, j)


def col(ap1d):
    return ap1d.rearrange("(p o) -> p o", o=1)


def build(n_layers=DEPTH, stop_after=None, debug=False, skip=()):
    nc = bass.Bass("TRN2", target_bir_lowering=False)
    dr = {}
    dr["x"] = nc.dram_tensor("x", [T, D], F32, kind="ExternalInput").ap()
    NLD = n_layers if debug else DEPTH
    dr["p"] = nc.dram_tensor("p", [NLD, T, 256], F32, kind="ExternalInput").ap()
    for n, s in WEIGHT_SPECS:
        s = list(s)
        if s[0] == DEPTH and n not in ("ln0_g", "ln0_b"):
            s[0] = NLD
        dr[n] = nc.dram_tensor(n, s, F32, kind="ExternalInput").ap()
    for n, s, dt in CONST_SPECS:
        dr[n] = nc.dram_tensor(n, s, dt, kind="ExternalInput").ap()
    out = nc.dram_tensor("out", [T, D], F32, kind="ExternalOutput").ap()
    skind = dict(kind="ExternalOutput") if debug else {}
    H32 = nc.dram_tensor("H32", [T, D], F32, **skind).ap()
    MIXT = nc.dram_tensor("MIXT", [8, 128, T], BF16, **skind).ap()

    es = ExitStack()
    arena_t = es.enter_context(nc.sbuf_tensor("arena", [128, 204 * 256], F32))
    PS = [es.enter_context(nc.psum_tensor("ps%d" % i, [128, 512], F32)) for i in range(8)]
    S = Sched(nc)
    A = Arena(arena_t[:], 204 * 1024)
    final_tokens = []

    hT = A.alloc([128, 8, T], BF16)
    ident = A.alloc([128, 128], F32)
    ones_f = A.alloc([128, 128], F32)
    zeros_b = A.alloc([128, 512], BF16)
    gates = A.alloc([128, NT, 16], F32)
    S.dma("sp", ident, dr["ident"], w=["ident"])
    S.op("dve", lambda e: e.memset(ones_f, 1.0), w=["ones_f"])
    S.op("dve", lambda e: e.memset(zeros_b, 0.0), w=["zeros_b"])

    def hTk(ks, tiles):
        return [("hT", k, i) for k in ks for i in tiles]

    K8 = list(range(8))

    def load_w(dst, src, key, eng="pool"):
        return S.dma(eng, dst, src.rearrange("(k p) n -> p k n", p=128), w=[key])

    def make_ln_bufs():
        b = {}
        b["st"] = Rot("ln_st", [A.alloc([128, 2, 6], F32) for _ in range(2)])
        b["mv"] = Rot("ln_mv", [A.alloc([128, 2], F32) for _ in range(2)])
        b["rs"] = Rot("ln_rs", [A.alloc([128, 1], F32) for _ in range(2)])
        b["hn"] = Rot("ln_hn", [A.alloc([128, D], F32) for _ in range(2)])
        b["bank"] = Rot("ps", [(PS[6], 6), (PS[7], 7)])
        return b

    def ln_tile(bufs, rt, rt_keys, i, g_rep, b_rep, gb_key, dst, dst_key, gating=None):
        st, stk = bufs["st"].next()
        mv, mvk = bufs["mv"].next()
        rs, rsk = bufs["rs"].next()
        hn, hnk = bufs["hn"].next()
        for hh in range(2):
            S.op("dve", lambda e, hh=hh: e.bn_stats(st[:, hh, :], rt[:, hh * 512:(hh + 1) * 512]), r=list(rt_keys), w=[stk + (hh,)])
        S.op("dve", lambda e: e.bn_aggr(mv, st.rearrange("p a b -> p (a b)")), r=[stk + (0,), stk + (1,)], w=[mvk])
        S.op("dve", lambda e: e.tensor_scalar(rs, mv[:, 1:2], LN_EPS, None, ALU.add), r=[mvk], w=[rsk])
        S.op("act", lambda e: e.activation(rs, rs, AF.Sqrt), r=[rsk], w=[rsk])
        S.op("dve", lambda e: e.reciprocal(rs, rs), r=[rsk], w=[rsk])
        S.op("dve", lambda e: e.tensor_scalar(hn, rt, mv[:, 0:1], rs[:, 0:1], ALU.subtract, ALU.mult), r=list(rt_keys) + [mvk, rsk], w=[hnk])
        S.op("dve", lambda e: e.tensor_tensor(hn, hn, g_rep, ALU.mult), r=[hnk, gb_key], w=[hnk])
        S.op("dve", lambda e: e.tensor_tensor(hn, hn, b_rep, ALU.add), r=[hnk, gb_key], w=[hnk])
        tok = S.dma("sp", dst[i * 128:(i + 1) * 128, :], hn, r=[hnk], w=[(dst_key, i)])
        for half in range(2):
            (bank, bi), _ = bufs["bank"].next()
            for j in range(4):
                k = half * 4 + j
                S.op("pe", lambda e, bank=bank, j=j, k=k: e.transpose(bank[:, j * 128:(j + 1) * 128], hn[:, k * 128:(k + 1) * 128], ident),
                     r=[hnk, "ident"], w=[("ps", bi)])
            S.op("act", lambda e, bank=bank, half=half: e.copy(hT[:, half * 4:(half + 1) * 4, i * 128:(i + 1) * 128],
                                                                bank[:, 0:512].rearrange("p (a b) -> p a b", a=4)),
                 r=[("ps", bi)], w=hTk(range(half * 4, half * 4 + 4), [i]))
            if gating is not None:
                if half == 0:
                    gating["cur"] = gating["hTf"].next()
                hTf, hTfk = gating["cur"]
                S.op("act", lambda e, bank=bank, half=half, hTf=hTf: e.copy(hTf[:, half * 4:(half + 1) * 4, :],
                                                                                   bank[:, 0:512].rearrange("p (a b) -> p a b", a=4)),
                     r=[("ps", bi)], w=[hTfk + (half,)])
        if gating is not None:
            hTf, hTfk = gating["cur"]
            gb, gbi = gating["bank"]
            for k in range(8):
                S.op("pe", lambda e, k=k, hTf=hTf: e.matmul(gb[:, 0:20], hTf[:, k, :], gating["wg"][:, k, :], start=(k == 0), stop=(k == 7)),
                     r=[hTfk + (k // 4,), "wg_f"], w=[("ps", gbi)])
            S.op("dve", lambda e: e.tensor_tensor(gating["logits"][:, i, :], gb[:, 0:20], gating["bg"], ALU.add),
                 r=[("ps", gbi), "bg_rep"], w=[("logits", i)])
        return tok

    dbg_list = []

    def dbg_dump(name, ap, shape, dt, rkeys=()):
        if not debug:
            return
        t = nc.dram_tensor("dbg_" + name, shape, dt, kind="ExternalOutput").ap()
        S.dma("sp", t, ap, r=list(rkeys))

    def dbg_stop(layer, phase):
        return stop_after is not None and stop_after == (layer, phase)

    A.push()
    g_rep = A.alloc([128, D], F32)
    b_rep = A.alloc([128, D], F32)
    S.dma("sp", g_rep, dr["ln0_g"].partition_broadcast(128), w=["gb0"])
    S.dma("sp", b_rep, dr["ln0_b"].partition_broadcast(128), w=["gb0"])
    lnb = make_ln_bufs()
    xt = Rot("xt", [A.alloc([128, D], F32) for _ in range(2)])
    for i in range(NT):
        x_t, xk = xt.next()
        S.dma("act", x_t, dr["x"][i * 128:(i + 1) * 128, :], w=[xk])
        ln_tile(lnb, x_t, [xk], i, g_rep, b_rep, "gb0", H32, "H32")
    A.pop()
    S.barrier()

    done = False
    for l in range(n_layers):
        lam_init = 0.8 - 0.6 * math.exp(-0.3 * l)
        last = (l == DEPTH - 1)
        for _ph in ([0] if "A" not in skip else []):
            A.push()
            PA = A.top
            VK = A.alloc([128, NT, 768], BF16)
            A.alloc([128, 16], F32)
            A.push()
            zT = A.alloc([33, T], F32)
            S.dma("sp", zT, dr["zT"], w=["zT"])
            fw1 = A.alloc([33, 64], F32)
            fw2 = A.alloc([64, 64], F32)
            fw3 = A.alloc([64, 512], F32)
            S.dma("sp", fw1, dr["hy_fw1"][l], w=["fw1"])
            S.dma("sp", fw2, dr["hy_fw2"][l], w=["fw2"])
            S.dma("sp", fw3, dr["hy_fw3"][l], w=["fw3"])
            fcol = A.alloc([64, 8], F32)
            S.dma("sp", fcol[:, 0:1], col(dr["hy_fb1"][l]), w=["fcol"])
            S.dma("sp", fcol[:, 1:2], col(dr["hy_freq1"][l]), w=["fcol"])
            S.dma("sp", fcol[:, 2:3], col(dr["hy_fb2"][l]), w=["fcol"])
            S.dma("sp", fcol[:, 3:4], col(dr["hy_freq2"][l]), w=["fcol"])
            S.op("dve", lambda e: e.tensor_tensor(fcol[:, 4:5], fcol[:, 0:1], fcol[:, 1:2], ALU.mult), r=["fcol"], w=["fcol"])
            S.op("dve", lambda e: e.tensor_tensor(fcol[:, 5:6], fcol[:, 2:3], fcol[:, 3:4], ALU.mult), r=["fcol"], w=["fcol"])
            hdn1 = A.alloc([64, T], F32)
            hdn2 = A.alloc([64, T], F32)
            arg = Rot("arg", [A.alloc([64, 512], F32) for _ in range(2)])
            m1b = Rot("m1b", [A.alloc([64, 512], F32) for _ in range(2)])
            m2b = Rot("m2b", [A.alloc([64, 512], F32) for _ in range(2)])
            absd = A.alloc([128, 256], F32)
            S.dma("sp", absd, dr["absd"].partition_broadcast(128), w=["absd"])
            negt = A.alloc([128, NT], F32)
            S.dma("sp", negt, dr["negt"], w=["negt"])
            hb = A.alloc([1, 256], F32)
            S.dma("sp", hb, dr["hy_bias"][l].rearrange("(o c) -> o c", o=1), w=["hb"])

            def sin_layer(ps, pk, fq, bs, dst, dkey):
                a, ak = arg.next()
                m1, m1k = m1b.next()
                m2, m2k = m2b.next()
                S.op("dve", lambda e: e.tensor_scalar(a, ps, fcol[:, fq:fq + 1], fcol[:, bs:bs + 1], ALU.mult, ALU.add), r=[pk, "fcol"], w=[ak])
                S.op("dve", lambda e: e.tensor_scalar(m1, a, -PI, 2 * PI, ALU.is_lt, ALU.mult), r=[ak], w=[m1k])
                S.op("dve", lambda e: e.tensor_scalar(m2, a, PI, 2 * PI, ALU.is_gt, ALU.mult), r=[ak], w=[m2k])
                S.op("dve", lambda e: e.tensor_tensor(a, a, m1, ALU.add), r=[ak, m1k], w=[ak])
                S.op("dve", lambda e: e.tensor_tensor(a, a, m2, ALU.subtract), r=[ak, m2k], w=[ak])
                S.op("act", lambda e: e.activation(dst, a, AF.Sin), r=[ak], w=[dkey])

            for tc in range(8):
                sl = slice(tc * 512, (tc + 1) * 512)
                S.op("pe", lambda e, sl=sl: e.matmul(PS[0][0:64, :], fw1, zT[:, sl], start=True, stop=True), r=["fw1", "zT"], w=[("ps", 0)])
                sin_layer(PS[0][0:64, :], ("ps", 0), 1, 4, hdn1[:, sl], ("hdn1", tc))
                S.op("pe", lambda e, sl=sl: e.matmul(PS[1][0:64, :], fw2, hdn1[:, sl], start=True, stop=True), r=["fw2", ("hdn1", tc)], w=[("ps", 1)])
                sin_layer(PS[1][0:64, :], ("ps", 1), 3, 5, hdn2[:, sl], ("hdn2", tc))

            win = Rot("win", [A.alloc([128, 256], F32) for _ in range(2)])
            kw = Rot("kw", [A.alloc([128, 512], F32) for _ in range(2)])
            sq = Rot("sq", [A.alloc([128, 512], F32) for _ in range(2)])
            nrm = A.alloc([128, 256], F32)
            kbank = Rot("ps", [(PS[2], 2), (PS[3], 3)])

            def filt_tile(i):
                (pb, pbi), _ = kbank.next()
                S.op("pe", lambda e: e.matmul(pb[:, :], hdn2[:, i * 128:(i + 1) * 128], fw3, start=True, stop=True),
                     r=[("hdn2", i // 4), "fw3"], w=[("ps", pbi)])
                wn, wnk = win.next()
                S.op("act", lambda e: e.activation(wn, absd, AF.Exp, scale=negt[:, i:i + 1]), r=["absd", "negt"], w=[wnk])
                k_, kk = kw.next()
                for hh in range(2):
                    S.op("dve", lambda e, hh=hh: e.tensor_tensor(k_[:, hh * 256:(hh + 1) * 256], pb[:, hh * 256:(hh + 1) * 256], wn, ALU.mult),
                         r=[("ps", pbi), wnk], w=[kk])
                if i == 0:
                    S.op("dve", lambda e: e.memset(k_[0:1, 256:512], 0.0), w=[kk])
                return k_, kk

            for i in range(NT):
                k_, kk = filt_tile(i)
                s_, sk = sq.next()
                S.op("act", lambda e, k_=k_, s_=s_: e.activation(s_, k_, AF.Square), r=[kk], w=[sk])
                S.op("pe", lambda e, s_=s_, i=i: e.matmul(PS[4][:, :], ones_f, s_, start=(i == 0), stop=(i == NT - 1)), r=["ones_f", sk], w=[("ps", 4)])
            S.op("dve", lambda e: e.tensor_copy(nrm, PS[4][:, 0:256]), r=[("ps", 4)], w=["nrm"])
            S.op("dve", lambda e: e.tensor_tensor(nrm, nrm, PS[4][:, 256:512], ALU.add), r=[("ps", 4), "nrm"], w=["nrm"])
            S.op("dve", lambda e: e.tensor_scalar(nrm, nrm, 1e-6, None, ALU.add), r=["nrm"], w=["nrm"])
            S.op("act", lambda e: e.activation(nrm, nrm, AF.Sqrt), r=["nrm"], w=["nrm"])
            S.op("dve", lambda e: e.reciprocal(nrm, nrm), r=["nrm"], w=["nrm"])
            for i in range(NT):
                k_, kk = filt_tile(i)
                s_, sk = sq.next()
                S.op("dve", lambda e, k_=k_, s_=s_: e.tensor_tensor(s_[:, 0:256], k_[:, 0:256], k_[:, 256:512], ALU.add), r=[kk], w=[sk])
                S.op("dve", lambda e, k_=k_, s_=s_: e.tensor_tensor(s_[:, 256:512], k_[:, 256:512], k_[:, 0:256], ALU.subtract), r=[kk], w=[sk])
                for hh in range(2):
                    S.op("dve", lambda e, s_=s_, hh=hh: e.tensor_tensor(s_[:, hh * 256:(hh + 1) * 256], s_[:, hh * 256:(hh + 1) * 256], nrm, ALU.mult),
                         r=[sk, "nrm"], w=[sk])
                if i == 0:
                    S.op("dve", lambda e, s_=s_: e.tensor_tensor(s_[0:1, 0:256], s_[0:1, 0:256], hb, ALU.add), r=[sk, "hb"], w=[sk])
                    S.op("dve", lambda e, s_=s_: e.tensor_tensor(s_[0:1, 256:512], s_[0:1, 256:512], hb, ALU.subtract), r=[sk, "hb"], w=[sk])
                S.op("act", lambda e, s_=s_, i=i: e.copy(VK[:, i, 0:256], s_[:, 0:256]), r=[sk], w=[("VKk", i)])
                S.op("act", lambda e, s_=s_, i=i: e.copy(VK[:, i, 512:768], s_[:, 256:512]), r=[sk], w=[("VKk", i)])
            A.pop()
            S.barrier()
            A.push()
            ub = A.alloc([128, T + 2], F32)
            c1 = A.alloc([128, T], F32)
            vp = A.alloc([128, T], F32)
            wch = Rot("wch", [A.alloc([128, 8, 128], BF16) for _ in range(2)])
            cw = Rot("cw", [A.alloc([128, 4], F32) for _ in range(2)])
            S.op("dve", lambda e: e.memset(ub[:, 0:1], 0.0), w=["ub_h"])
            S.op("dve", lambda e: e.memset(ub[:, T + 1:T + 2], 0.0), w=["ub_h"])
            pbank = Rot("ps", [(PS[0], 0), (PS[1], 1)])
            tbank = Rot("ps", [(PS[2], 2), (PS[3], 3)])
            for cc in range(2):
                for sname, coff in (("x1", 256), ("v", 512)):
                    c0 = coff + cc * 128
                    w_, wk = wch.next()
                    load_w(w_, dr["w_in"][l][:, c0:c0 + 128], wk)
                    cw_, cwk = cw.next()
                    for j in range(3):
                        S.dma("sp", cw_[:, j:j + 1], col(dr["hy_conv_w"][l, j, c0:c0 + 128]), w=[cwk])
                    S.dma("sp", cw_[:, 3:4], col(dr["hy_conv_b"][l, c0:c0 + 128]), w=[cwk])
                    for tc in range(8):
                        (pb, pbi), _ = pbank.next()
                        for k in range(8):
                            S.op("pe", lambda e, pb=pb, k=k, tc=tc, w_=w_: e.matmul(pb[:, :], w_[:, k, :], hT[:, k, tc * 512:(tc + 1) * 512], start=(k == 0), stop=(k == 7)),
                                 r=[wk] + hTk([k], range(tc * 4, tc * 4 + 4)), w=[("ps", pbi)])
                        S.op("act", lambda e, pb=pb, tc=tc: e.copy(ub[:, 1 + tc * 512:1 + (tc + 1) * 512], pb[:, :]), r=[("ps", pbi)], w=[("ub", tc)])
                    ubk = [("ub", tc) for tc in range(8)] + ["ub_h"]
                    dst = {"x1": c1, "v": vp}[sname]
                    dk = {"x1": "c1", "v": "vp"}[sname]
                    S.op("dve", lambda e, dst=dst, cw_=cw_: e.tensor_scalar(dst, ub[:, 0:T], cw_[:, 0:1], cw_[:, 3:4], ALU.mult, ALU.add), r=ubk + [cwk], w=[dk])
                    S.op("dve", lambda e, dst=dst, cw_=cw_: e.scalar_tensor_tensor(dst, ub[:, 1:T + 1], cw_[:, 1:2], dst, ALU.mult, ALU.add), r=ubk + [cwk, dk], w=[dk])
                    S.op("dve", lambda e, dst=dst, cw_=cw_: e.scalar_tensor_tensor(dst, ub[:, 2:T + 2], cw_[:, 2:3], dst, ALU.mult, ALU.add), r=ubk + [cwk, dk], w=[dk])
                    if sname == "v":
                        S.op("dve", lambda e: e.tensor_tensor(vp, vp, c1, ALU.mult), r=["vp", "c1"], w=["vp"])
                        for i4 in range(8):
                            (tb, tbi), _ = tbank.next()
                            for j in range(4):
                                i = i4 * 4 + j
                                S.op("pe", lambda e, tb=tb, j=j, i=i: e.transpose(tb[:, j * 128:(j + 1) * 128], vp[:, i * 128:(i + 1) * 128], ident),
                                     r=["vp", "ident"], w=[("ps", tbi)])
                            S.op("act", lambda e, tb=tb, i4=i4, cc=cc: e.copy(VK[:, i4 * 4:(i4 + 1) * 4, 256 + cc * 128:256 + (cc + 1) * 128],
                                                                             tb[:, 0:512].rearrange("p (a b) -> p a b", a=4)),
                                 r=[("ps", tbi)], w=[("VKv", cc, i4)])
            S.barrier()
            if l == 0:
                pass
                pass
                pass
                dbg_dump("cw", cw_, [128, 4], F32)
                pass
                S.barrier()
            A.pop()
            if dbg_stop(l, "A2"):
                break
            A.push()
            Zc = A.alloc([128, NFC, 256], BF16)
            Zs = A.alloc([128, NFC, 256], BF16)
            tabC = Rot("tabC", [A.alloc([128, NFC * 128], BF16) for _ in range(2)])
            tabS = Rot("tabS", [A.alloc([128, NFC * 128], BF16) for _ in range(2)])
            wfT = A.alloc([128, NFC], F32)
            S.dma("sp", wfT, dr["wfT"], w=["wfT"])
            T3 = A.top
            csb = Rot("csb", [A.alloc([128, 512], F32) for _ in range(2)])
            ssb = Rot("ssb", [A.alloc([128, 512], F32) for _ in range(2)])
            tm = Rot("tm", [A.alloc([128, 4, 256], F32) for _ in range(2)])
            cbank = Rot("ps", [(PS[0], 0), (PS[1], 1)])
            sbank = Rot("ps", [(PS[2], 2), (PS[3], 3)])
            for j in range(NFC):
                tc_, tck = tabC.next()
                ts_, tsk = tabS.next()
                S.dma("sp", tc_[:, 0:NT * 128], dr["dft"][0, j][:, 0:NT * 128], w=[tck])
                S.dma("act", ts_[:, 0:NT * 128], dr["dft"][1, j][:, 0:NT * 128], w=[tsk])
                (pc, pci), _ = cbank.next()
                (psn, psi), _ = sbank.next()
                for i in range(NT):
                    S.op("pe", lambda e, pc=pc, tc_=tc_, i=i: e.matmul(pc[:, :], tc_[:, i * 128:(i + 1) * 128], VK[:, i, 0:512], start=(i == 0), stop=(i == NT - 1)),
                         r=[tck], w=[("ps", pci)])
                for i in range(NT):
                    S.op("pe", lambda e, psn=psn, ts_=ts_, i=i: e.matmul(psn[:, :], ts_[:, i * 128:(i + 1) * 128], VK[:, i, 256:768], start=(i == 0), stop=(i == NT - 1)),
                         r=[tsk], w=[("ps", psi)])
                c_, ck = csb.next()
                s_, sk = ssb.next()
                t_, tk = tm.next()
                S.op("dve", lambda e, c_=c_, pc=pc, j=j: e.tensor_scalar(c_[:, 0:256], pc[:, 0:256], wfT[:, j:j + 1], None, ALU.mult), r=[("ps", pci), "wfT"], w=[ck])
                S.op("act", lambda e, c_=c_, pc=pc: e.copy(c_[:, 256:512], pc[:, 256:512]), r=[("ps", pci)], w=[ck])
                S.op("act", lambda e, s_=s_, psn=psn: e.copy(s_[:, 0:256], psn[:, 0:256]), r=[("ps", psi)], w=[sk])
                S.op("dve", lambda e, s_=s_, psn=psn, j=j: e.tensor_scalar(s_[:, 256:512], psn[:, 256:512], wfT[:, j:j + 1], None, ALU.mult), r=[("ps", psi), "wfT"], w=[sk])
                S.op("dve", lambda e, t_=t_, c_=c_: e.tensor_tensor(t_[:, 0, :], c_[:, 256:512], c_[:, 0:256], ALU.mult), r=[ck], w=[tk + (0,)])
                S.op("dve", lambda e, t_=t_, s_=s_: e.tensor_tensor(t_[:, 1, :], s_[:, 0:256], s_[:, 256:512], ALU.mult), r=[sk], w=[tk + (1,)])
                S.op("dve", lambda e, t_=t_, j=j: e.tensor_tensor(Zc[:, j, :], t_[:, 0, :], t_[:, 1, :], ALU.add), r=[tk + (0,), tk + (1,)], w=[("Z", j)])
                S.op("dve", lambda e, t_=t_, s_=s_, c_=c_: e.tensor_tensor(t_[:, 2, :], s_[:, 0:256], c_[:, 0:256], ALU.mult), r=[sk, ck], w=[tk + (2,)])
                S.op("dve", lambda e, t_=t_, s_=s_, c_=c_: e.tensor_tensor(t_[:, 3, :], c_[:, 256:512], s_[:, 256:512], ALU.mult), r=[sk, ck], w=[tk + (3,)])
                S.op("dve", lambda e, t_=t_, j=j: e.tensor_tensor(Zs[:, j, :], t_[:, 2, :], t_[:, 3, :], ALU.subtract), r=[tk + (2,), tk + (3,)], w=[("Z", j)])
            S.barrier()
            if l == 0:
                pass
                pass
                S.barrier()
            A.top = T3
            mst = A.alloc([128, 2, T], BF16)
            ysb = Rot("ysb", [A.alloc([128, 256], F32) for _ in range(2)])
            w_ = A.alloc([128, 8, 128], BF16)
            cw_ = A.alloc([128, 4], F32)
            TOPR = A.top
            A.top = PA
            ub = A.alloc([128, T + 2], F32)
            c1 = A.alloc([128, T], F32)
            x0c = A.alloc([128, 2, T], BF16)
            assert A.top <= T3 and A.top <= PA + 49152 + 64
            A.top = TOPR
            S.op("dve", lambda e: e.memset(ub[:, 0:1], 0.0), w=["ub_h"])
            S.op("dve", lambda e: e.memset(ub[:, T + 1:T + 2], 0.0), w=["ub_h"])
            pbank = Rot("ps", [(PS[0], 0), (PS[1], 1)])
            for cc in range(2):
                c0 = cc * 128
                load_w(w_, dr["w_in"][l][:, c0:c0 + 128], "wchx")
                for j in range(3):
                    S.dma("sp", cw_[:, j:j + 1], col(dr["hy_conv_w"][l, j, c0:c0 + 128]), w=["cwx"])
                S.dma("sp", cw_[:, 3:4], col(dr["hy_conv_b"][l, c0:c0 + 128]), w=["cwx"])
                for tc in range(8):
                    (pb, pbi), _ = pbank.next()
                    for k in range(8):
                        S.op("pe", lambda e, pb=pb, k=k, tc=tc: e.matmul(pb[:, :], w_[:, k, :], hT[:, k, tc * 512:(tc + 1) * 512], start=(k == 0), stop=(k == 7)),
                             r=["wchx"] + hTk([k], range(tc * 4, tc * 4 + 4)), w=[("ps", pbi)])
                    S.op("act", lambda e, pb=pb, tc=tc: e.copy(ub[:, 1 + tc * 512:1 + (tc + 1) * 512], pb[:, :]), r=[("ps", pbi)], w=[("ub", tc)])
                ubk = [("ub", tc) for tc in range(8)] + ["ub_h"]
                S.op("dve", lambda e: e.tensor_scalar(c1, ub[:, 0:T], cw_[:, 0:1], cw_[:, 3:4], ALU.mult, ALU.add), r=ubk + ["cwx"], w=["c1"])
                S.op("dve", lambda e: e.scalar_tensor_tensor(c1, ub[:, 1:T + 1], cw_[:, 1:2], c1, ALU.mult, ALU.add), r=ubk + ["cwx", "c1"], w=["c1"])
                S.op("dve", lambda e, cc=cc: e.scalar_tensor_tensor(x0c[:, cc, :], ub[:, 2:T + 2], cw_[:, 2:3], c1, ALU.mult, ALU.add), r=ubk + ["cwx", "c1"], w=[("x0c", cc)])
            ybank = Rot("ps", [(PS[4], 4), (PS[5], 5)])
            t2bank = Rot("ps", [(PS[6], 6), (PS[7], 7)])
            Zkeys = [("Z", j) for j in range(NFC)]
            for i in range(NT):
                tc_, tck = tabC.next()
                ts_, tsk = tabS.next()
                S.dma("sp", tc_, dr["dft"][0, i], w=[tck])
                S.dma("act", ts_, dr["dft"][1, i], w=[tsk])
                (py, pyi), _ = ybank.next()
                for jj in range(NFC):
                    S.op("pe", lambda e, py=py, tc_=tc_, jj=jj: e.matmul(py[:, 0:256], tc_[:, jj * 128:(jj + 1) * 128], Zc[:, jj, :], start=(jj == 0), stop=False),
                         r=[tck] + (Zkeys if jj == 0 else []), w=[("ps", pyi)])
                    S.op("pe", lambda e, py=py, ts_=ts_, jj=jj: e.matmul(py[:, 0:256], ts_[:, jj * 128:(jj + 1) * 128], Zs[:, jj, :], start=False, stop=(jj == NFC - 1)),
                         r=[tsk], w=[("ps", pyi)])
                y_, yk = ysb.next()
                S.op("act", lambda e, y_=y_, py=py: e.copy(y_, py[:, 0:256]), r=[("ps", pyi)], w=[yk])
                (tb, tbi), _ = t2bank.next()
                for cc in range(2):
                    S.op("pe", lambda e, tb=tb, y_=y_, cc=cc: e.transpose(tb[:, cc * 128:(cc + 1) * 128], y_[:, cc * 128:(cc + 1) * 128], ident),
                         r=[yk, "ident"], w=[("ps", tbi)])
                S.op("dve", lambda e, tb=tb, i=i: e.tensor_tensor(mst[:, :, i * 128:(i + 1) * 128], tb[:, 0:256].rearrange("p (a b) -> p a b", a=2),
                                                                  x0c[:, :, i * 128:(i + 1) * 128], ALU.mult),
                     r=[("ps", tbi), ("x0c", 0), ("x0c", 1)], w=[("mst", i)])
            for cc in range(2):
                S.dma("sp", MIXT[cc], mst[:, cc, :], r=[("mst", i) for i in range(NT)], w=[("MIXT", cc)])
            A.pop()
            A.pop()
            S.barrier()
        if dbg_stop(l, "A"):
            break

        for _ph in ([0] if "B" not in skip else []):
            A.push()
            cos_t = A.alloc([128, T], F32)
            sin_t = A.alloc([128, T], F32)
            S.dma("sp", cos_t, dr["cos_tab"], w=["cos_t"])
            S.dma("act", sin_t, dr["sin_tab"], w=["sin_t"])
            rotm = A.alloc([128, 128], F32)
            S.dma("sp", rotm, dr["rotm"], w=["rotm"])
            Va = A.alloc([128, NT, 4, 130], BF16)
            qa = A.alloc([128, T], BF16)
            qb = A.alloc([128, T], BF16)
            kT = A.alloc([128, T], BF16)
            S.op("dve", lambda e: e.memset(qa[64:128, :], 0.0), w=["qz"])
            S.op("dve", lambda e: e.memset(qb[0:64, :], 0.0), w=["qz"])
            wv = A.alloc([128, 8, 512], BF16)
            wqk = Rot("wqk", [A.alloc([128, 8, 128], BF16) for _ in range(2)])
            tmpf = Rot("tmpf", [A.alloc([128, 512], F32) for _ in range(2)])
            r1t = Rot("r1t", [A.alloc([128, 512], F32) for _ in range(2)])
            r2t = Rot("r2t", [A.alloc([128, 512], F32) for _ in range(2)])
            Pb = Rot("Pb", [A.alloc([128, 512], BF16) for _ in range(4)])
            mst = A.alloc([128, T], BF16)
            osb = Rot("osb", [A.alloc([128, 128], F32) for _ in range(2)])
            t2b = Rot("t2b", [A.alloc([128, 128], F32) for _ in range(2)])
            junk = A.alloc([128, 128], F32)
            sm = Rot("sm", [A.alloc([128, 8], F32) for _ in range(2)])
            gsub = A.alloc([128, 128], F32)
            lqk = A.alloc([128, 4, 64], F32)
            lam_t = A.alloc([128, 8], F32)
            for j, nm in enumerate(("att_lq1", "att_lk1", "att_lq2", "att_lk2")):
                S.dma("sp", lqk[:, j, :], dr[nm][l].partition_broadcast(128), w=["lqk"])
            S.op("dve", lambda e: e.tensor_tensor(lqk[:, 0, :], lqk[:, 0, :], lqk[:, 1, :], ALU.mult), r=["lqk"], w=["lqk"])
            S.op("dve", lambda e: e.tensor_tensor(lqk[:, 2, :], lqk[:, 2, :], lqk[:, 3, :], ALU.mult), r=["lqk"], w=["lqk"])
            S.op("dve", lambda e: e.tensor_reduce(lam_t[:, 0:1], lqk[:, 0, :], AX.X, ALU.add), r=["lqk"], w=["lam"])
            S.op("dve", lambda e: e.tensor_reduce(lam_t[:, 1:2], lqk[:, 2, :], AX.X, ALU.add), r=["lqk"], w=["lam"])
            S.op("act", lambda e: e.activation(lam_t[:, 2:4], lam_t[:, 0:2], AF.Exp), r=["lam"], w=["lam"])
            S.op("dve", lambda e: e.tensor_tensor(lam_t[:, 4:5], lam_t[:, 3:4], lam_t[:, 2:3], ALU.subtract), r=["lam"], w=["lam"])
            S.op("dve", lambda e: e.tensor_scalar(lam_t[:, 5:6], lam_t[:, 4:5], -lam_init, None, ALU.add), r=["lam"], w=["lam"])
            neglam = lam_t[:, 5:6]
            S.dma("sp", gsub, dr["att_subln_g"][l].partition_broadcast(128), w=["gsub"])
            S.op("dve", lambda e: e.tensor_scalar(gsub, gsub, 1.0 - lam_init, None, ALU.mult), r=["gsub"], w=["gsub"])
            S.op("dve", lambda e: e.memset(Va[:, :, :, 128:130], 1.0), w=["Va1"])
            S.dma("pool", wv, dr["w_in"][l][:, O_V:O_V + 512].rearrange("(k p) n -> p k n", p=128), w=["wv"])
            vbank = Rot("ps", [(PS[6], 6), (PS[7], 7)])
            for i in range(NT):
                (pb, pbi), _ = vbank.next()
                for k in range(8):
                    S.op("pe", lambda e, pb=pb, k=k, i=i: e.matmul(pb[:, :], hT[:, k, i * 128:(i + 1) * 128], wv[:, k, :], start=(k == 0), stop=(k == 7)),
                         r=["wv"] + hTk([k], [i]), w=[("ps", pbi)])
                S.op("act", lambda e, pb=pb, i=i: e.copy(Va[:, i, :, 0:128], pb[:, :].rearrange("p (a b) -> p a b", a=4)), r=[("ps", pbi)], w=[("Va", i)])
            Vkeys = [("Va", i) for i in range(NT)] + ["Va1"]
            accb = [(PS[2], 2), (PS[3], 3), (PS[4], 4), (PS[5], 5)]
            sbank = Rot("ps", [(PS[0], 0), (PS[1], 1), (PS[6], 6)])
            pjbank = Rot("ps", [(PS[2], 2), (PS[3], 3), (PS[4], 4), (PS[5], 5)])
            tbank7 = Rot("ps", [(PS[7], 7)])
            for hd in range(4):
                for which, dstT, dkey, coff in (("q", None, "qT", O_Q), ("k", kT, "kT", O_K)):
                    w_, wk = wqk.next()
                    load_w(w_, dr["w_in"][l][:, coff + hd * 128:coff + (hd + 1) * 128], wk)
                    for tc in range(8):
                        sl = slice(tc * 512, (tc + 1) * 512)
                        (pb, pbi), _ = pjbank.next()
                        for k in range(8):
                            S.op("pe", lambda e, pb=pb, k=k, sl=sl, w_=w_: e.matmul(pb[:, :], w_[:, k, :], hT[:, k, sl], start=(k == 0), stop=(k == 7)),
                                 r=[wk] + hTk([k], range(tc * 4, tc * 4 + 4)), w=[("ps", pbi)])
                        tf, tfk = tmpf.next()
                        S.op("act", lambda e, tf=tf, pb=pb: e.copy(tf, pb[:, :]), r=[("ps", pbi)], w=[tfk])
                        if which == "k":
                            S.op("act", lambda e, pb=pb, dstT=dstT, sl=sl: e.copy(dstT[:, sl], pb[:, :]), r=[("ps", pbi)], w=[(dkey, tc)])
                        else:
                            S.op("act", lambda e, pb=pb, sl=sl: e.copy(qa[0:64, sl], pb[0:64, :]), r=[("ps", pbi), "qz"], w=[(dkey, tc)])
                            S.op("act", lambda e, pb=pb, sl=sl: e.copy(qb[64:128, sl], pb[64:128, :]), r=[("ps", pbi), "qz"], w=[(dkey, tc)])
                        (pr, pri), _ = pjbank.next()
                        S.op("pe", lambda e, pr=pr, tf=tf: e.matmul(pr[:, :], rotm, tf, start=True, stop=True), r=["rotm", tfk], w=[("ps", pri)])
                        a1, a1k = r1t.next()
                        a2, a2k = r2t.next()
                        for base in (0, 64):
                            ps_ = slice(base, base + 16)
                            S.op("dve", lambda e, a1=a1, tf=tf, ps_=ps_, sl=sl: e.tensor_tensor(a1[ps_, :], tf[ps_, :], cos_t[ps_, sl], ALU.mult), r=[tfk, "cos_t"], w=[a1k])
                            S.op("dve", lambda e, a2=a2, pr=pr, ps_=ps_, sl=sl: e.tensor_tensor(a2[ps_, :], pr[ps_, :], sin_t[ps_, sl], ALU.mult), r=[("ps", pri), "sin_t"], w=[a2k])
                            dT = dstT if which == "k" else (qa if base == 0 else qb)
                            S.op("dve", lambda e, a1=a1, a2=a2, ps_=ps_, sl=sl, dT=dT: e.tensor_tensor(dT[ps_, sl], a1[ps_, :], a2[ps_, :], ALU.add), r=[a1k, a2k], w=[(dkey, tc)])
                kkeys = [("kT", tc) for tc in range(8)]
                for qc in range(8):
                    qsl = slice(qc * 512, (qc + 1) * 512)
                    for (ab, abi) in accb:
                        S.op("pe", lambda e, ab=ab: e.matmul(ab[:, :], zeros_b[:, 0:128], zeros_b[:, :], start=True, stop=False, skip_group_check=True),
                             r=["zeros_b"], w=[("ps", abi)])
                    its = [(kc, mp) for kc in range(NT) for mp in range(2)]

                    def emit_score(kc, mp):
                        (sb_, sbi), _ = sbank.next()
                        qq = qa if mp == 0 else qb
                        S.op("pe", lambda e, sb_=sb_, kc=kc, qsl=qsl, qq=qq: e.matmul(sb_[:, :], kT[:, kc * 128:(kc + 1) * 128], qq[:, qsl], start=True, stop=True),
                             r=[("kT", kc // 4), ("qT", qc), "qz"], w=[("ps", sbi)])
                        return sb_, sbi

                    pend = [emit_score(*its[0]), emit_score(*its[1])]
                    for n_, (kc, mp) in enumerate(its):
                        sb_, sbi = pend.pop(0)
                        if n_ + 2 < len(its):
                            pend.append(emit_score(*its[n_ + 2]))
                        if True:
                            P_, Pk = Pb.next()
                            S.op("act", lambda e, P_=P_, sb_=sb_: e.activation(P_, sb_[:, :], AF.Exp, scale=0.125), r=[("ps", sbi)], w=[Pk])
                            for sub in range(4):
                                ab, abi = accb[mp * 2 + sub // 2]
                                co = (sub % 2) * 256
                                S.op("pe", lambda e, ab=ab, co=co, P_=P_, sub=sub, kc=kc, hd=hd: e.matmul(ab[:, co:co + 130], P_[:, sub * 128:(sub + 1) * 128], Va[:, kc, hd, 0:130],
                                                                                                            start=False, stop=(kc == NT - 1), skip_group_check=True),
                                     r=[Pk, ("Va", kc), "Va1"], w=[("ps", abi)])
                    for sub in range(4):
                        i = qc * 4 + sub
                        co = (sub % 2) * 256
                        a1b, a1i = accb[sub // 2]
                        a2b, a2i = accb[2 + sub // 2]
                        s_, sk = sm.next()
                        o_, ok = osb.next()
                        t2, t2k = t2b.next()
                        S.op("dve", lambda e, s_=s_, a1b=a1b, co=co: e.reciprocal(s_[:, 0:1], a1b[:, co + 128:co + 129]), r=[("ps", a1i)], w=[sk])
                        S.op("dve", lambda e, s_=s_, a2b=a2b, co=co: e.reciprocal(s_[:, 1:2], a2b[:, co + 128:co + 129]), r=[("ps", a2i)], w=[sk])
                        S.op("dve", lambda e, s_=s_: e.tensor_tensor(s_[:, 2:3], s_[:, 1:2], neglam, ALU.mult), r=[sk, "lam"], w=[sk])
                        S.op("dve", lambda e, t2=t2, a2b=a2b, co=co, s_=s_: e.tensor_scalar(t2, a2b[:, co:co + 128], s_[:, 2:3], None, ALU.mult), r=[("ps", a2i), sk], w=[t2k])
                        S.op("dve", lambda e, o_=o_, a1b=a1b, co=co, s_=s_, t2=t2: e.scalar_tensor_tensor(o_, a1b[:, co:co + 128], s_[:, 0:1], t2, ALU.mult, ALU.add),
                             r=[("ps", a1i), sk, t2k], w=[ok])
                        S.op("act", lambda e, o_=o_, s_=s_: e.activation(junk, o_, AF.Square, accum_out=s_[:, 3:4]), r=[ok], w=[sk, "junk"])
                        S.op("dve", lambda e, s_=s_: e.tensor_scalar(s_[:, 4:5], s_[:, 3:4], 1.0 / 128.0, 1e-5, ALU.mult, ALU.add), r=[sk], w=[sk])
                        S.op("act", lambda e, s_=s_: e.activation(s_[:, 5:6], s_[:, 4:5], AF.Sqrt), r=[sk], w=[sk])
                        S.op("dve", lambda e, s_=s_: e.reciprocal(s_[:, 6:7], s_[:, 5:6]), r=[sk], w=[sk])
                        S.op("dve", lambda e, o_=o_, s_=s_: e.scalar_tensor_tensor(o_, o_, s_[:, 6:7], gsub, ALU.mult, ALU.mult), r=[ok, sk, "gsub"], w=[ok])
                        (tb, tbi), _ = tbank7.next()
                        S.op("pe", lambda e, tb=tb, o_=o_: e.transpose(tb[:, 0:128], o_, ident), r=[ok, "ident"], w=[("ps", tbi)])
                        S.op("act", lambda e, tb=tb, i=i: e.copy(mst[:, i * 128:(i + 1) * 128], tb[:, 0:128]), r=[("ps", tbi)], w=[("mstB", i)])
                S.dma("sp", MIXT[2 + hd], mst, r=[("mstB", i) for i in range(NT)], w=[("MIXT", 2 + hd)])
            A.pop()
            S.barrier()
        if dbg_stop(l, "B"):
            break

        for _ph in ([0] if "C" not in skip else []):
            A.push()
            LE = T + 32
            ub = A.alloc([128, LE], F32)
            wa = A.alloc([128, LE], F32)
            wb = A.alloc([128, LE], F32)
            yb = A.alloc([128, T], F32)
            bd = A.alloc([128, 128], F32)
            pcol = A.alloc([128, 2], F32)
            prc = A.alloc([128, 16], F32)
            etmp = A.alloc([128, 16], F32)
            mst = A.alloc([128, T], BF16)
            wch = Rot("wch", [A.alloc([128, 8, 128], BF16) for _ in range(2)])
            pbank = Rot("ps", [(PS[0], 0), (PS[1], 1)])
            obank = Rot("ps", [(PS[2], 2), (PS[3], 3)])
            wins = (2, 4, 8, 16)
            for cp in range(2):
                c0 = O_P + cp * 128
                w_, wk = wch.next()
                load_w(w_, dr["w_in"][l][:, c0:c0 + 128], wk)
                S.op("dve", lambda e: e.memset(ub, 0.0), w=["pub"])
                S.op("dve", lambda e: e.memset(wa, 0.0), w=["pwa"])
                S.op("dve", lambda e: e.memset(wb, 0.0), w=["pwb"])
                S.op("dve", lambda e: e.memset(bd, 0.0), w=["bd"])
                S.dma("sp", bd[0:64, 0:64], dr["pool_w"][l, 2 * cp], w=["bd"])
                S.dma("sp", bd[64:128, 64:128], dr["pool_w"][l, 2 * cp + 1], w=["bd"])
                S.dma("sp", pcol[:, 0:1], col(dr["pool_b"][l].rearrange("g c -> (g c)")[cp * 128:(cp + 1) * 128]), w=["pcol"])
                S.dma("sp", pcol[:, 1:2], col(dr["pool_scale"][l, cp * 128:(cp + 1) * 128]), w=["pcol"])
                S.dma("sp", prc, dr["poolrc"][cp], w=["prc"])
                for tc in range(8):
                    (pb, pbi), _ = pbank.next()
                    for k in range(8):
                        S.op("pe", lambda e, pb=pb, k=k, tc=tc, w_=w_: e.matmul(pb[:, :], w_[:, k, :], hT[:, k, tc * 512:(tc + 1) * 512], start=(k == 0), stop=(k == 7)),
                             r=[wk] + hTk([k], range(tc * 4, tc * 4 + 4)), w=[("ps", pbi)])
                    S.op("act", lambda e, pb=pb, tc=tc: e.copy(ub[:, 16 + tc * 512:16 + (tc + 1) * 512], pb[:, :]), r=[("ps", pbi)], w=["pub"])
                R0, R1 = 8, LE - 8
                n = R1 - R0
                S.op("dve", lambda e: e.tensor_tensor(wa[:, R0:R1], ub[:, R0 - 1:R1 - 1], ub[:, R0:R1], ALU.add), r=["pub"], w=["pwa"])
                S.op("dve", lambda e: e.tensor_tensor(wb[:, R0:R1], wa[:, R0 - 1:R1 - 1], wa[:, R0 + 1:R1 + 1], ALU.add), r=["pwa"], w=["pwb"])
                if cp == 0:
                    srcs = (wa, "pwa"), (wb, "pwb")
                else:
                    S.op("dve", lambda e: e.tensor_tensor(wa[:, R0:R1], wb[:, R0 - 2:R1 - 2], wb[:, R0 + 2:R1 + 2], ALU.add), r=["pwb"], w=["pwa"])
                    S.op("dve", lambda e: e.tensor_tensor(wb[:, R0:R1], wa[:, R0 - 4:R1 - 4], wa[:, R0 + 4:R1 + 4], ALU.add), r=["pwa"], w=["pwb"])
                    srcs = (wa, "pwa"), (wb, "pwb")
                for half in range(2):
                    rows = slice(half * 64, (half + 1) * 64)
                    src, srck = srcs[half]
                    wsz = wins[2 * cp + half]
                    S.op("dve", lambda e, rows=rows, src=src, wsz=wsz: e.scalar_tensor_tensor(yb[rows, :], src[rows, 16:16 + T], 1.0 / wsz, ub[rows, 16:16 + T], ALU.mult, ALU.subtract),
                         r=[srck, "pub"], w=["yb"])
                    for (e0, t0) in ((0, 0), (8, T - 8)):
                        S.op("dve", lambda e, rows=rows, src=src, e0=e0, t0=t0: e.tensor_tensor(etmp[rows, e0:e0 + 8], src[rows, 16 + t0:16 + t0 + 8], prc[rows, e0:e0 + 8], ALU.mult),
                             r=[srck, "prc"], w=["etmp"])
                        S.op("dve", lambda e, rows=rows, e0=e0, t0=t0: e.tensor_tensor(yb[rows, t0:t0 + 8], etmp[rows, e0:e0 + 8], ub[rows, 16 + t0:16 + t0 + 8], ALU.subtract),
                             r=["etmp", "pub", "yb"], w=["yb"])
                for tc in range(8):
                    (ob, obi), _ = obank.next()
                    S.op("pe", lambda e, ob=ob, tc=tc: e.matmul(ob[:, :], bd, yb[:, tc * 512:(tc + 1) * 512], start=True, stop=True), r=["bd", "yb"], w=[("ps", obi)])
                    S.op("dve", lambda e, ob=ob, tc=tc: e.tensor_scalar(mst[:, tc * 512:(tc + 1) * 512], ob[:, :], pcol[:, 0:1], pcol[:, 1:2], ALU.add, ALU.mult),
                         r=[("ps", obi), "pcol"], w=["mstC"])
                S.dma("sp", MIXT[6 + cp], mst, r=["mstC"], w=[("MIXT", 6 + cp)])
            A.pop()
            S.barrier()
        if dbg_stop(l, "C"):
            break

        A.push()
        wo = A.alloc([128, 8, D], BF16)
        for hh in range(2):
            S.dma("pool", wo[:, :, hh * 512:(hh + 1) * 512], dr["w_out"][l][:, hh * 512:(hh + 1) * 512].rearrange("(k p) n -> p k n", p=128), w=["wo"])
        g_rep = A.alloc([128, D], F32)
        b_rep = A.alloc([128, D], F32)
        S.dma("sp", g_rep, dr["ln1_g"][l].partition_broadcast(128), w=["gb1"])
        S.dma("sp", b_rep, dr["ln1_b"][l].partition_broadcast(128), w=["gb1"])
        wg_f = A.alloc([128, 8, 20], F32)
        S.dma("sp", wg_f[:, :, 0:4], dr["moe_wgc"][l].rearrange("(k p) n -> p k n", p=128), w=["wg_f"])
        S.dma("sp", wg_f[:, :, 4:20], dr["moe_wgf"][l].rearrange("(k p) n -> p k n", p=128), w=["wg_f"])
        bg_rep = A.alloc([128, 20], F32)
        S.dma("sp", bg_rep[:, 0:4], dr["moe_bgc"][l].partition_broadcast(128), w=["bg_rep"])
        S.dma("sp", bg_rep[:, 4:20], dr["moe_bgf"][l].partition_broadcast(128), w=["bg_rep"])
        logits = A.alloc([128, NT, 20], F32)
        lnb = make_ln_bufs()
        gating = dict(hTf=Rot("hTf", [A.alloc([128, 8, 128], F32) for _ in range(2)]), bank=(PS[4], 4), wg=wg_f, bg=bg_rep, logits=logits, cur=None)
        import os as _os
        DBG_D = _os.environ.get("DBG_D", "").split(",")
        if "nogate" in DBG_D:
            gating = None
        mt = Rot("mt", [A.alloc([128, 8, 128], BF16) for _ in range(2)])
        ht = Rot("ht", [A.alloc([128, D], F32) for _ in range(2)])
        rt = Rot("rt", [A.alloc([128, D], F32) for _ in range(2)])
        obank = Rot("ps", [(PS[0], 0), (PS[1], 1), (PS[2], 2), (PS[3], 3)])
        MIXr = MIXT.rearrange("c p t -> p c t")
        for i in range(NT):
            m_, mk = mt.next()
            h_, hk = ht.next()
            r_, rk = rt.next()
            if "nomixdma" not in DBG_D:
                S.dma("sp", m_, MIXr[:, :, i * 128:(i + 1) * 128], r=[("MIXT", c) for c in range(8)], w=[mk])
            S.dma("act", h_, H32[i * 128:(i + 1) * 128, :], r=[("H32", i)], w=[hk])
            for half in range(2):
                (ob, obi), _ = obank.next()
                if "nomm" in DBG_D:
                    S.op("dve", lambda e, h_=h_, r_=r_, half=half: e.tensor_scalar(r_[:, half * 512:(half + 1) * 512], h_[:, half * 512:(half + 1) * 512], ALPHA, None, ALU.mult), r=[hk], w=[rk])
                    continue
                for k in range(8):
                    S.op("pe", lambda e, ob=ob, m_=m_, k=k, half=half: e.matmul(ob[:, :], m_[:, k, :], wo[:, k, half * 512:(half + 1) * 512], start=(k == 0), stop=(k == 7)),
                         r=[mk, "wo"], w=[("ps", obi)])
                if half == 0:
                    S.op("act", lambda e, h_=h_: e.mul(h_, h_, ALPHA), r=[hk], w=[hk])
                S.op("dve", lambda e, ob=ob, h_=h_, r_=r_, half=half: e.tensor_tensor(r_[:, half * 512:(half + 1) * 512], ob[:, :], h_[:, half * 512:(half + 1) * 512], ALU.add),
                     r=[("ps", obi), hk], w=[rk])
            ln_tile(lnb, r_, [rk], i, g_rep, b_rep, "gb1", H32, "H32", gating=gating)
        if "nogate" in DBG_D or "noroute" in DBG_D:
            A.pop()
            S.barrier()
            break
        rA = A.alloc([128, NT, 16], F32)
        rB = A.alloc([128, NT, 16], F32)
        rC = A.alloc([128, NT, 16], F32)
        r1 = A.alloc([128, NT, 8], F32)
        lkeys = [("logits", i) for i in range(NT)]
        lc = logits[:, :, 0:4]
        lf = logits[:, :, 4:20]
        BIG = 1.0e30
        mc = r1[:, :, 0]
        S.op("dve", lambda e: e.tensor_reduce(mc, lc, AX.X, ALU.max), r=lkeys, w=["r1"])
        ec = rA[:, :, 0:4]
        S.op("dve", lambda e: e.tensor_tensor(ec, lc, mc.unsqueeze(2).broadcast_to([128, NT, 4]), ALU.subtract), r=lkeys + ["r1"], w=["rA"])
        S.op("act", lambda e: e.activation(ec, ec, AF.Exp), r=["rA"], w=["rA"])
        S.op("dve", lambda e: e.tensor_reduce(r1[:, :, 1], ec, AX.X, ALU.add), r=["rA"], w=["r1"])
        S.op("dve", lambda e: e.reciprocal(r1[:, :, 2], r1[:, :, 1]), r=["r1"], w=["r1"])
        oh = rA[:, :, 4:8]
        S.op("dve", lambda e: e.tensor_tensor(oh, lc, mc.unsqueeze(2).broadcast_to([128, NT, 4]), ALU.is_equal), r=lkeys + ["r1", "rA"], w=["rA"])
        pen = rA[:, :, 8:12]
        S.op("dve", lambda e: e.tensor_scalar(pen, oh, 1.0, BIG, ALU.subtract, ALU.mult), r=["rA"], w=["rA"])
        lfm = rB
        S.op("dve", lambda e: e.tensor_tensor(lfm.rearrange("p t (g j) -> p t g j", g=4), lf.rearrange("p t (g j) -> p t g j", g=4),
                                              pen.unsqueeze(3).broadcast_to([128, NT, 4, 4]), ALU.add), r=lkeys + ["rA"], w=["rB"])
        S.op("dve", lambda e: e.tensor_reduce(r1[:, :, 3], lfm, AX.X, ALU.max), r=["rB"], w=["r1"])
        eq1 = rC
        S.op("dve", lambda e: e.tensor_tensor(eq1, lfm, r1[:, :, 3].unsqueeze(2).broadcast_to([128, NT, 16]), ALU.is_equal), r=["rB", "r1"], w=["rC"])
        S.op("dve", lambda e: e.scalar_tensor_tensor(lfm, eq1, -BIG, lfm, ALU.mult, ALU.add), r=["rB", "rC"], w=["rB"])
        S.op("dve", lambda e: e.tensor_reduce(r1[:, :, 4], lfm, AX.X, ALU.max), r=["rB"], w=["r1"])
        eq2 = rA
        S.op("dve", lambda e: e.tensor_tensor(eq2, lfm, r1[:, :, 4].unsqueeze(2).broadcast_to([128, NT, 16]), ALU.is_equal), r=["rB", "r1", "rA"], w=["rA"])
        S.op("dve", lambda e: e.tensor_tensor(r1[:, :, 5], r1[:, :, 4], r1[:, :, 3], ALU.subtract), r=["r1"], w=["r1"])
        S.op("act", lambda e: e.activation(r1[:, :, 5], r1[:, :, 5], AF.Exp), r=["r1"], w=["r1"])
        S.op("dve", lambda e: e.tensor_scalar(r1[:, :, 6], r1[:, :, 5], 1.0, None, ALU.add), r=["r1"], w=["r1"])
        S.op("dve", lambda e: e.reciprocal(r1[:, :, 6], r1[:, :, 6]), r=["r1"], w=["r1"])
        S.op("dve", lambda e: e.tensor_tensor(r1[:, :, 7], r1[:, :, 5], r1[:, :, 6], ALU.mult), r=["r1"], w=["r1"])
        S.op("dve", lambda e: e.tensor_tensor(r1[:, :, 6], r1[:, :, 6], r1[:, :, 2], ALU.mult), r=["r1"], w=["r1"])
        S.op("dve", lambda e: e.tensor_tensor(r1[:, :, 7], r1[:, :, 7], r1[:, :, 2], ALU.mult), r=["r1"], w=["r1"])
        S.op("dve", lambda e: e.tensor_tensor(eq1, eq1, r1[:, :, 6].unsqueeze(2).broadcast_to([128, NT, 16]), ALU.mult), r=["rC", "r1"], w=["rC"])
        S.op("dve", lambda e: e.tensor_tensor(eq2, eq2, r1[:, :, 7].unsqueeze(2).broadcast_to([128, NT, 16]), ALU.mult), r=["rA", "r1"], w=["rA"])
        S.op("dve", lambda e: e.tensor_tensor(gates, eq1, eq2, ALU.add), r=["rC", "rA"], w=["gates"])
        A.pop()
        S.barrier()
        if dbg_stop(l, "D"):
            break

        A.push()
        wgp = A.alloc([128, 8, D], BF16)
        wpp = A.alloc([128, 2, D], BF16)
        for hh in range(2):
            S.dma("pool", wgp[:, :, hh * 512:(hh + 1) * 512], dr["ple_wg"][l][:, hh * 512:(hh + 1) * 512].rearrange("(k p) n -> p k n", p=128), w=["wgp"])
        S.dma("pool", wpp, dr["ple_wp"][l].rearrange("(k p) n -> p k n", p=128), w=["wpp"])
        bgp = A.alloc([128, D], F32)
        S.dma("sp", bgp, dr["ple_bg"][l].partition_broadcast(128), w=["bgp"])
        g_rep = A.alloc([128, D], F32)
        b_rep = A.alloc([128, D], F32)
        S.dma("sp", g_rep, dr["ln2_g"][l].partition_broadcast(128), w=["gb2"])
        S.dma("sp", b_rep, dr["ln2_b"][l].partition_broadcast(128), w=["gb2"])
        lnb = make_ln_bufs()
        yacc = A.alloc([128, 8, D], F32)
        w13 = Rot("w13", [A.alloc([128, 2, 8, 256], BF16) for _ in range(2)])
        w2r = Rot("w2r", [A.alloc([128, 2, D], BF16) for _ in range(2)])
        actT = Rot("actT", [A.alloc([128, 2, 1024], BF16) for _ in range(2)])
        sa = Rot("sa", [A.alloc([128, 512], F32) for _ in range(3)])
        pt = Rot("pt", [A.alloc([128, 256], F32) for _ in range(2)])
        pT = Rot("pT", [A.alloc([128, 2, 128], BF16) for _ in range(2)])
        ht = Rot("htE", [A.alloc([128, D], F32) for _ in range(2)])
        sg = Rot("sg", [A.alloc([128, 512], F32) for _ in range(2)])
        b01 = Rot("ps", [(PS[0], 0), (PS[1], 1)])
        b23 = Rot("ps", [(PS[2], 2), (PS[3], 3)])
        b45 = Rot("ps", [(PS[4], 4), (PS[5], 5)])
        dstD = out if last else H32
        for tcb in range(4):
            tiles = range(tcb * 8, tcb * 8 + 8)
            for ii, i in enumerate(tiles):
                p_, pk = pt.next()
                S.dma("sp", p_, dr["p"][l, i * 128:(i + 1) * 128, :], w=[pk])
                h_, hk = ht.next()
                S.dma("act", h_, H32[i * 128:(i + 1) * 128, :], r=[("H32", i)], w=[hk])
                (tb, tbi), _ = b45.next()
                for kk in range(2):
                    S.op("pe", lambda e, tb=tb, p_=p_, kk=kk: e.transpose(tb[:, kk * 128:(kk + 1) * 128], p_[:, kk * 128:(kk + 1) * 128], ident), r=[pk, "ident"], w=[("ps", tbi)])
                pT_, pTk = pT.next()
                S.op("act", lambda e, pT_=pT_, tb=tb: e.copy(pT_, tb[:, 0:256].rearrange("p (a b) -> p a b", a=2)), r=[("ps", tbi)], w=[pTk])
                for half in range(2):
                    hs = slice(half * 512, (half + 1) * 512)
                    (pg, pgi), _ = b01.next()
                    (pp, ppi), _ = b23.next()
                    for k in range(8):
                        S.op("pe", lambda e, pg=pg, k=k, i=i, hs=hs: e.matmul(pg[:, :], hT[:, k, i * 128:(i + 1) * 128], wgp[:, k, hs], start=(k == 0), stop=(k == 7)),
                             r=["wgp"] + hTk([k], [i]), w=[("ps", pgi)])
                    for kk in range(2):
                        S.op("pe", lambda e, pp=pp, kk=kk, pT_=pT_, hs=hs: e.matmul(pp[:, :], pT_[:, kk, :], wpp[:, kk, hs], start=(kk == 0), stop=(kk == 1)),
                             r=["wpp", pTk], w=[("ps", ppi)])
                    s_, sk = sg.next()
                    S.op("dve", lambda e, s_=s_, pg=pg, hs=hs: e.tensor_tensor(s_, pg[:, :], bgp[:, hs], ALU.add), r=[("ps", pgi), "bgp"], w=[sk])
                    S.op("act", lambda e, s_=s_: e.activation(s_, s_, AF.Sigmoid), r=[sk], w=[sk])
                    S.op("dve", lambda e, s_=s_, pp=pp: e.tensor_tensor(s_, s_, pp[:, :], ALU.mult), r=[sk, ("ps", ppi)], w=[sk])
                    S.op("dve", lambda e, s_=s_, h_=h_, ii=ii, hs=hs: e.scalar_tensor_tensor(yacc[:, ii, hs], h_[:, hs], ALPHA, s_, ALU.mult, ALU.add),
                         r=[sk, hk], w=[("yacc", ii, half)])
            for ex in range(16):
                w13_, w13k = w13.next()
                w2_, w2k = w2r.next()
                S.dma("pool", w13_[:, 0], dr["moe_w1"][l, ex].rearrange("(k p) n -> p k n", p=128), w=[w13k + (0,)])
                S.dma("pool", w13_[:, 1], dr["moe_w3"][l, ex].rearrange("(k p) n -> p k n", p=128), w=[w13k + (1,)])
                S.dma("pool", w2_, dr["moe_w2"][l, ex].rearrange("(k p) n -> p k n", p=128), w=[w2k])
                aT, aTk = actT.next()
                for sub in range(2):
                    tsl = slice(tcb * 1024 + sub * 512, tcb * 1024 + (sub + 1) * 512)
                    tl = range(tcb * 8 + sub * 4, tcb * 8 + sub * 4 + 4)
                    for fc in range(2):
                        (pa, pai), _ = b01.next()
                        (pc, pci), _ = b23.next()
                        for k in range(8):
                            S.op("pe", lambda e, pa=pa, k=k, fc=fc, tsl=tsl, w13_=w13_: e.matmul(pa[:, :], w13_[:, 0, k, fc * 128:(fc + 1) * 128], hT[:, k, tsl], start=(k == 0), stop=(k == 7)),
                                 r=[w13k + (0,)] + hTk([k], tl), w=[("ps", pai)])
                        for k in range(8):
                            S.op("pe", lambda e, pc=pc, k=k, fc=fc, tsl=tsl, w13_=w13_: e.matmul(pc[:, :], w13_[:, 1, k, fc * 128:(fc + 1) * 128], hT[:, k, tsl], start=(k == 0), stop=(k == 7)),
                                 r=[w13k + (1,)] + hTk([k], tl), w=[("ps", pci)])
                        s_, sk = sa.next()
                        S.op("act", lambda e, s_=s_, pa=pa: e.activation(s_, pa[:, :], AF.Silu), r=[("ps", pai)], w=[sk])
                        S.op("dve", lambda e, s_=s_, pc=pc, aT=aT, fc=fc, sub=sub: e.tensor_tensor(aT[:, fc, sub * 512:(sub + 1) * 512], s_, pc[:, :], ALU.mult),
                             r=[sk, ("ps", pci)], w=[aTk + (sub,)])
                for ii, i in enumerate(tiles):
                    for half in range(2):
                        hs = slice(half * 512, (half + 1) * 512)
                        (py, pyi), _ = b45.next()
                        for fc in range(2):
                            S.op("pe", lambda e, py=py, aT=aT, fc=fc, ii=ii, hs=hs, w2_=w2_: e.matmul(py[:, :], aT[:, fc, ii * 128:(ii + 1) * 128], w2_[:, fc, hs], start=(fc == 0), stop=(fc == 1)),
                                 r=[aTk + (ii // 4,), w2k], w=[("ps", pyi)])
                        S.op("dve", lambda e, py=py, ii=ii, hs=hs, i=i, ex=ex: e.scalar_tensor_tensor(yacc[:, ii, hs], py[:, :], gates[:, i, ex:ex + 1], yacc[:, ii, hs], ALU.mult, ALU.add),
                             r=[("ps", pyi), ("yacc", ii, half)], w=[("yacc", ii, half)])
            for ii, i in enumerate(tiles):
                tok = ln_tile(lnb, yacc[:, ii, :], [("yacc", ii, 0), ("yacc", ii, 1)], i, g_rep, b_rep, "gb2", dstD, "OUT" if last else "H32")
                if last:
                    final_tokens.append(tok)
        A.pop()
        S.barrier()
        if dbg_stop(l, "E"):
            break

    for h in S.dma_hist:
        if h:
            final_tokens.append(h[-1])
    S.emit(final_tokens=final_tokens)
    es.close()
    return nc


_NC_CACHE = {}


def make_in_maps(inputs, nld=DEPTH):
    c = host_consts()
    x = np.ascontiguousarray(np.asarray(inputs["x"], dtype=np.float32))
    p = np.asarray(inputs["p"], dtype=np.float32)[:nld]
    shared = {}
    for n, sh in WEIGHT_SPECS:
        a = np.asarray(inputs[n], dtype=np.float32)
        if sh[0] == DEPTH and n not in ("ln0_g", "ln0_b"):
            a = a[:nld]
        shared[n] = np.ascontiguousarray(a)
    for n, _, _ in CONST_SPECS:
        shared[n] = c[n]
    in_maps = []
    for core in range(8):
        b = core % 4
        m = dict(shared)
        m["x"] = x[b]
        m["p"] = np.ascontiguousarray(p[:, b])
        in_maps.append(m)
    return in_maps


def kernel(**inputs):
    if "nc" not in _NC_CACHE:
        _NC_CACHE["nc"] = build()
    nc = _NC_CACHE["nc"]
    in_maps = make_in_maps(inputs)
    res = run_bass_kernel_spmd(nc, in_maps, core_ids=list(range(8)))
    outs = [np.asarray(res.results[b]["out"], dtype=np.float32) for b in range(4)]
    return np.stack(outs, 0)
```
